# Optimizing a Trainium2 kernel written in Bass

```python
import math
import jax, jax.numpy as jnp
from jax import lax
import numpy as np

D_MODEL = 2048
BATCH = 8
SEQ = 2048
DEPTH = 2

GRID_W = 64
CTX_LEN = 256
N_GROUPS = 4
GROUP_W = D_MODEL // N_GROUPS
MIX_W = N_GROUPS * GROUP_W
HEAD_DIM = 64
CONV_A_WIDTH = 31
DIFF_HEADS = GROUP_W // (2 * HEAD_DIM)
ROPE_BASE = 10000.0
Q_BLOCK = 128
CONV_C_WIDTH = 3
LRU_BLOCKS = GROUP_W // HEAD_DIM
LRU_CONV_WIDTH = 4
LRU_C = 8.0
N_EXPERTS = 32
TOP_K = 4
D_FF = D_MODEL // 2
SWIGLU_LIMIT = 7.0
SWIGLU_ALPHA = 1.702
EPS = 1e-6
COLS_A = 2 * GROUP_W
COLS_B = 3 * GROUP_W
COLS_C = 3 * GROUP_W
COLS_D = 2 * GROUP_W
N_IN = COLS_A + COLS_B + COLS_C + COLS_D
COL_SPLITS = (COLS_A, COLS_A + COLS_B, COLS_A + COLS_B + COLS_C)
F32 = jnp.float32

kernel_name = 'hybrid_parallel_groups_moe_dit_block'


def rms_norm(x, g):
    xf = x.astype(F32)
    y = xf * lax.rsqrt(jnp.mean(xf * xf, axis=-1, keepdims=True) + EPS)
    return (y * g).astype(x.dtype)


def layer_norm(x, g, b):
    xf = x.astype(F32)
    mu = jnp.mean(xf, axis=-1, keepdims=True)
    var = jnp.mean(jnp.square(xf - mu), axis=-1, keepdims=True)
    return ((xf - mu) * lax.rsqrt(var + EPS) * g + b).astype(x.dtype)


def depthwise_conv(x, w, pad):
    return lax.conv_general_dilated(
        x, w[:, None, :].astype(x.dtype), (1,), [pad],
        dimension_numbers=('NWC', 'WIO', 'NWC'), feature_group_count=x.shape[-1])


def axial_rope_tables(seq_len):
    rows = seq_len // GRID_W
    row = jnp.repeat(jnp.arange(rows, dtype=F32), GRID_W)
    col = jnp.tile(jnp.arange(GRID_W, dtype=F32), rows)
    half = HEAD_DIM // 2
    inv = ROPE_BASE ** (-jnp.arange(0, half, 2, dtype=F32) / half)
    ar = row[:, None] * inv
    ac = col[:, None] * inv
    ang = jnp.concatenate([ar, ar, ac, ac], axis=-1)
    return jnp.cos(ang), jnp.sin(ang)


def apply_axial_rope(x, cos, sin):
    xf = x.astype(F32)
    x1, x2, x3, x4 = jnp.split(xf, 4, axis=-1)
    rot = jnp.concatenate([-x2, x1, -x4, x3], axis=-1)
    return (xf * cos[:, None, :] + rot * sin[:, None, :]).astype(x.dtype)


def conformer_conv(u, conv_w, conv_b, ln_g, ln_b):
    val, gate = jnp.split(u, 2, axis=-1)
    z = val * jax.nn.sigmoid(gate)
    half = (CONV_A_WIDTH - 1) // 2
    z = depthwise_conv(z, conv_w, (half, half)) + conv_b
    return jax.nn.silu(layer_norm(z, ln_g, ln_b))


def split_diff_heads(u):
    bsz, n, _ = u.shape
    q, k, v = jnp.split(u, 3, axis=-1)
    return (q.reshape(bsz, n, 2 * DIFF_HEADS, HEAD_DIM),
            k.reshape(bsz, n, 2 * DIFF_HEADS, HEAD_DIM),
            v.reshape(bsz, n, DIFF_HEADS, 2 * HEAD_DIM))


def diff_softmax_attend(q, k, v, lam):
    s = jnp.einsum('bqhd,bkhd->bhqk', q, k).astype(F32) * (HEAD_DIM ** -0.5)
    p = jax.nn.softmax(s, axis=-1)
    bsz, _, lq, lk = p.shape
    p = p.reshape(bsz, DIFF_HEADS, 2, lq, lk)
    w = (p[:, :, 0] - lam * p[:, :, 1]).astype(v.dtype)
    return jnp.einsum('bhqk,bkhe->bqhe', w, v)


def diff_head_norm(o, g, lam_init):
    o = rms_norm(o, g) * (1.0 - lam_init)
    return o.reshape(o.shape[0], o.shape[1], DIFF_HEADS * 2 * HEAD_DIM)


def diff_attention(ub, cb, lam_vecs, norm_g, lam_init, cos, sin, need_ctx):
    q, k, v = split_diff_heads(ub)
    qc, kc, vc = split_diff_heads(cb)
    q = apply_axial_rope(q, cos, sin)
    k = apply_axial_rope(k, cos, sin)
    lv = lam_vecs.astype(F32)
    lam = jnp.exp(jnp.sum(lv[0] * lv[1])) - jnp.exp(jnp.sum(lv[2] * lv[3])) + lam_init
    k_all = jnp.concatenate([kc, k], axis=1)
    v_all = jnp.concatenate([vc, v], axis=1)
    bsz, seq_len = q.shape[0], q.shape[1]
    n_blk = seq_len // Q_BLOCK
    q_blocks = q.reshape(bsz, n_blk, Q_BLOCK, 2 * DIFF_HEADS, HEAD_DIM).swapaxes(0, 1)
    o = lax.map(lambda qb: diff_softmax_attend(qb, k_all, v_all, lam), q_blocks)
    o = o.swapaxes(0, 1).reshape(bsz, seq_len, DIFF_HEADS, 2 * HEAD_DIM)
    y = diff_head_norm(o, norm_g, lam_init)
    y_ctx = diff_head_norm(diff_softmax_attend(qc, kc, vc, lam), norm_g, lam_init) if need_ctx else None
    return y, y_ctx


def gated_short_conv(u, conv_w, norm_g):
    bg, cg, v = jnp.split(u, 3, axis=-1)
    half = (CONV_C_WIDTH - 1) // 2
    y = bg * depthwise_conv(cg * v, conv_w, (half, half))
    return rms_norm(y, norm_g)


def linear_scan(a, b, h0, reverse):
    idx = -1 if reverse else 0
    b = b.at[:, idx].add(a[:, idx] * h0)

    def combine(left, right):
        a_l, b_l = left
        a_r, b_r = right
        return a_l * a_r, a_r * b_l + b_r

    _, h = lax.associative_scan(combine, (a, b), reverse=reverse, axis=1)
    return h


def rglru_direction(xr, conv_w, conv_b, wa, ba, wx, bx, lam, h0, reverse):
    pad = (0, LRU_CONV_WIDTH - 1) if reverse else (LRU_CONV_WIDTH - 1, 0)
    xcv = depthwise_conv(xr, conv_w, pad) + conv_b
    bsz, n, w = xcv.shape
    xh = xcv.reshape(bsz, n, LRU_BLOCKS, w // LRU_BLOCKS)
    r = jax.nn.sigmoid(jnp.einsum('blhi,hij->blhj', xh, wa).reshape(bsz, n, w) + ba)
    ig = jax.nn.sigmoid(jnp.einsum('blhi,hij->blhj', xh, wx).reshape(bsz, n, w) + bx)
    log_a = -LRU_C * r.astype(F32) * jax.nn.softplus(-lam.astype(F32))
    a = jnp.exp(log_a)
    b = jnp.sqrt(-jnp.expm1(2.0 * log_a)) * (ig * xcv).astype(F32)
    h = linear_scan(a, b, h0, reverse)
    h_last = h[:, 0] if reverse else h[:, -1]
    return h, h_last


def bidir_rglru(ud, cd, conv_w, conv_b, wa, ba, wx, bx, lam, norm_g, need_ctx):
    gate_l, rec_l = jnp.split(ud, 2, axis=-1)
    gate_c, rec_c = jnp.split(cd, 2, axis=-1)
    h0 = jnp.zeros((cd.shape[0], GROUP_W), F32)
    out_l = jnp.zeros(rec_l.shape, F32)
    out_c = jnp.zeros(rec_c.shape, F32)
    for d in range(2):
        rev = d == 1
        prm = (conv_w[d], conv_b[d], wa[d], ba[d], wx[d], bx[d], lam[d])
        hs_c, h_ctx_final = rglru_direction(rec_c, *prm, h0, rev)
        hs_l, _ = rglru_direction(rec_l, *prm, h_ctx_final, rev)
        out_l = out_l + hs_l
        out_c = out_c + hs_c
    y = rms_norm(jax.nn.gelu(gate_l) * out_l.astype(ud.dtype), norm_g)
    y_ctx = rms_norm(jax.nn.gelu(gate_c) * out_c.astype(cd.dtype), norm_g) if need_ctx else None
    return y, y_ctx


def token_mixers(h, hc, w_in, w_out, conv_a_w, conv_a_b, ln_a_g, ln_a_b, diff_lambda, diff_norm_g,
                 conv_c_w, norm_c_g, lru_conv_w, lru_conv_b, lru_wa, lru_ba, lru_wx, lru_bx,
                 lru_lam, norm_d_g, cos, sin, lam_init, need_ctx):
    ua, ub, us, ud = jnp.split(h @ w_in, COL_SPLITS, axis=-1)
    ca, cb, cs, cd = jnp.split(hc @ w_in, COL_SPLITS, axis=-1)
    ya = conformer_conv(ua, conv_a_w, conv_a_b, ln_a_g, ln_a_b)
    yb, yb_c = diff_attention(ub, cb, diff_lambda, diff_norm_g, lam_init, cos, sin, need_ctx)
    ys = gated_short_conv(us, conv_c_w, norm_c_g)
    yd, yd_c = bidir_rglru(ud, cd, lru_conv_w, lru_conv_b, lru_wa, lru_ba, lru_wx, lru_bx,
                           lru_lam, norm_d_g, need_ctx)
    y = jnp.concatenate([ya, yb, ys, yd], axis=-1) @ w_out
    y_ctx = None
    if need_ctx:
        ya_c = conformer_conv(ca, conv_a_w, conv_a_b, ln_a_g, ln_a_b)
        ys_c = gated_short_conv(cs, conv_c_w, norm_c_g)
        y_ctx = jnp.concatenate([ya_c, yb_c, ys_c, yd_c], axis=-1) @ w_out
    return y, y_ctx


def moe_ffn(t, router_w, router_b, w1, b1, w2, b2):
    logits = (t @ router_w + router_b).astype(F32)
    top_v, top_i = lax.top_k(logits, TOP_K)
    gates = jax.nn.softmax(top_v, axis=-1)
    combine = jnp.einsum('tk,tke->te', gates, jax.nn.one_hot(top_i, N_EXPERTS, dtype=F32))
    acc = jnp.zeros(t.shape, F32)
    for e in range(N_EXPERTS):
        gu = t @ w1[e] + b1[e]
        g = jnp.minimum(gu[:, 0::2], SWIGLU_LIMIT)
        up = jnp.clip(gu[:, 1::2], -SWIGLU_LIMIT, SWIGLU_LIMIT)
        act = (up + 1.0) * (g * jax.nn.sigmoid(SWIGLU_ALPHA * g))
        acc = acc + combine[:, e:e + 1] * (act @ w2[e] + b2[e]).astype(F32)
    return acc.astype(t.dtype)


def setup_inputs(seed: int = 0) -> dict:
    key = jax.random.key(seed)
    ks = iter(jax.random.split(key, 48))

    def nrm(shape, s):
        return jax.random.normal(next(ks), shape, F32) * s

    L = DEPTH
    u = jax.random.uniform(next(ks), (L, 2, GROUP_W), F32, 0.9, 0.999)
    a = u ** (1.0 / LRU_C)
    lru_lam = jnp.log(a) - jnp.log1p(-a)
    return {
        'x': nrm((BATCH, SEQ, D_MODEL), 1.0),
        'c': nrm((BATCH, D_MODEL), 1.0),
        'ctx': nrm((BATCH, CTX_LEN, D_MODEL), 1.0),
        'c_ctx': nrm((D_MODEL,), 1.0),
        'ada_w': nrm((L, D_MODEL, 6 * D_MODEL), 0.5 * D_MODEL ** -0.5),
        'ada_b': nrm((L, 6 * D_MODEL), 0.02),
        'norm1_g': 1.0 + nrm((L, D_MODEL), 0.02),
        'norm2_g': 1.0 + nrm((L, D_MODEL), 0.02),
        'w_in': nrm((L, D_MODEL, N_IN), D_MODEL ** -0.5),
        'w_out': nrm((L, MIX_W, D_MODEL), MIX_W ** -0.5),
        'conv_a_w': nrm((L, CONV_A_WIDTH, GROUP_W), CONV_A_WIDTH ** -0.5),
        'conv_a_b': nrm((L, GROUP_W), 0.02),
        'ln_a_g': 1.0 + nrm((L, GROUP_W), 0.02),
        'ln_a_b': nrm((L, GROUP_W), 0.02),
        'diff_lambda': nrm((L, 4, HEAD_DIM), 0.1),
        'diff_norm_g': 1.0 + nrm((L, 2 * HEAD_DIM), 0.02),
        'conv_c_w': nrm((L, CONV_C_WIDTH, GROUP_W), CONV_C_WIDTH ** -0.5),
        'norm_c_g': 1.0 + nrm((L, GROUP_W), 0.02),
        'lru_conv_w': nrm((L, 2, LRU_CONV_WIDTH, GROUP_W), LRU_CONV_WIDTH ** -0.5),
        'lru_conv_b': nrm((L, 2, GROUP_W), 0.02),
        'lru_wa': nrm((L, 2, LRU_BLOCKS, HEAD_DIM, HEAD_DIM), HEAD_DIM ** -0.5),
        'lru_ba': nrm((L, 2, GROUP_W), 0.02),
        'lru_wx': nrm((L, 2, LRU_BLOCKS, HEAD_DIM, HEAD_DIM), HEAD_DIM ** -0.5),
        'lru_bx': nrm((L, 2, GROUP_W), 0.02),
        'lru_lam': lru_lam,
        'norm_d_g': 1.0 + nrm((L, GROUP_W), 0.02),
        'router_w': nrm((L, D_MODEL, N_EXPERTS), D_MODEL ** -0.5),
        'router_b': nrm((L, N_EXPERTS), 0.01),
        'exp_w1': nrm((L, N_EXPERTS, D_MODEL, 2 * D_FF), D_MODEL ** -0.5),
        'exp_b1': nrm((L, N_EXPERTS, 2 * D_FF), 0.02),
        'exp_w2': nrm((L, N_EXPERTS, D_FF, D_MODEL), D_FF ** -0.5),
        'exp_b2': nrm((L, N_EXPERTS, D_MODEL), 0.02),
        'final_g': 1.0 + nrm((D_MODEL,), 0.02),
    }


def reference(x, c, ctx, c_ctx, ada_w, ada_b, norm1_g, norm2_g, w_in, w_out,
              conv_a_w, conv_a_b, ln_a_g, ln_a_b, diff_lambda, diff_norm_g,
              conv_c_w, norm_c_g, lru_conv_w, lru_conv_b, lru_wa, lru_ba, lru_wx,
              lru_bx, lru_lam, norm_d_g, router_w, router_b, exp_w1, exp_b1,
              exp_w2, exp_b2, final_g):
    bsz, seq_len, _ = x.shape
    cos, sin = axial_rope_tables(seq_len)
    xc = ctx
    for i in range(DEPTH):
        need_ctx = i < DEPTH - 1
        lam_init = 0.8 - 0.6 * math.exp(-0.3 * i)
        mod = jax.nn.silu(c) @ ada_w[i] + ada_b[i]
        mod_c = jax.nn.silu(c_ctx) @ ada_w[i] + ada_b[i]
        sh1, sc1, g1, sh2, sc2, g2 = jnp.split(mod[:, None, :], 6, axis=-1)
        csh1, csc1, cg1, csh2, csc2, cg2 = jnp.split(mod_c, 6, axis=-1)
        h = rms_norm(x, norm1_g[i]) * (1.0 + sc1) + sh1
        hc = rms_norm(xc, norm1_g[i]) * (1.0 + csc1) + csh1
        y, y_ctx = token_mixers(h, hc, w_in[i], w_out[i], conv_a_w[i], conv_a_b[i], ln_a_g[i], ln_a_b[i],
                                diff_lambda[i], diff_norm_g[i], conv_c_w[i], norm_c_g[i],
                                lru_conv_w[i], lru_conv_b[i], lru_wa[i], lru_ba[i], lru_wx[i], lru_bx[i],
                                lru_lam[i], norm_d_g[i], cos, sin, lam_init, need_ctx)
        x = x + g1 * y
        h2 = rms_norm(x, norm2_g[i]) * (1.0 + sc2) + sh2
        moe_args = (router_w[i], router_b[i], exp_w1[i], exp_b1[i], exp_w2[i], exp_b2[i])
        if need_ctx:
            xc = xc + cg1 * y_ctx
            hc2 = rms_norm(xc, norm2_g[i]) * (1.0 + csc2) + csh2
            n_lat = bsz * seq_len
            tokens = jnp.concatenate([h2.reshape(n_lat, D_MODEL), hc2.reshape(-1, D_MODEL)], axis=0)
            f = moe_ffn(tokens, *moe_args)
            x = x + g2 * f[:n_lat].reshape(x.shape)
            xc = xc + cg2 * f[n_lat:].reshape(xc.shape)
        else:
            x = x + g2 * moe_ffn(h2.reshape(-1, D_MODEL), *moe_args).reshape(x.shape)
    return rms_norm(x, final_g)
```

```python
import math
import numpy as np
import concourse.bass as bass
import concourse.mybir as mybir
from concourse.bass_utils import run_bass_kernel_spmd

F32 = mybir.dt.float32
BF16 = mybir.dt.bfloat16
AF = mybir.ActivationFunctionType
ALU = mybir.AluOpType
AX = mybir.AxisListType

D = 2048
SEQ = 2048
CTX = 256
T = SEQ + CTX
NT = T // 128
DEPTH = 2
GW = 512
N_IN = 5120
NE = 32
DFF = 1024
EPS = 1e-6
KC = D // 128


class Buf:
    __slots__ = ("ap", "w", "r", "name")

    def __init__(self, ap, name=""):
        self.ap = ap
        self.w = None
        self.r = {}
        self.name = name

    def __getitem__(self, idx):
        return self.ap[idx]


class KB:
    RING = 8

    def __init__(self, nc):
        self.nc = nc
        self.eng = {"pe": nc.tensor, "act": nc.scalar, "dve": nc.vector, "pool": nc.gpsimd, "sp": nc.sync}
        self.csem = {}
        self.cnt = {}
        for e in ("pe", "act", "dve", "pool"):
            self.csem[e] = nc.alloc_semaphore("c_" + e)
            self.cnt[e] = 0
        self.pending = {e: False for e in self.cnt}
        self.rings = {}
        self.dcount = {}
        for q in ("sp", "act", "pool"):
            self.rings[q] = [nc.alloc_semaphore(f"r_{q}{i}") for i in range(self.RING)]
            self.dcount[q] = 0
        self.waited = {}
        self.n_ins = 0
        self.n_wait = 0
        self._uid = 0

    def sb(self, shape, dtype, name=None):
        self._uid += 1
        name = f"{name or 't'}_{self._uid}"
        return Buf(self.nc.alloc_sbuf_tensor(name, list(shape), dtype).ap(), name)

    def ps(self, shape, dtype=F32, name=None):
        self._uid += 1
        name = f"{name or 'p'}_{self._uid}"
        return Buf(self.nc.alloc_psum_tensor(name, list(shape), dtype).ap(), name)

    def dram(self, name, shape, dtype, kind="Internal"):
        return Buf(self.nc.dram_tensor(name, list(shape), dtype, kind=kind).ap(), name)

    def mark(self):
        nc = self.nc
        return (nc.sbuf_base, nc.sbuf_top, nc.psum_base, nc.psum_top)

    def release(self, m):
        self.barrier()
        nc = self.nc
        nc.sbuf_base, nc.sbuf_top, nc.psum_base, nc.psum_top = m

    def _wait(self, ename, ev):
        sem, val = ev
        key = (ename, sem.num if hasattr(sem, "num") else id(sem))
        if self.waited.get(key, 0) >= val:
            return
        self.eng[ename].wait_ge(sem, val)
        self.waited[key] = val
        self.n_wait += 1

    def _deps(self, ename, r, w):
        evs = []
        for b in r:
            if b.w is not None:
                evs.append(b.w)
        for b in w:
            if b.w is not None:
                evs.append(b.w)
            evs.extend(b.r.values())
        own = self.csem.get(ename)
        for ev in evs:
            if ename == "pe" and ev[0] is own:
                continue
            self._wait(ename, ev)

    def _record(self, ev, r, w):
        sid = id(ev[0])
        for b in r:
            cur = b.r.get(sid)
            if cur is None or cur[1] < ev[1]:
                b.r[sid] = ev
        for b in w:
            b.w = ev
            b.r = {}

    def op(self, ename, fn, r=(), w=(), signal=True):
        self._deps(ename, r, w)
        ins = fn(self.eng[ename])
        self.n_ins += 1
        if signal:
            self.cnt[ename] += 1
            ins.then_inc(self.csem[ename], 1)
            ev = (self.csem[ename], self.cnt[ename])
        else:
            ev = (self.csem[ename], self.cnt[ename] + 1)
        self._record(ev, r, w)
        return ins

    def dma(self, q, out, in_, r=(), w=(), **kw):
        self._deps(q, r, w)
        i = self.dcount[q]
        slot = i % self.RING
        sem = self.rings[q][slot]
        if i >= self.RING:
            self._wait(q, (sem, 16 * (i // self.RING)))
        ins = self.eng[q].dma_start(out=out, in_=in_, **kw)
        ins.then_inc(sem, 16)
        self.n_ins += 1
        self.dcount[q] = i + 1
        ev = (sem, 16 * (i // self.RING + 1))
        self._record(ev, r, w)
        return ev

    def all_events(self):
        evs = [(self.csem[e], self.cnt[e]) for e in self.cnt if self.cnt[e] > 0]
        for q in self.rings:
            n = self.dcount[q]
            for slot in range(self.RING):
                k = (n - slot + self.RING - 1) // self.RING
                if k > 0:
                    evs.append((self.rings[q][slot], 16 * k))
        return evs

    def barrier(self, engines=("pe", "act", "dve", "pool", "sp")):
        evs = self.all_events()
        for e in engines:
            for ev in evs:
                self._wait(e, ev)

    def mm(self, out_ap, lhsT, rhs, start, stop, r=(), w=(), signal=None, **kw):
        if signal is None:
            signal = True
        return self.op("pe", lambda e: e.matmul(out_ap, lhsT, rhs, start=start, stop=stop, **kw),
                       r=r, w=w, signal=signal)

    def act(self, out, in_, func, r=(), w=(), eng="act", **kw):
        return self.op(eng, lambda e: e.activation(out=out, in_=in_, func=func, **kw), r=r, w=w)

    def tt(self, eng, out, in0, in1, op, r=(), w=()):
        return self.op(eng, lambda e: e.tensor_tensor(out=out, in0=in0, in1=in1, op=op), r=r, w=w)

    def ts(self, eng, out, in0, s1, op0, s2=None, op1=None, r=(), w=(), **kw):
        if op1 is None:
            return self.op(eng, lambda e: e.tensor_scalar(out=out, in0=in0, scalar1=s1, scalar2=None,
                                                          op0=op0, **kw), r=r, w=w)
        return self.op(eng, lambda e: e.tensor_scalar(out=out, in0=in0, scalar1=s1, scalar2=s2,
                                                      op0=op0, op1=op1, **kw), r=r, w=w)

    def stt(self, out, in0, scalar, in1, op0, op1, r=(), w=(), **kw):
        return self.op("dve", lambda e: e.scalar_tensor_tensor(out=out, in0=in0, scalar=scalar, in1=in1,
                                                               op0=op0, op1=op1, **kw), r=r, w=w)

    def copy(self, eng, out, in_, r=(), w=()):
        if eng == "act":
            return self.op("act", lambda e: e.activation(out=out, in_=in_, func=AF.Copy), r=r, w=w)
        return self.op(eng, lambda e: e.tensor_copy(out=out, in_=in_), r=r, w=w)


def _pp_layout():
    cols = {}
    off = 0

    def add(name, n):
        nonlocal off
        cols[name] = (off, n)
        off += n

    add("cvec", 32)
    for l in range(DEPTH):
        add(f"conv_a_w{l}", 4 * 31)
        add(f"conv_a_b{l}", 4)
        add(f"ln_a_g{l}", 4)
        add(f"ln_a_b{l}", 4)
        add(f"diff_norm_g{l}", 1)
        add(f"conv_c_w{l}", 4 * 3)
        add(f"norm_c_g{l}", 4)
        add(f"lru_conv_w{l}", 2 * 4 * 4)
        add(f"lru_conv_b{l}", 8)
        add(f"lru_ba{l}", 8)
        add(f"lru_bx{l}", 8)
        add(f"lru_lam{l}", 8)
        add(f"norm_d_g{l}", 4)
        add(f"b1g{l}", NE * 8)
        add(f"b1u{l}", NE * 8)
    return cols, off


PP_COLS, NPP = _pp_layout()


def _pack_pp(inp, b):
    pp = np.zeros((128, NPP), np.float32)

    def put(name, arr):
        o, n = PP_COLS[name]
        pp[:, o:o + n] = np.ascontiguousarray(arr, dtype=np.float32).reshape(128, n)

    def chp(v):
        return np.asarray(v).reshape(4, 128).T

    cv = np.concatenate([np.asarray(inp["c"][b]).reshape(16, 128).T,
                         np.asarray(inp["c_ctx"]).reshape(16, 128).T], axis=1)
    put("cvec", cv)
    for l in range(DEPTH):
        put(f"conv_a_w{l}", np.asarray(inp["conv_a_w"][l]).reshape(31, 4, 128).transpose(2, 1, 0))
        put(f"conv_a_b{l}", chp(inp["conv_a_b"][l]))
        put(f"ln_a_g{l}", chp(inp["ln_a_g"][l]))
        put(f"ln_a_b{l}", chp(inp["ln_a_b"][l]))
        put(f"diff_norm_g{l}", np.asarray(inp["diff_norm_g"][l]).reshape(128, 1))
        put(f"conv_c_w{l}", np.asarray(inp["conv_c_w"][l]).reshape(3, 4, 128).transpose(2, 1, 0))
        put(f"norm_c_g{l}", chp(inp["norm_c_g"][l]))
        put(f"lru_conv_w{l}", np.asarray(inp["lru_conv_w"][l]).reshape(2, 4, 4, 128).transpose(3, 0, 2, 1))
        for nm in ("lru_conv_b", "lru_ba", "lru_bx", "lru_lam"):
            put(f"{nm}{l}", np.asarray(inp[nm][l]).reshape(2, 4, 128).transpose(2, 0, 1))
        put(f"norm_d_g{l}", chp(inp["norm_d_g"][l]))
        b1 = np.asarray(inp["exp_b1"][l])
        put(f"b1g{l}", b1[:, 0::2].reshape(NE, 8, 128).transpose(2, 0, 1))
        put(f"b1u{l}", b1[:, 1::2].reshape(NE, 8, 128).transpose(2, 0, 1))
    return pp


def _rope_tables():
    GRID_W = 64
    rows = SEQ // GRID_W
    row = np.repeat(np.arange(rows, dtype=np.float32), GRID_W)
    col = np.tile(np.arange(GRID_W, dtype=np.float32), rows)
    half = 32
    inv = (np.float32(10000.0) ** (-np.arange(0, half, 2, dtype=np.float32) / np.float32(half))).astype(np.float32)
    ar = row[:, None] * inv
    ac = col[:, None] * inv
    ang = np.concatenate([ar, ar, ac, ac], axis=-1)
    cos = np.cos(ang).astype(np.float32)
    sin = np.sin(ang).astype(np.float32)
    sgn = np.concatenate([-np.ones(16), np.ones(16), -np.ones(16), np.ones(16)]).astype(np.float32)
    sin = sin * sgn[None, :]
    tab = np.zeros((2, 128, T), np.float32)
    tab[0, :, :CTX] = 1.0
    tab[0, 0:64, CTX:] = cos.T
    tab[0, 64:128, CTX:] = cos.T
    tab[1, 0:64, CTX:] = sin.T
    tab[1, 64:128, CTX:] = sin.T
    return tab


class Prog:
    pass


def tok_chunks(t0=0, t1=T, step=512):
    out = []
    t = t0
    while t < t1:
        n = min(step, t1 - t)
        out.append((t, n))
        t += n
    return out


def declare_io(k, G):
    nc = k.nc

    def inp(name, shape, dt=F32):
        return k.dram(name, shape, dt, kind="ExternalInput")

    G.x = inp("x", [SEQ, D])
    G.ctx = inp("ctx", [CTX, D])
    G.pp = inp("pp", [128, NPP])
    G.rope = inp("rope", [2, 128, T])
    G.ada_w = inp("ada_w", [DEPTH, D, 6 * D])
    G.ada_b = inp("ada_b", [DEPTH, 6 * D])
    G.norm1_g = inp("norm1_g", [DEPTH, D])
    G.norm2_g = inp("norm2_g", [DEPTH, D])
    G.final_g = inp("final_g", [1, D])
    G.w_in = inp("w_in", [DEPTH, D, N_IN])
    G.w_out = inp("w_out", [DEPTH, D, D])
    G.diff_lambda = inp("diff_lambda", [DEPTH, 256])
    G.lru_wa = inp("lru_wa", [DEPTH, 2, 8, 64, 64])
    G.lru_wx = inp("lru_wx", [DEPTH, 2, 8, 64, 64])
    G.router_w = inp("router_w", [DEPTH, D, NE])
    G.router_b = inp("router_b", [DEPTH, NE])
    if G.with_moe:
        G.exp_w1 = inp("exp_w1", [DEPTH, NE, D, 2 * DFF])
        G.exp_w2 = inp("exp_w2", [DEPTH, NE, DFF, D])
        G.exp_b2 = inp("exp_b2", [DEPTH, NE, D])
    G.out = k.dram("out", [SEQ, D], F32, kind="ExternalOutput")
    G.xres = k.dram("xres", [T, D], F32)
    G.modv = [k.dram(f"modv{l}", [2, 6, D], F32) for l in range(DEPTH)]
    G.UF = k.dram("UF", [3584, T], F32)
    G.QK = k.dram("QK", [1024, T], BF16)
    G.V = k.dram("Vtm", [T, 512], BF16)
    G.YC = k.dram("YC", [D, T], BF16)
    G.H2T = k.dram("H2T", [NT, 128, KC, 128], BF16)
    G.COMB = k.dram("COMB", [T, NE], F32)
    G.WOB = [k.dram(f"wob{l}", [128, KC * D], BF16) for l in range(DEPTH)]
    G.RWB = [k.dram(f"rwb{l}", [128, KC * NE], BF16) for l in range(DEPTH)]
    if G.with_moe:
        G.W1B = [[k.dram(f"w1b{l}_{e}", [4, 128, KC * 512], BF16) for e in range(NE)] for l in range(DEPTH)]
        G.W2B = [[k.dram(f"w2b{l}_{e}", [2, 128, 4 * D], BF16) for e in range(NE)] for l in range(DEPTH)]


def phase_consts(k, G):
    G.ppt = k.sb([128, NPP], F32, "pp")
    k.dma("sp", G.ppt.ap, G.pp.ap, r=[G.pp], w=[G.ppt])
    G.ident_f = k.sb([128, 128], F32, "identf")
    G.ident_b = k.sb([128, 128], BF16, "identb")
    G.ones_f = k.sb([128, 128], F32, "onesf")
    k.op("pool", lambda e: e.memset(G.ident_f.ap, 0.0), w=[G.ident_f])
    k.op("pool", lambda e: e.memset(G.ones_f.ap, 1.0), w=[G.ones_f])
    k.op("pool", lambda e: e.affine_select(out=G.ident_f.ap, in_=G.ones_f.ap, pattern=[[-1, 128]],
                                           compare_op=ALU.is_equal, fill=0.0, base=0, channel_multiplier=1),
         r=[G.ones_f], w=[G.ident_f])
    k.copy("dve", G.ident_b.ap, G.ident_f.ap, r=[G.ident_f], w=[G.ident_b])
    G.eps = k.sb([128, 1], F32, "eps")
    k.op("pool", lambda e: e.memset(G.eps.ap, EPS), w=[G.eps])
    k.dma("sp", G.xres.ap[0:CTX, :], G.ctx.ap, r=[G.ctx], w=[G.xres])
    k.dma("sp", G.xres.ap[CTX:T, :], G.x.ap, r=[G.x], w=[G.xres])


def ppcol(G, name, i=0, n=1):
    o, _ = PP_COLS[name]
    return G.ppt.ap[:, o + i:o + i + n]


def phase_mod(k, G, l):
    m = k.mark()
    s = k.sb([128, 16, 2], F32, "silu_c")
    o, _ = PP_COLS["cvec"]
    k.act(s.ap[:, :, 0], G.ppt.ap[:, o:o + 16], AF.Silu, r=[G.ppt], w=[s])
    k.act(s.ap[:, :, 1], G.ppt.ap[:, o + 16:o + 32], AF.Silu, r=[G.ppt], w=[s])
    mod = k.sb([2, 6 * D], F32, "mod")
    adab = k.sb([2, 6 * D], F32, "adab")
    k.dma("sp", adab.ap, G.ada_b.ap[l:l + 1, :].to_broadcast([2, 6 * D]), r=[G.ada_b], w=[adab])
    wbufs = [k.sb([128, KC, 512], F32, f"adaw{i}") for i in range(2)]
    pss = [k.ps([2, 512], F32, f"modps{i}") for i in range(2)]
    awv = G.ada_w.ap[l].rearrange("(kc p) n -> p kc n", p=128)
    for cb in range(24):
        wb = wbufs[cb % 2]
        ps = pss[cb % 2]
        k.dma("sp" if cb % 2 == 0 else "act", wb.ap, awv[:, :, cb * 512:(cb + 1) * 512], r=[G.ada_w], w=[wb])
        for kc in range(KC):
            k.mm(ps.ap, s.ap[:, kc, :], wb.ap[:, kc, :], start=(kc == 0), stop=(kc == KC - 1),
                 r=[s, wb], w=[ps])
        k.tt("dve", mod.ap[:, cb * 512:(cb + 1) * 512], ps.ap, adab.ap[:, cb * 512:(cb + 1) * 512], ALU.add,
             r=[ps, adab], w=[mod])
    n1 = k.sb([2, D], F32, "n1")
    n2 = k.sb([2, D], F32, "n2")
    k.dma("sp", n1.ap, G.norm1_g.ap[l:l + 1, :].to_broadcast([2, D]), r=[G.norm1_g], w=[n1])
    k.dma("sp", n2.ap, G.norm2_g.ap[l:l + 1, :].to_broadcast([2, D]), r=[G.norm2_g], w=[n2])
    gs1 = k.sb([2, D], F32, "gs1")
    gs2 = k.sb([2, D], F32, "gs2")
    k.stt(gs1.ap, mod.ap[:, D:2 * D], 1.0, n1.ap, ALU.add, ALU.mult, r=[mod, n1], w=[gs1])
    k.stt(gs2.ap, mod.ap[:, 4 * D:5 * D], 1.0, n2.ap, ALU.add, ALU.mult, r=[mod, n2], w=[gs2])
    mv = G.modv[l]
    k.dma("sp", mv.ap[:, 0, :], gs1.ap, r=[gs1], w=[mv])
    k.dma("sp", mv.ap[:, 1, :], mod.ap[:, 0:D], r=[mod], w=[mv])
    k.dma("sp", mv.ap[:, 2, :], mod.ap[:, 2 * D:3 * D], r=[mod], w=[mv])
    k.dma("sp", mv.ap[:, 3, :], gs2.ap, r=[gs2], w=[mv])
    k.dma("sp", mv.ap[:, 4, :], mod.ap[:, 3 * D:4 * D], r=[mod], w=[mv])
    k.dma("sp", mv.ap[:, 5, :], mod.ap[:, 5 * D:6 * D], r=[mod], w=[mv])
    k.release(m)


def load_bcast(k, G, l, sec, which, name):
    t = k.sb([128, D], F32, name)
    mv = G.modv[l]
    k.dma("sp", t.ap, mv.ap[which:which + 1, sec, :].to_broadcast([128, D]), r=[mv], w=[t])
    return t


def norm_mod_tile(k, G, xt, gs, sh, hb, scr, ssq, rstd, tmp):
    k.act(scr.ap, xt.ap, AF.Square, r=[xt], w=[scr, ssq], accum_out=ssq.ap)
    k.act(rstd.ap, ssq.ap, AF.Sqrt, r=[ssq, G.eps], w=[rstd], scale=1.0 / D, bias=G.eps.ap)
    k.op("dve", lambda e: e.reciprocal(out=rstd.ap, in_=rstd.ap), r=[rstd], w=[rstd])
    k.stt(tmp.ap, xt.ap, rstd.ap, gs.ap, ALU.mult, ALU.mult, r=[xt, rstd, gs], w=[tmp])
    k.tt("pool", hb.ap, tmp.ap, sh.ap, ALU.add, r=[tmp, sh], w=[hb])


def phase_inproj(k, G, l):
    m = k.mark()
    hT_t = k.nc.alloc_sbuf_tensor(f"hT{l}", [128, KC, T], BF16).ap()
    hT = [Buf(hT_t, f"hT{i}") for i in range(NT)]
    m1 = k.mark()
    gs = [load_bcast(k, G, l, 0, w, "gs1") for w in range(2)]
    sh = [load_bcast(k, G, l, 1, w, "sh1") for w in range(2)]
    xts = [k.sb([128, D], F32, "xt") for _ in range(2)]
    tmp = k.sb([128, D], F32, "tmp")
    scr = k.sb([128, D], BF16, "scr")
    hbs = [k.sb([128, D], BF16, "hb") for _ in range(2)]
    ssq = k.sb([128, 1], F32, "ssq")
    rstd = k.sb([128, 1], F32, "rstd")
    pts = [k.ps([128, 1024], BF16, "pt") for _ in range(2)]
    for i in range(NT):
        which = 1 if i < 2 else 0
        xt = xts[i % 2]
        hb = hbs[i % 2]
        k.dma("sp", xt.ap, G.xres.ap[i * 128:(i + 1) * 128, :], r=[G.xres], w=[xt])
        norm_mod_tile(k, G, xt, gs[which], sh[which], hb, scr, ssq, rstd, tmp)
        for half in range(2):
            pt = pts[half]
            for j in range(8):
                kc = half * 8 + j
                k.op("pe", lambda e: e.transpose(out=pt.ap[:, j * 128:(j + 1) * 128],
                                                 in_=hb.ap[:, kc * 128:(kc + 1) * 128], identity=G.ident_b.ap),
                     r=[hb, G.ident_b], w=[pt])
            k.copy("act" if half == 0 else "dve",
                   hT_t[:, half * 8:(half + 1) * 8, i * 128:(i + 1) * 128],
                   pt.ap.rearrange("p (a b) -> p a b", a=8), r=[pt], w=[hT[i]])
    k.release(m1)
    cos2 = k.sb([128, T], F32, "cos2")
    sin2 = k.sb([128, T], F32, "sin2")
    k.dma("sp", cos2.ap, G.rope.ap[0], r=[G.rope], w=[cos2])
    k.dma("sp", sin2.ap, G.rope.ap[1], r=[G.rope], w=[sin2])
    wbufs = [k.sb([128, KC, 512], BF16, "wblk") for _ in range(2)]
    wperm = k.sb([128, KC, 512], BF16, "wperm")
    pss = [k.ps([128, 512], F32, "ps") for _ in range(6)]
    stage_f = [k.sb([128, T], F32, "stf") for _ in range(2)]
    stage_b = [k.sb([128, T], BF16, "stb") for _ in range(2)]
    t1 = k.sb([128, 512], F32, "ropet1")
    t2 = k.sb([128, 512], F32, "ropet2")
    vst = [k.sb([128, 512], BF16, "vst") for _ in range(2)]
    wv = G.w_in.ap[l].rearrange("(kc p) n -> p kc n", p=128)
    chunks = tok_chunks()
    psi = [0]
    evi = [0]

    def nextps():
        p = pss[psi[0] % len(pss)]
        psi[0] += 1
        return p

    def proj_fm(wb, mblk, n0, nn):
        ps = nextps()
        tiles = [hT[i] for i in range(n0 // 128, (n0 + nn) // 128)]
        for kc in range(KC):
            k.mm(ps.ap[:, :nn], wb.ap[:, kc, mblk * 128:(mblk + 1) * 128], hT_t[:, kc, n0:n0 + nn],
                 start=(kc == 0), stop=(kc == KC - 1), r=[wb] + tiles, w=[ps])
        return ps

    uf_row = 0
    nst = 0
    for cb in range(10):
        wb = wbufs[cb % 2]
        k.dma("pool", wb.ap, wv[:, :, cb * 512:(cb + 1) * 512], r=[G.w_in], w=[wb])
        if cb in (2, 3):
            src = wb.ap.rearrange("p kc (g s j) -> p (kc g) s j", s=2, j=16)
            dst = wperm.ap.rearrange("p kc (g s j) -> p (kc g) s j", s=2, j=16)
            k.copy("dve", dst[:, :, 0, :], src[:, :, 1, :], r=[wb], w=[wperm])
            k.copy("dve", dst[:, :, 1, :], src[:, :, 0, :], r=[wb], w=[wperm])
            for mblk in range(4):
                st = stage_b[nst % 2]
                nst += 1
                for (n0, nn) in chunks:
                    pa = proj_fm(wb, mblk, n0, nn)
                    pb = proj_fm(wperm, mblk, n0, nn)
                    k.tt("dve", t1.ap[:, :nn], pa.ap[:, :nn], cos2.ap[:, n0:n0 + nn], ALU.mult,
                         r=[pa, cos2], w=[t1])
                    k.tt("dve", t2.ap[:, :nn], pb.ap[:, :nn], sin2.ap[:, n0:n0 + nn], ALU.mult,
                         r=[pb, sin2], w=[t2])
                    k.tt("pool", st.ap[:, n0:n0 + nn], t1.ap[:, :nn], t2.ap[:, :nn], ALU.add,
                         r=[t1, t2], w=[st])
                row = (cb - 2) * 512 + mblk * 128
                k.dma("sp", G.QK.ap[row:row + 128, :], st.ap, r=[st], w=[G.QK])
        elif cb == 4:
            for i in range(NT):
                ps = nextps()
                for kc in range(KC):
                    k.mm(ps.ap, hT_t[:, kc, i * 128:(i + 1) * 128], wb.ap[:, kc, :],
                         start=(kc == 0), stop=(kc == KC - 1), r=[wb, hT[i]], w=[ps])
                vs = vst[i % 2]
                k.copy("act" if i % 2 == 0 else "dve", vs.ap, ps.ap, r=[ps], w=[vs])
                k.dma("sp", G.V.ap[i * 128:(i + 1) * 128, :], vs.ap, r=[vs], w=[G.V])
        else:
            for mblk in range(4):
                st = stage_f[nst % 2]
                nst += 1
                for (n0, nn) in chunks:
                    ps = proj_fm(wb, mblk, n0, nn)
                    k.copy("act" if evi[0] % 2 == 0 else "dve", st.ap[:, n0:n0 + nn], ps.ap[:, :nn],
                           r=[ps], w=[st])
                    evi[0] += 1
                k.dma("sp", G.UF.ap[uf_row:uf_row + 128, :], st.ap, r=[st], w=[G.UF])
                uf_row += 128
    assert uf_row == 3584
    k.release(m)


def build_program(upto="all", dbg=(), with_moe=True):
    nc = bass.Bass("TRN2", target_bir_lowering=False)
    k = KB(nc)
    G = Prog()
    G.with_moe = with_moe
    declare_io(k, G)
    phase_consts(k, G)
    done = False
    for l in range(DEPTH):
        phase_mod(k, G, l)
    if upto == "mod":
        done = True
    for l in range(DEPTH):
        if done:
            break
        phase_inproj(k, G, l)
        if upto == f"inproj{l}":
            break
        precast_outproj(k, G, l)
        if l == 0 and G.with_moe:
            precast_experts(k, G, 0)
        phase_mixers(k, G, l, need_ctx=(l < DEPTH - 1))
        if upto == f"mix{l}":
            break
        phase_outproj(k, G, l, need_ctx=(l < DEPTH - 1))
        if upto == f"outproj{l}":
            break
        if l + 1 < DEPTH and G.with_moe:
            precast_experts(k, G, l + 1)
        phase_moe(k, G, l, need_ctx=(l < DEPTH - 1), last=(l == DEPTH - 1))
        if upto == f"moe{l}":
            break
    for name in dbg:
        src = getattr(G, name) if not name.startswith("modv") else G.modv[int(name[4:])]
        o = k.dram("dbg_" + name, list(src.ap.shape), src.ap.dtype, kind="ExternalOutput")
        k.dma("sp", o.ap, src.ap, r=[src], w=[o])
    k.barrier(engines=("sp",))
    G.k = k
    return nc, G


def make_in_maps(inp, cores):
    rope = _rope_tables()
    shared = {
        "rope": rope,
        "ada_w": np.ascontiguousarray(inp["ada_w"], dtype=np.float32),
        "ada_b": np.ascontiguousarray(inp["ada_b"], dtype=np.float32),
        "norm1_g": np.ascontiguousarray(inp["norm1_g"], dtype=np.float32),
        "norm2_g": np.ascontiguousarray(inp["norm2_g"], dtype=np.float32),
        "final_g": np.ascontiguousarray(inp["final_g"], dtype=np.float32).reshape(1, D),
        "w_in": np.ascontiguousarray(inp["w_in"], dtype=np.float32),
        "w_out": np.ascontiguousarray(inp["w_out"], dtype=np.float32),
        "diff_lambda": np.ascontiguousarray(inp["diff_lambda"], dtype=np.float32).reshape(DEPTH, 256),
        "lru_wa": np.ascontiguousarray(inp["lru_wa"], dtype=np.float32),
        "lru_wx": np.ascontiguousarray(inp["lru_wx"], dtype=np.float32),
        "router_w": np.ascontiguousarray(inp["router_w"], dtype=np.float32),
        "router_b": np.ascontiguousarray(inp["router_b"], dtype=np.float32),
        "exp_w1": np.ascontiguousarray(inp["exp_w1"], dtype=np.float32),
        "exp_w2": np.ascontiguousarray(inp["exp_w2"], dtype=np.float32),
        "exp_b2": np.ascontiguousarray(inp["exp_b2"], dtype=np.float32),
    }
    maps = []
    for b in cores:
        mp = dict(shared)
        mp["x"] = np.ascontiguousarray(inp["x"][b], dtype=np.float32)
        mp["ctx"] = np.ascontiguousarray(inp["ctx"][b], dtype=np.float32)
        mp["pp"] = _pack_pp(inp, b)
        maps.append(mp)
    return maps


def useg(gap, step=512):
    out = [(0, CTX, 0)]
    for (t0, n) in tok_chunks(CTX, T, step):
        out.append((t0, n, t0 + gap))
    return out


def load_gapped(k, G, dst, dst_off_ctx, dst_off_lat, src_buf, row0, q="sp"):
    k.dma(q, dst.ap[:, dst_off_ctx:dst_off_ctx + CTX], src_buf.ap[row0:row0 + 128, 0:CTX], r=[src_buf], w=[dst])
    k.dma(q, dst.ap[:, dst_off_lat:dst_off_lat + SEQ], src_buf.ap[row0:row0 + 128, CTX:T], r=[src_buf], w=[dst])


def finish_norm(k, G, ys, gap, kind, l, yc_row0, gname, bname=None):
    m = k.mark()
    ps_sq = [k.ps([128, 512], F32, "pssq") for _ in range(2)]
    ps_su = [k.ps([128, 512], F32, "pssu") for _ in range(2)] if kind == "ln_silu" else None
    sq = [k.sb([128, 512], F32, "sq") for _ in range(2)]
    rstd = [k.sb([128, 512], F32, "rstd") for _ in range(2)]
    mean = [k.sb([128, 512], F32, "mean") for _ in range(2)]
    msq = k.sb([128, 512], F32, "msq")
    t1 = [k.sb([128, 512], F32, "t1") for _ in range(2)]
    stage = [k.sb([128, T], BF16, "ystage") for _ in range(4)]
    for ci, (t0, n, u0) in enumerate(useg(gap)):
        pq = ps_sq[ci % 2]
        rs = rstd[ci % 2]
        mn = mean[ci % 2]
        for c in range(4):
            s = sq[c % 2]
            k.act(s.ap[:, :n], ys[c].ap[:, u0:u0 + n], AF.Square, r=[ys[c]], w=[s])
            k.mm(pq.ap[:, :n], G.ones_f.ap, s.ap[:, :n], start=(c == 0), stop=(c == 3), r=[G.ones_f, s], w=[pq])
        if kind == "ln_silu":
            pu = ps_su[ci % 2]
            for c in range(4):
                k.mm(pu.ap[:, :n], G.ones_f.ap, ys[c].ap[:, u0:u0 + n], start=(c == 0), stop=(c == 3),
                     r=[G.ones_f, ys[c]], w=[pu])
            k.ts("dve", mn.ap[:, :n], pu.ap[:, :n], 1.0 / GW, ALU.mult, r=[pu], w=[mn])
            k.tt("dve", msq.ap[:, :n], mn.ap[:, :n], mn.ap[:, :n], ALU.mult, r=[mn], w=[msq])
            k.stt(rs.ap[:, :n], pq.ap[:, :n], 1.0 / GW, msq.ap[:, :n], ALU.mult, ALU.subtract, r=[pq, msq], w=[rs])
            k.act(rs.ap[:, :n], rs.ap[:, :n], AF.Sqrt, r=[rs, G.eps], w=[rs], bias=G.eps.ap)
        else:
            k.act(rs.ap[:, :n], pq.ap[:, :n], AF.Sqrt, r=[pq, G.eps], w=[rs], scale=1.0 / GW, bias=G.eps.ap)
        k.op("dve", lambda e: e.reciprocal(out=rs.ap[:, :n], in_=rs.ap[:, :n]), r=[rs], w=[rs])
        for c in range(4):
            gcol = ppcol(G, f"{gname}{l}", c)
            if kind == "ln_silu":
                bcol = ppcol(G, f"{bname}{l}", c)
                t = t1[c % 2]
                k.tt("dve", t.ap[:, :n], ys[c].ap[:, u0:u0 + n], mn.ap[:, :n], ALU.subtract, r=[ys[c], mn], w=[t])
                k.tt("pool", t.ap[:, :n], t.ap[:, :n], rs.ap[:, :n], ALU.mult, r=[t, rs], w=[t])
                k.act(stage[c].ap[:, t0:t0 + n], t.ap[:, :n], AF.Silu, r=[t, G.ppt], w=[stage[c]],
                      scale=gcol, bias=bcol)
            else:
                k.stt(stage[c].ap[:, t0:t0 + n], ys[c].ap[:, u0:u0 + n], gcol, rs.ap[:, :n], ALU.mult, ALU.mult,
                      r=[ys[c], rs, G.ppt], w=[stage[c]])
    for c in range(4):
        k.dma("sp", G.YC.ap[yc_row0 + c * 128:yc_row0 + (c + 1) * 128, :], stage[c].ap, r=[stage[c]], w=[G.YC])
    k.release(m)


def mixer_conformer(k, G, l):
    m = k.mark()
    GAP = 30
    NU = T + GAP
    ZW = NU + 30
    ys = [k.sb([128, NU], F32, "convA") for _ in range(4)]
    zps = [k.sb([128, ZW], F32, "zpA") for _ in range(2)]
    vals = [k.sb([128, T], F32, "valA") for _ in range(2)]
    gates = [k.sb([128, T], F32, "gateA") for _ in range(2)]
    for zp in zps:
        k.op("pool", lambda e: e.memset(zp.ap, 0.0), w=[zp])
    wo, _ = PP_COLS[f"conv_a_w{l}"]
    for c in range(4):
        zp = zps[c % 2]
        va = vals[c % 2]
        ga = gates[c % 2]
        k.dma("sp", va.ap, G.UF.ap[c * 128:(c + 1) * 128, :], r=[G.UF], w=[va])
        k.dma("act", ga.ap, G.UF.ap[512 + c * 128:512 + (c + 1) * 128, :], r=[G.UF], w=[ga])
        k.act(ga.ap, ga.ap, AF.Sigmoid, r=[ga], w=[ga])
        k.tt("pool", zp.ap[:, 15:15 + CTX], va.ap[:, 0:CTX], ga.ap[:, 0:CTX], ALU.mult, r=[va, ga], w=[zp])
        k.tt("pool", zp.ap[:, 45 + CTX:45 + CTX + SEQ], va.ap[:, CTX:T], ga.ap[:, CTX:T], ALU.mult,
             r=[va, ga], w=[zp])
        y = ys[c]
        wcol = lambda kk: G.ppt.ap[:, wo + c * 31 + kk:wo + c * 31 + kk + 1]
        k.ts("dve", y.ap, zp.ap[:, 0:NU], wcol(0), ALU.mult, ppcol(G, f"conv_a_b{l}", c), ALU.add,
             r=[zp, G.ppt], w=[y])
        for kk in range(1, 31):
            k.stt(y.ap, zp.ap[:, kk:kk + NU], wcol(kk), y.ap, ALU.mult, ALU.add, r=[zp, y, G.ppt], w=[y])
    finish_norm(k, G, ys, GAP, "ln_silu", l, 0, "ln_a_g", "ln_a_b")
    k.release(m)


def mixer_sconv(k, G, l):
    m = k.mark()
    GAP = 2
    NU = T + GAP
    ZW = NU + 2
    ys = [k.sb([128, NU], F32, "convC") for _ in range(4)]
    zps = [k.sb([128, ZW], F32, "zpC") for _ in range(2)]
    bgs = [k.sb([128, NU], F32, "bgC") for _ in range(2)]
    cgs = [k.sb([128, T], F32, "cgC") for _ in range(2)]
    vs = [k.sb([128, T], F32, "vC") for _ in range(2)]
    for zp in zps:
        k.op("pool", lambda e: e.memset(zp.ap, 0.0), w=[zp])
    for bg in bgs:
        k.op("pool", lambda e: e.memset(bg.ap, 0.0), w=[bg])
    wo, _ = PP_COLS[f"conv_c_w{l}"]
    for c in range(4):
        zp, bg, cg, v = zps[c % 2], bgs[c % 2], cgs[c % 2], vs[c % 2]
        load_gapped(k, G, bg, 0, CTX + GAP, G.UF, 1024 + c * 128, q="sp")
        k.dma("act", cg.ap, G.UF.ap[1536 + c * 128:1536 + (c + 1) * 128, :], r=[G.UF], w=[cg])
        k.dma("sp", v.ap, G.UF.ap[2048 + c * 128:2048 + (c + 1) * 128, :], r=[G.UF], w=[v])
        k.tt("pool", zp.ap[:, 1:1 + CTX], cg.ap[:, 0:CTX], v.ap[:, 0:CTX], ALU.mult, r=[cg, v], w=[zp])
        k.tt("pool", zp.ap[:, 3 + CTX:3 + CTX + SEQ], cg.ap[:, CTX:T], v.ap[:, CTX:T], ALU.mult, r=[cg, v], w=[zp])
        y = ys[c]
        wcol = lambda kk: G.ppt.ap[:, wo + c * 3 + kk:wo + c * 3 + kk + 1]
        k.ts("dve", y.ap, zp.ap[:, 0:NU], wcol(0), ALU.mult, r=[zp, G.ppt], w=[y])
        for kk in range(1, 3):
            k.stt(y.ap, zp.ap[:, kk:kk + NU], wcol(kk), y.ap, ALU.mult, ALU.add, r=[zp, y, G.ppt], w=[y])
        k.tt("dve", y.ap, y.ap, bg.ap, ALU.mult, r=[y, bg], w=[y])
    finish_norm(k, G, ys, GAP, "rms", l, 1024, "norm_c_g")
    k.release(m)


def mixer_lru(k, G, l):
    m = k.mark()
    GAP = 6
    NU = T + GAP
    XW = NU + 6
    ys = [k.sb([128, NU], F32, "yD") for _ in range(4)]
    xp = k.sb([128, XW], F32, "xpD")
    xcv = k.sb([128, NU], F32, "xcvD")
    ra = k.sb([128, NU], F32, "raD")
    gx = k.sb([128, NU], F32, "gxD")
    sq = k.sb([128, NU], F32, "sqD")
    hd = [k.sb([128, NU], F32, "hD") for _ in range(2)]
    gt = k.sb([128, NU], F32, "gtD")
    gt2 = k.sb([128, NU], F32, "gt2D")
    bd_a = k.sb([128, 128], F32, "bdA")
    bd_x = k.sb([128, 128], F32, "bdX")
    negsp = k.sb([128, 8], F32, "negsp")
    one_c = k.sb([128, 1], F32, "onec")
    pss = [k.ps([128, 512], F32, "psD") for _ in range(4)]
    k.op("pool", lambda e: e.memset(one_c.ap, 1.0), w=[one_c])
    k.op("pool", lambda e: e.memset(xp.ap, 0.0), w=[xp])
    k.op("pool", lambda e: e.memset(gt.ap, 0.0), w=[gt])
    for hh in hd:
        k.op("pool", lambda e: e.memset(hh.ap, 0.0), w=[hh])
    k.op("pool", lambda e: e.memset(bd_a.ap, 0.0), w=[bd_a])
    k.op("pool", lambda e: e.memset(bd_x.ap, 0.0), w=[bd_x])
    lo, _ = PP_COLS[f"lru_lam{l}"]
    k.act(negsp.ap, G.ppt.ap[:, lo:lo + 8], AF.Exp, r=[G.ppt], w=[negsp], scale=-1.0)
    k.act(negsp.ap, negsp.ap, AF.Ln, r=[negsp, one_c], w=[negsp], bias=one_c.ap)
    k.ts("dve", negsp.ap, negsp.ap, -8.0, ALU.mult, r=[negsp], w=[negsp])
    wo, _ = PP_COLS[f"lru_conv_w{l}"]
    uchunks = tok_chunks(0, NU, 512)
    pi = 0
    for c in range(4):
        load_gapped(k, G, xp, 3, 9 + CTX, G.UF, 3072 + c * 128, q="sp")
        load_gapped(k, G, gt, 0, CTX + GAP, G.UF, 2560 + c * 128, q="act")
        for d in range(2):
            col = d * 4 + c
            sh = 0 if d == 0 else 3
            wcol = lambda kk: G.ppt.ap[:, wo + col * 4 + kk:wo + col * 4 + kk + 1]
            k.ts("dve", xcv.ap, xp.ap[:, sh:sh + NU], wcol(0), ALU.mult, ppcol(G, f"lru_conv_b{l}", col), ALU.add,
                 r=[xp, G.ppt], w=[xcv])
            for kk in range(1, 4):
                k.stt(xcv.ap, xp.ap[:, sh + kk:sh + kk + NU], wcol(kk), xcv.ap, ALU.mult, ALU.add,
                      r=[xp, xcv, G.ppt], w=[xcv])
            for (bd, wsrc) in ((bd_a, G.lru_wa), (bd_x, G.lru_wx)):
                k.dma("sp", bd.ap[0:64, 0:64], wsrc.ap[l, d, 2 * c], r=[wsrc], w=[bd])
                k.dma("sp", bd.ap[64:128, 64:128], wsrc.ap[l, d, 2 * c + 1], r=[wsrc], w=[bd])
            for (u0, n) in uchunks:
                pa = pss[pi % 4]
                px = pss[(pi + 1) % 4]
                pi += 2
                k.mm(pa.ap[:, :n], bd_a.ap, xcv.ap[:, u0:u0 + n], start=True, stop=True, r=[bd_a, xcv], w=[pa])
                k.mm(px.ap[:, :n], bd_x.ap, xcv.ap[:, u0:u0 + n], start=True, stop=True, r=[bd_x, xcv], w=[px])
                k.act(ra.ap[:, u0:u0 + n], pa.ap[:, :n], AF.Sigmoid, r=[pa, G.ppt], w=[ra],
                      bias=ppcol(G, f"lru_ba{l}", col))
                k.act(gx.ap[:, u0:u0 + n], px.ap[:, :n], AF.Sigmoid, r=[px, G.ppt], w=[gx],
                      bias=ppcol(G, f"lru_bx{l}", col))
            k.act(ra.ap, ra.ap, AF.Exp, r=[ra, negsp], w=[ra], scale=negsp.ap[:, col:col + 1])
            k.tt("pool", gx.ap, gx.ap, xcv.ap, ALU.mult, r=[gx, xcv], w=[gx])
            k.tt("dve", sq.ap, ra.ap, ra.ap, ALU.mult, r=[ra], w=[sq])
            k.act(sq.ap, sq.ap, AF.Sqrt, r=[sq, one_c], w=[sq], scale=-1.0, bias=one_c.ap)
            k.tt("dve", gx.ap, gx.ap, sq.ap, ALU.mult, r=[gx, sq], w=[gx])
            h = hd[d]
            if d == 0:
                k.op("dve", lambda e: e.tensor_tensor_scan(out=h.ap[:, 0:CTX], data0=ra.ap[:, 0:CTX],
                                                           data1=gx.ap[:, 0:CTX], initial=0.0,
                                                           op0=ALU.mult, op1=ALU.add), r=[ra, gx], w=[h])
                k.op("dve", lambda e: e.tensor_tensor_scan(out=h.ap[:, CTX + GAP:NU], data0=ra.ap[:, CTX + GAP:NU],
                                                           data1=gx.ap[:, CTX + GAP:NU],
                                                           initial=h.ap[:, CTX - 1:CTX],
                                                           op0=ALU.mult, op1=ALU.add), r=[ra, gx, h], w=[h])
            else:
                k.op("dve", lambda e: e.tensor_tensor_scan(out=h.ap[:, CTX - 1::-1], data0=ra.ap[:, CTX - 1::-1],
                                                           data1=gx.ap[:, CTX - 1::-1], initial=0.0,
                                                           op0=ALU.mult, op1=ALU.add), r=[ra, gx], w=[h])
                lo_ = CTX + GAP - 1
                k.op("dve", lambda e: e.tensor_tensor_scan(out=h.ap[:, NU - 1:lo_:-1], data0=ra.ap[:, NU - 1:lo_:-1],
                                                           data1=gx.ap[:, NU - 1:lo_:-1],
                                                           initial=h.ap[:, 0:1],
                                                           op0=ALU.mult, op1=ALU.add), r=[ra, gx, h], w=[h])
        y = ys[c]
        k.act(gt2.ap, gt.ap, AF.Square, r=[gt], w=[gt2])
        k.ts("dve", gt2.ap, gt2.ap, 0.044715, ALU.mult, 1.0, ALU.add, r=[gt2], w=[gt2])
        k.tt("dve", gt2.ap, gt2.ap, gt.ap, ALU.mult, r=[gt2, gt], w=[gt2])
        k.act(gt2.ap, gt2.ap, AF.Sigmoid, r=[gt2], w=[gt2], scale=1.5957691216057308)
        k.tt("dve", gt2.ap, gt2.ap, gt.ap, ALU.mult, r=[gt2, gt], w=[gt2])
        k.tt("pool", y.ap[:, 0:CTX], hd[0].ap[:, 0:CTX], hd[1].ap[:, 0:CTX], ALU.add, r=[hd[0], hd[1]], w=[y])
        k.tt("pool", y.ap[:, CTX:NU], hd[0].ap[:, CTX:NU], hd[1].ap[:, CTX:NU], ALU.add, r=[hd[0], hd[1]], w=[y])
        k.tt("dve", y.ap, y.ap, gt2.ap, ALU.mult, r=[y, gt2], w=[y])
    finish_norm(k, G, ys, GAP, "rms", l, 1536, "norm_d_g")
    k.release(m)


def mixer_attn(k, G, l, need_ctx):
    m = k.mark()
    lam_init = 0.8 - 0.6 * math.exp(-0.3 * l)
    dl = k.sb([128, 256], F32, "dlam")
    k.dma("sp", dl.ap, G.diff_lambda.ap[l:l + 1, :].to_broadcast([128, 256]), r=[G.diff_lambda], w=[dl])
    pr = k.sb([128, 128], F32, "dlpr")
    s2 = k.sb([128, 2], F32, "dls")
    dlv = dl.ap.rearrange("p (a b) -> p a b", a=4)
    k.tt("dve", pr.ap[:, 0:64], dlv[:, 0, :], dlv[:, 1, :], ALU.mult, r=[dl], w=[pr])
    k.tt("dve", pr.ap[:, 64:128], dlv[:, 2, :], dlv[:, 3, :], ALU.mult, r=[dl], w=[pr])
    k.op("dve", lambda e: e.reduce_sum(out=s2.ap, in_=pr.ap.rearrange("p (a b) -> p a b", a=2), axis=AX.X),
         r=[pr], w=[s2])
    k.act(s2.ap, s2.ap, AF.Exp, r=[s2], w=[s2])
    neg_lam = k.sb([128, 1], F32, "neglam")
    k.tt("dve", neg_lam.ap, s2.ap[:, 1:2], s2.ap[:, 0:1], ALU.subtract, r=[s2], w=[neg_lam])
    k.ts("dve", neg_lam.ap, neg_lam.ap, -lam_init, ALU.add, r=[neg_lam], w=[neg_lam])
    gsc = k.sb([128, 1], F32, "gsc")
    k.ts("dve", gsc.ap, ppcol(G, f"diff_norm_g{l}"), 1.0 - lam_init, ALU.mult, r=[G.ppt], w=[gsc])
    ones_b = k.sb([128, 128], BF16, "onesb")
    k.copy("dve", ones_b.ap, G.ones_f.ap, r=[G.ones_f], w=[ones_b])
    vt = k.sb([128, NT, 512], BF16, "vtm")
    k.dma("sp", vt.ap, G.V.ap.rearrange("(t p) e -> p t e", p=128), r=[G.V], w=[vt])
    qT = [k.sb([64, T], BF16, "qT") for _ in range(2)]
    kT = [k.sb([64, T], BF16, "kT") for _ in range(2)]
    pts = [k.sb([128, 512], BF16, "pexp") for _ in range(3)]
    ps_s = [k.ps([128, 512], F32, "ps_s") for _ in range(2)]
    ps_acc = [k.ps([128, 512], F32, "ps_acc") for _ in range(2)]
    ps_den = [k.ps([128, 512], F32, "ps_den") for _ in range(2)]
    ps_n = k.ps([128, 512], F32, "ps_n")
    rden = [k.sb([128, 512], F32, "rden") for _ in range(2)]
    tnum = [k.sb([128, 512], F32, "tnum") for _ in range(2)]
    osb = k.sb([128, 512], F32, "osb")
    osq = k.sb([128, 512], F32, "osq")
    rs = k.sb([128, 512], F32, "rsb")
    stage = [k.sb([128, T], BF16, "ystB") for _ in range(2)]
    si = 0
    pi = 0
    for h in range(4):
        for mi in range(2):
            j = 2 * h + mi
            k.dma("sp", qT[mi].ap, G.QK.ap[j * 64:(j + 1) * 64, :], r=[G.QK], w=[qT[mi]])
            k.dma("act", kT[mi].ap, G.QK.ap[512 + j * 64:512 + (j + 1) * 64, :], r=[G.QK], w=[kT[mi]])
        st = stage[h % 2]
        qchunks = [(t0, n, NT) for (t0, n) in tok_chunks(CTX, T, 512)]
        if need_ctx:
            qchunks = [(0, CTX, 2)] + qchunks
        for (t0, n, nkt) in qchunks:
            for mi in range(2):
                for kt in range(nkt):
                    pss = ps_s[si % 2]
                    si += 1
                    k.mm(pss.ap[:, :n], kT[mi].ap[:, kt * 128:(kt + 1) * 128], qT[mi].ap[:, t0:t0 + n],
                         start=True, stop=True, r=[kT[mi], qT[mi]], w=[pss])
                    pt = pts[pi % 3]
                    pi += 1
                    k.act(pt.ap[:, :n], pss.ap[:, :n], AF.Exp, r=[pss], w=[pt], scale=0.125)
                    k.mm(ps_acc[mi].ap[:, :n], vt.ap[:, kt, h * 128:(h + 1) * 128], pt.ap[:, :n],
                         start=(kt == 0), stop=(kt == nkt - 1), r=[vt, pt], w=[ps_acc[mi]])
                    k.mm(ps_den[mi].ap[:, :n], ones_b.ap, pt.ap[:, :n],
                         start=(kt == 0), stop=(kt == nkt - 1), r=[ones_b, pt], w=[ps_den[mi]])
            for mi in range(2):
                k.op("dve", lambda e: e.reciprocal(out=rden[mi].ap[:, :n], in_=ps_den[mi].ap[:, :n]),
                     r=[ps_den[mi]], w=[rden[mi]])
                k.tt("dve", tnum[mi].ap[:, :n], ps_acc[mi].ap[:, :n], rden[mi].ap[:, :n], ALU.mult,
                     r=[ps_acc[mi], rden[mi]], w=[tnum[mi]])
            k.stt(osb.ap[:, :n], tnum[1].ap[:, :n], neg_lam.ap, tnum[0].ap[:, :n], ALU.mult, ALU.add,
                  r=[tnum[0], tnum[1], neg_lam], w=[osb])
            k.act(osq.ap[:, :n], osb.ap[:, :n], AF.Square, r=[osb], w=[osq])
            k.mm(ps_n.ap[:, :n], G.ones_f.ap, osq.ap[:, :n], start=True, stop=True, r=[G.ones_f, osq], w=[ps_n])
            k.act(rs.ap[:, :n], ps_n.ap[:, :n], AF.Sqrt, r=[ps_n, G.eps], w=[rs], scale=1.0 / 128, bias=G.eps.ap)
            k.op("dve", lambda e: e.reciprocal(out=rs.ap[:, :n], in_=rs.ap[:, :n]), r=[rs], w=[rs])
            k.stt(st.ap[:, t0:t0 + n], osb.ap[:, :n], gsc.ap, rs.ap[:, :n], ALU.mult, ALU.mult,
                  r=[osb, gsc, rs], w=[st])
        if need_ctx:
            k.dma("sp", G.YC.ap[512 + h * 128:512 + (h + 1) * 128, :], st.ap, r=[st], w=[G.YC])
        else:
            k.dma("sp", G.YC.ap[512 + h * 128:512 + (h + 1) * 128, CTX:T], st.ap[:, CTX:T], r=[st], w=[G.YC])
    k.release(m)


MIXSEL = "bacd"


def phase_mixers(k, G, l, need_ctx):
    if "b" in MIXSEL:
        mixer_attn(k, G, l, need_ctx)
    if "a" in MIXSEL:
        mixer_conformer(k, G, l)
    if "c" in MIXSEL:
        mixer_sconv(k, G, l)
    if "d" in MIXSEL:
        mixer_lru(k, G, l)


def phase_outproj(k, G, l, need_ctx):
    m = k.mark()
    wo = k.sb([128, KC, D], BF16, "wout")
    wsrc = G.WOB[l].ap.rearrange("p (kc n) -> p kc n", kc=KC)
    for j in range(4):
        k.dma("act", wo.ap[:, j * 4:(j + 1) * 4, :], wsrc[:, j * 4:(j + 1) * 4, :], r=[G.WOB[l]], w=[wo])
    rw = k.sb([128, KC, NE], BF16, "rw")
    k.dma("act", rw.ap, G.RWB[l].ap.rearrange("p (kc e) -> p kc e", kc=KC), r=[G.RWB[l]], w=[rw])
    rb = k.sb([128, NE], F32, "rb")
    k.dma("sp", rb.ap, G.router_b.ap[l:l + 1, :].to_broadcast([128, NE]), r=[G.router_b], w=[rb])
    g1 = k.sb([128, D], F32, "g1b")
    gs2 = k.sb([128, D], F32, "gs2b")
    sh2 = k.sb([128, D], F32, "sh2b")
    ycs = [k.sb([128, KC, 512], BF16, "ycblk") for _ in range(2)]
    xts = [k.sb([128, D], F32, "xt") for _ in range(2)]
    tmp = k.sb([128, D], F32, "tmp")
    scr = k.sb([128, D], BF16, "scr")
    hbs = [k.sb([128, D], BF16, "hb") for _ in range(2)]
    h2s = [k.sb([128, KC, 128], BF16, "h2s") for _ in range(2)]
    ssq = k.sb([128, 1], F32, "ssq")
    rstd = k.sb([128, 1], F32, "rstd")
    lg = k.sb([128, NE], F32, "lg")
    ex = k.sb([128, NE], F32, "ex")
    msk = k.sb([128, NE], F32, "msk")
    top8 = k.sb([128, 8], F32, "top8")
    nmx = k.sb([128, 1], F32, "nmx")
    ssum = k.sb([128, 1], F32, "ssum")
    cmb = [k.sb([128, NE], F32, "cmb") for _ in range(2)]
    ps_y = [k.ps([128, 512], F32, "psy") for _ in range(4)]
    pts = [k.ps([128, 1024], BF16, "pt") for _ in range(2)]
    ps_l = k.ps([128, NE], F32, "psl")
    mv = G.modv[l]
    tiles = list(range(NT)) if need_ctx else list(range(2, NT))
    cur_which = None
    cur_blk = None
    nblk = 0
    for i in tiles:
        which = 1 if i < 2 else 0
        if which != cur_which:
            for (t, sec) in ((g1, 2), (gs2, 3), (sh2, 4)):
                k.dma("sp", t.ap, mv.ap[which:which + 1, sec, :].to_broadcast([128, D]), r=[mv], w=[t])
            cur_which = which
        blk = i // 4
        if blk != cur_blk:
            yc = ycs[nblk % 2]
            nblk += 1
            nt_ = min(512, T - blk * 512)
            for kc in range(KC):
                k.dma("sp" if kc % 2 == 0 else "act", yc.ap[:, kc, :nt_],
                      G.YC.ap[kc * 128:(kc + 1) * 128, blk * 512:blk * 512 + nt_], r=[G.YC], w=[yc])
            cur_blk = blk
        xt = xts[i % 2]
        hb = hbs[i % 2]
        h2 = h2s[i % 2]
        cb_ = cmb[i % 2]
        k.dma("sp", xt.ap, G.xres.ap[i * 128:(i + 1) * 128, :], r=[G.xres], w=[xt])
        off = (i % 4) * 128
        for dblk in range(4):
            ps = ps_y[dblk]
            for kc in range(KC):
                k.mm(ps.ap, yc.ap[:, kc, off:off + 128], wo.ap[:, kc, dblk * 512:(dblk + 1) * 512],
                     start=(kc == 0), stop=(kc == KC - 1), r=[yc, wo], w=[ps])
            sl = slice(dblk * 512, (dblk + 1) * 512)
            k.tt("dve", tmp.ap[:, sl], ps.ap, g1.ap[:, sl], ALU.mult, r=[ps, g1], w=[tmp])
            k.tt("pool", xt.ap[:, sl], xt.ap[:, sl], tmp.ap[:, sl], ALU.add, r=[xt, tmp], w=[xt])
        k.dma("sp", G.xres.ap[i * 128:(i + 1) * 128, :], xt.ap, r=[xt], w=[G.xres])
        norm_mod_tile(k, G, xt, gs2, sh2, hb, scr, ssq, rstd, tmp)
        for half in range(2):
            pt = pts[half]
            for j in range(8):
                kc = half * 8 + j
                k.op("pe", lambda e: e.transpose(out=pt.ap[:, j * 128:(j + 1) * 128],
                                                 in_=hb.ap[:, kc * 128:(kc + 1) * 128], identity=G.ident_b.ap),
                     r=[hb, G.ident_b], w=[pt])
            k.copy("act" if half == 0 else "dve", h2.ap[:, half * 8:(half + 1) * 8, :],
                   pt.ap.rearrange("p (a b) -> p a b", a=8), r=[pt], w=[h2])
        k.dma("sp", G.H2T.ap[i], h2.ap, r=[h2], w=[G.H2T])
        for kc in range(KC):
            k.mm(ps_l.ap, h2.ap[:, kc, :], rw.ap[:, kc, :], start=(kc == 0), stop=(kc == KC - 1),
                 r=[h2, rw], w=[ps_l])
        k.tt("dve", lg.ap, ps_l.ap, rb.ap, ALU.add, r=[ps_l, rb], w=[lg])
        k.op("dve", lambda e: e.max(out=top8.ap, in_=lg.ap), r=[lg], w=[top8])
        k.ts("dve", msk.ap, lg.ap, top8.ap[:, 3:4], ALU.is_ge, r=[lg, top8], w=[msk])
        k.ts("dve", nmx.ap, top8.ap[:, 0:1], -1.0, ALU.mult, r=[top8], w=[nmx])
        k.act(ex.ap, lg.ap, AF.Exp, r=[lg, nmx], w=[ex], bias=nmx.ap)
        k.tt("dve", ex.ap, ex.ap, msk.ap, ALU.mult, r=[ex, msk], w=[ex])
        k.op("dve", lambda e: e.reduce_sum(out=ssum.ap, in_=ex.ap, axis=AX.X), r=[ex], w=[ssum])
        k.op("dve", lambda e: e.reciprocal(out=ssum.ap, in_=ssum.ap), r=[ssum], w=[ssum])
        k.ts("dve", cb_.ap, ex.ap, ssum.ap, ALU.mult, r=[ex, ssum], w=[cb_])
        k.dma("sp", G.COMB.ap[i * 128:(i + 1) * 128, :], cb_.ap, r=[cb_], w=[G.COMB])
    k.release(m)


class WStream:
    def __init__(self, k, bufs, srcs, q="pool", hold=1):
        self.k, self.bufs, self.srcs, self.q, self.hold = k, bufs, srcs, q, hold
        self.nxt = 0

    def get(self, n):
        nb = len(self.bufs)
        while self.nxt < len(self.srcs) and self.nxt <= n + nb - self.hold:
            j = self.nxt
            out_fn, in_ap, srcbuf = self.srcs[j]
            b = self.bufs[j % nb]
            self.k.dma(self.q, out_fn(b), in_ap, r=[srcbuf], w=[b])
            self.nxt += 1
        return self.bufs[n % nb]


def precast_outproj(k, G, l):
    wov = G.w_out.ap[l].rearrange("(kc p) n -> p kc n", p=128)
    dst = G.WOB[l].ap.rearrange("p (kc n) -> p kc n", kc=KC)
    for j in range(4):
        k.dma("pool", dst[:, :, j * 512:(j + 1) * 512], wov[:, :, j * 512:(j + 1) * 512], r=[G.w_out], w=[G.WOB[l]])
    k.dma("pool", G.RWB[l].ap.rearrange("p (kc e) -> p kc e", kc=KC),
          G.router_w.ap[l].rearrange("(kc p) e -> p kc e", p=128), r=[G.router_w], w=[G.RWB[l]])


def precast_experts(k, G, l):
    w1v = G.exp_w1.ap[l].rearrange("e (kc p) n -> e p kc n", p=128)
    w2v = G.exp_w2.ap[l].rearrange("e (fc p) n -> e p fc n", p=128)
    for e in range(NE):
        for cbk in range(4):
            k.dma("pool", G.W1B[l][e].ap[cbk].rearrange("p (kc c) -> p kc c", kc=KC),
                  w1v[e][:, :, cbk * 512:(cbk + 1) * 512], r=[G.exp_w1], w=[G.W1B[l][e]])
        for hf in range(2):
            k.dma("pool", G.W2B[l][e].ap[hf].rearrange("p (fc c) -> p fc c", fc=4),
                  w2v[e][:, hf * 4:(hf + 1) * 4, :], r=[G.exp_w2], w=[G.W2B[l][e]])


def phase_moe(k, G, l, need_ctx, last):
    m0 = k.mark()
    tiles = list(range(NT)) if need_ctx else list(range(2, NT))
    blocks = [tiles[i:i + 4] for i in range(0, len(tiles), 4)]
    b2 = k.sb([NE, D], F32, "b2")
    k.dma("sp", b2.ap, G.exp_b2.ap[l], r=[G.exp_b2], w=[b2])
    b1u1 = k.sb([128, NE * 8], F32, "b1u1")
    ou, _ = PP_COLS[f"b1u{l}"]
    og, _ = PP_COLS[f"b1g{l}"]
    k.ts("dve", b1u1.ap, G.ppt.ap[:, ou:ou + NE * 8], 1.0, ALU.add, r=[G.ppt], w=[b1u1])
    mv = G.modv[l]
    for blk in blocks:
        nt = len(blk)
        ntok = nt * 128
        mb = k.mark()
        acc = k.sb([128, nt, D], F32, "acc")
        comb = k.sb([128, nt, NE], F32, "comb")
        k.dma("sp", comb.ap, G.COMB.ap[blk[0] * 128:(blk[0] + nt) * 128, :].rearrange("(t p) e -> p t e", p=128),
              r=[G.COMB], w=[comb])
        me = k.mark()
        h2 = k.sb([128, KC, 512], BF16, "h2blk")
        for ti, i in enumerate(blk):
            k.dma("sp" if ti % 2 == 0 else "act", h2.ap[:, :, ti * 128:(ti + 1) * 128], G.H2T.ap[i],
                  r=[G.H2T], w=[h2])
        actT = [k.sb([128, 8, 512], BF16, "actT") for _ in range(2)]
        w1r = [k.sb([128, KC, 512], BF16, "w1r") for _ in range(3)]
        w2r = [k.sb([128, 4, D], BF16, "w2r") for _ in range(3)]
        gt = [k.sb([128, 512], F32, "g") for _ in range(2)]
        sg = [k.sb([128, 512], F32, "sig") for _ in range(2)]
        u1 = [k.sb([128, 512], F32, "u1") for _ in range(2)]
        combT = k.sb([NE, 128], F32, "combT")
        ps_g = [k.ps([128, 512], F32, "psg") for _ in range(2)]
        ps_u = [k.ps([128, 512], F32, "psu") for _ in range(2)]
        ps_o = [k.ps([128, 512], F32, "pso") for _ in range(4)]
        s1 = WStream(k, w1r, [((lambda b: b.ap), G.W1B[l][e].ap[cbk].rearrange("p (kc c) -> p kc c", kc=KC),
                               G.W1B[l][e]) for e in range(NE) for cbk in range(4)], q="sp")
        s2 = WStream(k, w2r, [((lambda b: b.ap), G.W2B[l][e].ap[hf].rearrange("p (fc c) -> p fc c", fc=4),
                               G.W2B[l][e]) for e in range(NE) for hf in range(2)], q="act", hold=2)
        oi = 0
        for ti in range(nt):
            pT = ps_o[oi % 4]
            oi += 1
            k.op("pe", lambda e: e.transpose(out=pT.ap[:NE, :128], in_=comb.ap[:, ti, :], identity=G.ident_f.ap),
                 r=[comb, G.ident_f], w=[pT])
            k.copy("dve", combT.ap, pT.ap[:NE, :128], r=[pT], w=[combT])
            for dblk in range(4):
                po = ps_o[oi % 4]
                oi += 1
                k.mm(po.ap, combT.ap, b2.ap[:, dblk * 512:(dblk + 1) * 512], start=True, stop=True,
                     r=[combT, b2], w=[po])
                k.copy("act", acc.ap[:, ti, dblk * 512:(dblk + 1) * 512], po.ap, r=[po], w=[acc])

        fi = [0]

        def first(e):
            at = actT[e % 2]
            for cbk in range(4):
                wb = s1.get(e * 4 + cbk)
                for s in range(2):
                    fc = cbk * 2 + s
                    pg = ps_g[fi[0] % 2]
                    pu = ps_u[fi[0] % 2]
                    g, sig, uu = gt[fi[0] % 2], sg[fi[0] % 2], u1[fi[0] % 2]
                    fi[0] += 1
                    wsl = wb.ap[:, :, s * 256:(s + 1) * 256].rearrange("p kc (f two) -> p kc f two", two=2)
                    for kc in range(KC):
                        k.mm(pg.ap[:, :ntok], wsl[:, kc, :, 0], h2.ap[:, kc, :ntok], start=(kc == 0),
                             stop=(kc == KC - 1), r=[wb, h2], w=[pg])
                    for kc in range(KC):
                        k.mm(pu.ap[:, :ntok], wsl[:, kc, :, 1], h2.ap[:, kc, :ntok], start=(kc == 0),
                             stop=(kc == KC - 1), r=[wb, h2], w=[pu])
                    col = e * 8 + fc
                    k.ts("dve", g.ap[:, :ntok], pg.ap[:, :ntok], G.ppt.ap[:, og + col:og + col + 1], ALU.add,
                         7.0, ALU.min, r=[pg, G.ppt], w=[g])
                    k.act(sig.ap[:, :ntok], g.ap[:, :ntok], AF.Sigmoid, r=[g], w=[sig], scale=1.702)
                    k.ts("dve", uu.ap[:, :ntok], pu.ap[:, :ntok], b1u1.ap[:, col:col + 1], ALU.add,
                         -6.0, ALU.max, r=[pu, b1u1], w=[uu])
                    k.tt("dve", g.ap[:, :ntok], g.ap[:, :ntok], sig.ap[:, :ntok], ALU.mult, r=[g, sig], w=[g])
                    k.stt(at.ap[:, fc, :ntok], uu.ap[:, :ntok], 8.0, g.ap[:, :ntok], ALU.min, ALU.mult,
                          r=[uu, g], w=[at])

        oi2 = [oi]

        def second(e):
            at = actT[e % 2]
            wh = [s2.get(e * 2), s2.get(e * 2 + 1)]
            for ti in range(nt):
                for dblk in range(4):
                    po = ps_o[oi2[0] % 4]
                    oi2[0] += 1
                    for fc in range(8):
                        wb = wh[fc // 4]
                        k.mm(po.ap, at.ap[:, fc, ti * 128:(ti + 1) * 128],
                             wb.ap[:, fc % 4, dblk * 512:(dblk + 1) * 512], start=(fc == 0), stop=(fc == 7),
                             r=[at, wb], w=[po])
                    sl = slice(dblk * 512, (dblk + 1) * 512)
                    k.stt(acc.ap[:, ti, sl], po.ap, comb.ap[:, ti, e:e + 1], acc.ap[:, ti, sl], ALU.mult, ALU.add,
                          r=[po, comb, acc], w=[acc])

        for e in range(NE):
            first(e)
            if e > 0:
                second(e - 1)
        second(NE - 1)
        k.release(me)
        g2 = [None, None]
        xts = [k.sb([128, D], F32, "xt") for _ in range(2)]
        fg = None
        if last:
            fg = k.sb([128, D], F32, "fgb")
            k.dma("sp", fg.ap, G.final_g.ap.to_broadcast([128, D]), r=[G.final_g], w=[fg])
            scr = k.sb([128, D], BF16, "scr")
            ssq = k.sb([128, 1], F32, "ssq")
            rstd = k.sb([128, 1], F32, "rstd")
            ots = [k.sb([128, D], F32, "ot") for _ in range(2)]
        for ti, i in enumerate(blk):
            which = 1 if i < 2 else 0
            if g2[which] is None:
                g2[which] = k.sb([128, D], F32, "g2b")
                k.dma("sp", g2[which].ap, mv.ap[which:which + 1, 5, :].to_broadcast([128, D]), r=[mv], w=[g2[which]])
            xt = xts[ti % 2]
            k.dma("sp", xt.ap, G.xres.ap[i * 128:(i + 1) * 128, :], r=[G.xres], w=[xt])
            k.tt("dve", acc.ap[:, ti, :], acc.ap[:, ti, :], g2[which].ap, ALU.mult, r=[acc, g2[which]], w=[acc])
            k.tt("pool", xt.ap, xt.ap, acc.ap[:, ti, :], ALU.add, r=[xt, acc], w=[xt])
            if not last:
                k.dma("sp", G.xres.ap[i * 128:(i + 1) * 128, :], xt.ap, r=[xt], w=[G.xres])
            else:
                ot = ots[ti % 2]
                k.act(scr.ap, xt.ap, AF.Square, r=[xt], w=[scr, ssq], accum_out=ssq.ap)
                k.act(rstd.ap, ssq.ap, AF.Sqrt, r=[ssq, G.eps], w=[rstd], scale=1.0 / D, bias=G.eps.ap)
                k.op("dve", lambda e: e.reciprocal(out=rstd.ap, in_=rstd.ap), r=[rstd], w=[rstd])
                k.stt(ot.ap, xt.ap, rstd.ap, fg.ap, ALU.mult, ALU.mult, r=[xt, rstd, fg], w=[ot])
                k.dma("sp", G.out.ap[(i - 2) * 128:(i - 1) * 128, :], ot.ap, r=[ot], w=[G.out])
        k.release(mb)
    k.release(m0)


_CACHE = {}


def kernel(**inputs):
    n = 8
    if "nc" not in _CACHE:
        _CACHE["nc"] = build_program(upto="all")[0]
    nc = _CACHE["nc"]
    maps = make_in_maps(inputs, list(range(n)))
    res = run_bass_kernel_spmd(nc, maps, core_ids=list(range(n)))
    out = np.stack([np.asarray(r["out"], dtype=np.float32) for r in res.results], axis=0)
    return out
```

```python
import math
import numpy as np
import concourse.bass as bass
import concourse.mybir as mybir
from concourse.bass_utils import run_bass_kernel_spmd

F32 = mybir.dt.float32
BF16 = mybir.dt.bfloat16
I32 = mybir.dt.int32
AF = mybir.ActivationFunctionType
ALU = mybir.AluOpType
AX = mybir.AxisListType

D = 2048
SEQ = 2048
CTX = 256
T = SEQ + CTX
NT = T // 128
DEPTH = 2
GW = 512
N_IN = 5120
NE = 32
DFF = 1024
EPS = 1e-6
KC = D // 128
NTLMAX = NT * 4 + NE
BIG = 1.0e6


class Buf:
    __slots__ = ("ap", "w", "r", "name")

    def __init__(self, ap, name=""):
        self.ap = ap
        self.w = None
        self.r = {}
        self.name = name

    def __getitem__(self, idx):
        return self.ap[idx]


class KB:
    RING = 8

    def __init__(self, nc):
        self.nc = nc
        self.eng = {"pe": nc.tensor, "act": nc.scalar, "dve": nc.vector, "pool": nc.gpsimd, "sp": nc.sync}
        self.csem = {}
        self.cnt = {}
        for e in ("pe", "act", "dve", "pool"):
            self.csem[e] = nc.alloc_semaphore("c_" + e)
            self.cnt[e] = 0
        self.pending = {e: False for e in self.cnt}
        self.rings = {}
        self.dcount = {}
        for q in ("sp", "act", "pool"):
            self.rings[q] = [nc.alloc_semaphore(f"r_{q}{i}") for i in range(self.RING)]
            self.dcount[q] = 0
        self.waited = {}
        self.n_ins = 0
        self.n_wait = 0
        self._uid = 0

    def sb(self, shape, dtype, name=None):
        self._uid += 1
        name = f"{name or 't'}_{self._uid}"
        return Buf(self.nc.alloc_sbuf_tensor(name, list(shape), dtype).ap(), name)

    def ps(self, shape, dtype=F32, name=None):
        self._uid += 1
        name = f"{name or 'p'}_{self._uid}"
        return Buf(self.nc.alloc_psum_tensor(name, list(shape), dtype).ap(), name)

    def dram(self, name, shape, dtype, kind="Internal"):
        return Buf(self.nc.dram_tensor(name, list(shape), dtype, kind=kind).ap(), name)

    def mark(self):
        nc = self.nc
        return (nc.sbuf_base, nc.sbuf_top, nc.psum_base, nc.psum_top)

    def release(self, m):
        self.barrier()
        nc = self.nc
        nc.sbuf_base, nc.sbuf_top, nc.psum_base, nc.psum_top = m

    def _wait(self, ename, ev):
        sem, val = ev
        key = (ename, sem.num if hasattr(sem, "num") else id(sem))
        if self.waited.get(key, 0) >= val:
            return
        self.eng[ename].wait_ge(sem, val)
        self.waited[key] = val
        self.n_wait += 1

    def _deps(self, ename, r, w):
        evs = []
        for b in r:
            if b.w is not None:
                evs.append(b.w)
        for b in w:
            if b.w is not None:
                evs.append(b.w)
            evs.extend(b.r.values())
        own = self.csem.get(ename)
        for ev in evs:
            if ename == "pe" and ev[0] is own:
                continue
            self._wait(ename, ev)

    def _record(self, ev, r, w):
        sid = id(ev[0])
        for b in r:
            cur = b.r.get(sid)
            if cur is None or cur[1] < ev[1]:
                b.r[sid] = ev
        for b in w:
            b.w = ev
            b.r = {}

    def op(self, ename, fn, r=(), w=(), signal=True):
        self._deps(ename, r, w)
        ins = fn(self.eng[ename])
        self.n_ins += 1
        if signal:
            self.cnt[ename] += 1
            ins.then_inc(self.csem[ename], 1)
            ev = (self.csem[ename], self.cnt[ename])
        else:
            ev = (self.csem[ename], self.cnt[ename] + 1)
        self._record(ev, r, w)
        return ins

    def dma(self, q, out, in_, r=(), w=(), **kw):
        self._deps(q, r, w)
        i = self.dcount[q]
        slot = i % self.RING
        sem = self.rings[q][slot]
        if i >= self.RING:
            self._wait(q, (sem, 16 * (i // self.RING)))
        ins = self.eng[q].dma_start(out=out, in_=in_, **kw)
        ins.then_inc(sem, 16)
        self.n_ins += 1
        self.dcount[q] = i + 1
        ev = (sem, 16 * (i // self.RING + 1))
        self._record(ev, r, w)
        return ev

    def bound_reg(self, val):
        if not hasattr(self, "_bregs"):
            self._bregs = {}
        if val not in self._bregs:
            reg = self.nc.alloc_register(mybir.EngineType.Pool, f"bnd{val}")
            self.nc.reg_mov(reg, val)
            self._bregs[val] = reg
        return self._bregs[val]

    def idma(self, out, in_, out_off=None, in_off=None, bounds=None, r=(), w=()):
        q = "pool"
        self._deps(q, r, w)
        i = self.dcount[q]
        slot = i % self.RING
        sem = self.rings[q][slot]
        if i >= self.RING:
            self._wait(q, (sem, 16 * (i // self.RING)))
        oo = bass.IndirectOffsetOnAxis(ap=out_off, axis=0) if out_off is not None else None
        io = bass.IndirectOffsetOnAxis(ap=in_off, axis=0) if in_off is not None else None
        ins = self.nc.gpsimd.indirect_dma_start(out=out, out_offset=oo, in_=in_, in_offset=io,
                                                bounds_check=self.bound_reg(bounds), oob_is_err=False)
        ins.then_inc(sem, 16)
        self.n_ins += 1
        self.dcount[q] = i + 1
        ev = (sem, 16 * (i // self.RING + 1))
        self._record(ev, r, w)
        return ev

    def all_events(self):
        evs = [(self.csem[e], self.cnt[e]) for e in self.cnt if self.cnt[e] > 0]
        for q in self.rings:
            n = self.dcount[q]
            for slot in range(self.RING):
                k = (n - slot + self.RING - 1) // self.RING
                if k > 0:
                    evs.append((self.rings[q][slot], 16 * k))
        return evs

    def barrier(self, engines=("pe", "act", "dve", "pool", "sp")):
        evs = self.all_events()
        for e in engines:
            for ev in evs:
                self._wait(e, ev)

    def mm(self, out_ap, lhsT, rhs, start, stop, r=(), w=(), signal=None, **kw):
        if signal is None:
            signal = True
        return self.op("pe", lambda e: e.matmul(out_ap, lhsT, rhs, start=start, stop=stop, **kw),
                       r=r, w=w, signal=signal)

    def act(self, out, in_, func, r=(), w=(), eng="act", **kw):
        return self.op(eng, lambda e: e.activation(out=out, in_=in_, func=func, **kw), r=r, w=w)

    def tt(self, eng, out, in0, in1, op, r=(), w=()):
        return self.op(eng, lambda e: e.tensor_tensor(out=out, in0=in0, in1=in1, op=op), r=r, w=w)

    def ts(self, eng, out, in0, s1, op0, s2=None, op1=None, r=(), w=(), **kw):
        if op1 is None:
            return self.op(eng, lambda e: e.tensor_scalar(out=out, in0=in0, scalar1=s1, scalar2=None,
                                                          op0=op0, **kw), r=r, w=w)
        return self.op(eng, lambda e: e.tensor_scalar(out=out, in0=in0, scalar1=s1, scalar2=s2,
                                                      op0=op0, op1=op1, **kw), r=r, w=w)

    def stt(self, out, in0, scalar, in1, op0, op1, r=(), w=(), **kw):
        return self.op("dve", lambda e: e.scalar_tensor_tensor(out=out, in0=in0, scalar=scalar, in1=in1,
                                                               op0=op0, op1=op1, **kw), r=r, w=w)

    def copy(self, eng, out, in_, r=(), w=()):
        if eng == "act":
            return self.op("act", lambda e: e.activation(out=out, in_=in_, func=AF.Copy), r=r, w=w)
        return self.op(eng, lambda e: e.tensor_copy(out=out, in_=in_), r=r, w=w)


def _pp_layout():
    cols = {}
    off = 0

    def add(name, n):
        nonlocal off
        cols[name] = (off, n)
        off += n

    add("cvec", 32)
    add("iota", NTLMAX)
    add("pidx", 1)
    add("ltri", 128)
    for l in range(DEPTH):
        add(f"conv_a_w{l}", 4 * 31)
        add(f"conv_a_b{l}", 4)
        add(f"ln_a_g{l}", 4)
        add(f"ln_a_b{l}", 4)
        add(f"diff_norm_g{l}", 1)
        add(f"conv_c_w{l}", 4 * 3)
        add(f"norm_c_g{l}", 4)
        add(f"lru_conv_w{l}", 2 * 4 * 4)
        add(f"lru_conv_b{l}", 8)
        add(f"lru_ba{l}", 8)
        add(f"lru_bx{l}", 8)
        add(f"lru_lam{l}", 8)
        add(f"norm_d_g{l}", 4)
    return cols, off


PP_COLS, NPP = _pp_layout()


def _pack_pp(inp, b):
    pp = np.zeros((128, NPP), np.float32)

    def put(name, arr):
        o, n = PP_COLS[name]
        pp[:, o:o + n] = np.ascontiguousarray(arr, dtype=np.float32).reshape(128, n)

    def chp(v):
        return np.asarray(v).reshape(4, 128).T

    cv = np.concatenate([np.asarray(inp["c"][b]).reshape(16, 128).T,
                         np.asarray(inp["c_ctx"]).reshape(16, 128).T], axis=1)
    put("cvec", cv)
    put("iota", np.tile(np.arange(NTLMAX, dtype=np.float32), (128, 1)))
    put("pidx", np.arange(128, dtype=np.float32).reshape(128, 1))
    put("ltri", (np.arange(128)[:, None] < np.arange(128)[None, :]).astype(np.float32))
    for l in range(DEPTH):
        put(f"conv_a_w{l}", np.asarray(inp["conv_a_w"][l]).reshape(31, 4, 128).transpose(2, 1, 0))
        put(f"conv_a_b{l}", chp(inp["conv_a_b"][l]))
        put(f"ln_a_g{l}", chp(inp["ln_a_g"][l]))
        put(f"ln_a_b{l}", chp(inp["ln_a_b"][l]))
        put(f"diff_norm_g{l}", np.asarray(inp["diff_norm_g"][l]).reshape(128, 1))
        put(f"conv_c_w{l}", np.asarray(inp["conv_c_w"][l]).reshape(3, 4, 128).transpose(2, 1, 0))
        put(f"norm_c_g{l}", chp(inp["norm_c_g"][l]))
        put(f"lru_conv_w{l}", np.asarray(inp["lru_conv_w"][l]).reshape(2, 4, 4, 128).transpose(3, 0, 2, 1))
        for nm in ("lru_conv_b", "lru_ba", "lru_bx", "lru_lam"):
            put(f"{nm}{l}", np.asarray(inp[nm][l]).reshape(2, 4, 128).transpose(2, 0, 1))
        put(f"norm_d_g{l}", chp(inp["norm_d_g"][l]))
    return pp


def _pack_b1t(inp, l):
    b1 = np.asarray(inp["exp_b1"][l], dtype=np.float32)
    g = b1[:, 0::2].reshape(NE, 8, 128).transpose(0, 2, 1).reshape(NE * 128, 8)
    u = b1[:, 1::2].reshape(NE, 8, 128).transpose(0, 2, 1).reshape(NE * 128, 8)
    return np.ascontiguousarray(np.concatenate([g, u], axis=1))


def _rope_tables():
    GRID_W = 64
    rows = SEQ // GRID_W
    row = np.repeat(np.arange(rows, dtype=np.float32), GRID_W)
    col = np.tile(np.arange(GRID_W, dtype=np.float32), rows)
    half = 32
    inv = (np.float32(10000.0) ** (-np.arange(0, half, 2, dtype=np.float32) / np.float32(half))).astype(np.float32)
    ar = row[:, None] * inv
    ac = col[:, None] * inv
    ang = np.concatenate([ar, ar, ac, ac], axis=-1)
    cos = np.cos(ang).astype(np.float32)
    sin = np.sin(ang).astype(np.float32)
    sgn = np.concatenate([-np.ones(16), np.ones(16), -np.ones(16), np.ones(16)]).astype(np.float32)
    sin = sin * sgn[None, :]
    tab = np.zeros((2, 128, T), np.float32)
    tab[0, :, :CTX] = 1.0
    tab[0, 0:64, CTX:] = cos.T
    tab[0, 64:128, CTX:] = cos.T
    tab[1, 0:64, CTX:] = sin.T
    tab[1, 64:128, CTX:] = sin.T
    return tab


class Prog:
    pass


def tok_chunks(t0=0, t1=T, step=512):
    out = []
    t = t0
    while t < t1:
        n = min(step, t1 - t)
        out.append((t, n))
        t += n
    return out


def declare_io(k, G):
    nc = k.nc

    def inp(name, shape, dt=F32):
        return k.dram(name, shape, dt, kind="ExternalInput")

    G.x = inp("x", [SEQ, D])
    G.ctx = inp("ctx", [CTX, D])
    G.pp = inp("pp", [128, NPP])
    G.rope = inp("rope", [2, 128, T])
    G.ada_w = inp("ada_w", [DEPTH, D, 6 * D])
    G.ada_b = inp("ada_b", [DEPTH, 6 * D])
    G.norm1_g = inp("norm1_g", [DEPTH, D])
    G.norm2_g = inp("norm2_g", [DEPTH, D])
    G.final_g = inp("final_g", [1, D])
    G.w_in = inp("w_in", [DEPTH, D, N_IN])
    G.w_out = inp("w_out", [DEPTH, D, D])
    G.diff_lambda = inp("diff_lambda", [DEPTH, 256])
    G.lru_wa = inp("lru_wa", [DEPTH, 2, 8, 64, 64])
    G.lru_wx = inp("lru_wx", [DEPTH, 2, 8, 64, 64])
    G.router_w = inp("router_w", [DEPTH, D, NE])
    G.router_b = inp("router_b", [DEPTH, NE])
    if G.with_moe:
        G.exp_w1 = inp("exp_w1", [DEPTH, NE, D, 2 * DFF])
        G.exp_w2 = inp("exp_w2", [DEPTH, NE, DFF, D])
        G.exp_b2 = inp("exp_b2", [DEPTH, NE, D])
        G.b1t = [inp(f"b1t{l}", [NE * 128, 16]) for l in range(DEPTH)]
    G.out = k.dram("out", [SEQ, D], F32, kind="ExternalOutput")
    G.xres = k.dram("xres", [T, D], F32)
    G.modv = [k.dram(f"modv{l}", [2, 6, D], F32) for l in range(DEPTH)]
    G.UF = k.dram("UF", [3584, T], F32)
    G.QK = k.dram("QK", [1024, T], BF16)
    G.V = k.dram("Vtm", [T, 512], BF16)
    G.YC = k.dram("YC", [D, T], BF16)
    G.H2TM = k.dram("H2TM", [T, D], BF16)
    G.COMB = k.dram("COMB", [T, NE], F32)
    G.LGD = k.dram("LGD", [T, NE], F32)
    G.TOPD = k.dram("TOPD", [T, 8], F32)
    G.RANKD = k.dram("RANKD", [T, NE], F32)
    G.CNTD = k.dram("CNTD", [128, NE], F32)
    G.XS = k.dram("XS", [NTLMAX * 128, D], BF16)
    G.YS = k.dram("YS", [NTLMAX * 128, D], F32)
    G.WOB = [k.dram(f"wob{l}", [128, KC * D], BF16) for l in range(DEPTH)]
    G.RWB = [k.dram(f"rwb{l}", [128, KC * NE], BF16) for l in range(DEPTH)]
    if G.with_moe:
        G.W1B = [[k.dram(f"w1b{l}_{c}", [NE * 128, KC * 512], BF16) for c in range(4)] for l in range(DEPTH)]
        G.W2B = [[k.dram(f"w2b{l}_{h}", [NE * 128, 4 * D], BF16) for h in range(2)] for l in range(DEPTH)]


def phase_consts(k, G):
    G.ppt = k.sb([128, NPP], F32, "pp")
    k.dma("sp", G.ppt.ap, G.pp.ap, r=[G.pp], w=[G.ppt])
    G.ident_f = k.sb([128, 128], F32, "identf")
    G.ident_b = k.sb([128, 128], BF16, "identb")
    G.ones_f = k.sb([128, 128], F32, "onesf")
    k.op("pool", lambda e: e.memset(G.ident_f.ap, 0.0), w=[G.ident_f])
    k.op("pool", lambda e: e.memset(G.ones_f.ap, 1.0), w=[G.ones_f])
    k.op("pool", lambda e: e.affine_select(out=G.ident_f.ap, in_=G.ones_f.ap, pattern=[[-1, 128]],
                                           compare_op=ALU.is_equal, fill=0.0, base=0, channel_multiplier=1),
         r=[G.ones_f], w=[G.ident_f])
    k.copy("dve", G.ident_b.ap, G.ident_f.ap, r=[G.ident_f], w=[G.ident_b])
    G.eps = k.sb([128, 1], F32, "eps")
    k.op("pool", lambda e: e.memset(G.eps.ap, EPS), w=[G.eps])
    k.dma("sp", G.xres.ap[0:CTX, :], G.ctx.ap, r=[G.ctx], w=[G.xres])
    k.dma("sp", G.xres.ap[CTX:T, :], G.x.ap, r=[G.x], w=[G.xres])


def ppcol(G, name, i=0, n=1):
    o, _ = PP_COLS[name]
    return G.ppt.ap[:, o + i:o + i + n]


def phase_mod(k, G, l):
    m = k.mark()
    s = k.sb([128, 16, 2], F32, "silu_c")
    o, _ = PP_COLS["cvec"]
    k.act(s.ap[:, :, 0], G.ppt.ap[:, o:o + 16], AF.Silu, r=[G.ppt], w=[s])
    k.act(s.ap[:, :, 1], G.ppt.ap[:, o + 16:o + 32], AF.Silu, r=[G.ppt], w=[s])
    mod = k.sb([2, 6 * D], F32, "mod")
    adab = k.sb([2, 6 * D], F32, "adab")
    k.dma("sp", adab.ap, G.ada_b.ap[l:l + 1, :].to_broadcast([2, 6 * D]), r=[G.ada_b], w=[adab])
    wbufs = [k.sb([128, KC, 512], F32, f"adaw{i}") for i in range(2)]
    pss = [k.ps([2, 512], F32, f"modps{i}") for i in range(2)]
    awv = G.ada_w.ap[l].rearrange("(kc p) n -> p kc n", p=128)
    for cb in range(24):
        wb = wbufs[cb % 2]
        ps = pss[cb % 2]
        k.dma("sp" if cb % 2 == 0 else "act", wb.ap, awv[:, :, cb * 512:(cb + 1) * 512], r=[G.ada_w], w=[wb])
        for kc in range(KC):
            k.mm(ps.ap, s.ap[:, kc, :], wb.ap[:, kc, :], start=(kc == 0), stop=(kc == KC - 1),
                 r=[s, wb], w=[ps])
        k.tt("dve", mod.ap[:, cb * 512:(cb + 1) * 512], ps.ap, adab.ap[:, cb * 512:(cb + 1) * 512], ALU.add,
             r=[ps, adab], w=[mod])
    n1 = k.sb([2, D], F32, "n1")
    n2 = k.sb([2, D], F32, "n2")
    k.dma("sp", n1.ap, G.norm1_g.ap[l:l + 1, :].to_broadcast([2, D]), r=[G.norm1_g], w=[n1])
    k.dma("sp", n2.ap, G.norm2_g.ap[l:l + 1, :].to_broadcast([2, D]), r=[G.norm2_g], w=[n2])
    gs1 = k.sb([2, D], F32, "gs1")
    gs2 = k.sb([2, D], F32, "gs2")
    k.stt(gs1.ap, mod.ap[:, D:2 * D], 1.0, n1.ap, ALU.add, ALU.mult, r=[mod, n1], w=[gs1])
    k.stt(gs2.ap, mod.ap[:, 4 * D:5 * D], 1.0, n2.ap, ALU.add, ALU.mult, r=[mod, n2], w=[gs2])
    mv = G.modv[l]
    k.dma("sp", mv.ap[:, 0, :], gs1.ap, r=[gs1], w=[mv])
    k.dma("sp", mv.ap[:, 1, :], mod.ap[:, 0:D], r=[mod], w=[mv])
    k.dma("sp", mv.ap[:, 2, :], mod.ap[:, 2 * D:3 * D], r=[mod], w=[mv])
    k.dma("sp", mv.ap[:, 3, :], gs2.ap, r=[gs2], w=[mv])
    k.dma("sp", mv.ap[:, 4, :], mod.ap[:, 3 * D:4 * D], r=[mod], w=[mv])
    k.dma("sp", mv.ap[:, 5, :], mod.ap[:, 5 * D:6 * D], r=[mod], w=[mv])
    k.release(m)


def load_bcast(k, G, l, sec, which, name):
    t = k.sb([128, D], F32, name)
    mv = G.modv[l]
    k.dma("sp", t.ap, mv.ap[which:which + 1, sec, :].to_broadcast([128, D]), r=[mv], w=[t])
    return t


def norm_mod_tile(k, G, xt, gs, sh, hb, scr, ssq, rstd, tmp):
    k.act(scr.ap, xt.ap, AF.Square, r=[xt], w=[scr, ssq], accum_out=ssq.ap)
    k.act(rstd.ap, ssq.ap, AF.Sqrt, r=[ssq, G.eps], w=[rstd], scale=1.0 / D, bias=G.eps.ap)
    k.op("dve", lambda e: e.reciprocal(out=rstd.ap, in_=rstd.ap), r=[rstd], w=[rstd])
    k.stt(tmp.ap, xt.ap, rstd.ap, gs.ap, ALU.mult, ALU.mult, r=[xt, rstd, gs], w=[tmp])
    k.tt("pool", hb.ap, tmp.ap, sh.ap, ALU.add, r=[tmp, sh], w=[hb])


def phase_inproj(k, G, l):
    m = k.mark()
    hT_t = k.nc.alloc_sbuf_tensor(f"hT{l}", [128, KC, T], BF16).ap()
    hT = [Buf(hT_t, f"hT{i}") for i in range(NT)]
    m1 = k.mark()
    gs = [load_bcast(k, G, l, 0, w, "gs1") for w in range(2)]
    sh = [load_bcast(k, G, l, 1, w, "sh1") for w in range(2)]
    xts = [k.sb([128, D], F32, "xt") for _ in range(2)]
    tmp = k.sb([128, D], F32, "tmp")
    scr = k.sb([128, D], BF16, "scr")
    hbs = [k.sb([128, D], BF16, "hb") for _ in range(2)]
    ssq = k.sb([128, 1], F32, "ssq")
    rstd = k.sb([128, 1], F32, "rstd")
    pts = [k.ps([128, 1024], BF16, "pt") for _ in range(2)]
    for i in range(NT):
        which = 1 if i < 2 else 0
        xt = xts[i % 2]
        hb = hbs[i % 2]
        k.dma("sp", xt.ap, G.xres.ap[i * 128:(i + 1) * 128, :], r=[G.xres], w=[xt])
        norm_mod_tile(k, G, xt, gs[which], sh[which], hb, scr, ssq, rstd, tmp)
        for half in range(2):
            pt = pts[half]
            for j in range(8):
                kc = half * 8 + j
                k.op("pe", lambda e: e.transpose(out=pt.ap[:, j * 128:(j + 1) * 128],
                                                 in_=hb.ap[:, kc * 128:(kc + 1) * 128], identity=G.ident_b.ap),
                     r=[hb, G.ident_b], w=[pt])
            k.copy("act" if half == 0 else "dve",
                   hT_t[:, half * 8:(half + 1) * 8, i * 128:(i + 1) * 128],
                   pt.ap.rearrange("p (a b) -> p a b", a=8), r=[pt], w=[hT[i]])
    k.release(m1)
    cos2 = k.sb([128, T], F32, "cos2")
    sin2 = k.sb([128, T], F32, "sin2")
    k.dma("sp", cos2.ap, G.rope.ap[0], r=[G.rope], w=[cos2])
    k.dma("sp", sin2.ap, G.rope.ap[1], r=[G.rope], w=[sin2])
    wbufs = [k.sb([128, KC, 512], BF16, "wblk") for _ in range(2)]
    wperm = k.sb([128, KC, 512], BF16, "wperm")
    pss = [k.ps([128, 512], F32, "ps") for _ in range(6)]
    stage_f = [k.sb([128, T], F32, "stf") for _ in range(2)]
    stage_b = [k.sb([128, T], BF16, "stb") for _ in range(2)]
    t1 = k.sb([128, 512], F32, "ropet1")
    t2 = k.sb([128, 512], F32, "ropet2")
    vst = [k.sb([128, 512], BF16, "vst") for _ in range(2)]
    wv = G.w_in.ap[l].rearrange("(kc p) n -> p kc n", p=128)
    chunks = tok_chunks()
    psi = [0]
    evi = [0]

    def nextps():
        p = pss[psi[0] % len(pss)]
        psi[0] += 1
        return p

    def proj_fm(wb, mblk, n0, nn):
        ps = nextps()
        tiles = [hT[i] for i in range(n0 // 128, (n0 + nn) // 128)]
        for kc in range(KC):
            k.mm(ps.ap[:, :nn], wb.ap[:, kc, mblk * 128:(mblk + 1) * 128], hT_t[:, kc, n0:n0 + nn],
                 start=(kc == 0), stop=(kc == KC - 1), r=[wb] + tiles, w=[ps])
        return ps

    uf_row = 0
    nst = 0
    for cb in range(10):
        wb = wbufs[cb % 2]
        k.dma("pool", wb.ap, wv[:, :, cb * 512:(cb + 1) * 512], r=[G.w_in], w=[wb])
        if cb in (2, 3):
            src = wb.ap.rearrange("p kc (g s j) -> p (kc g) s j", s=2, j=16)
            dst = wperm.ap.rearrange("p kc (g s j) -> p (kc g) s j", s=2, j=16)
            k.copy("dve", dst[:, :, 0, :], src[:, :, 1, :], r=[wb], w=[wperm])
            k.copy("dve", dst[:, :, 1, :], src[:, :, 0, :], r=[wb], w=[wperm])
            for mblk in range(4):
                st = stage_b[nst % 2]
                nst += 1
                for (n0, nn) in chunks:
                    pa = proj_fm(wb, mblk, n0, nn)
                    pb = proj_fm(wperm, mblk, n0, nn)
                    k.tt("dve", t1.ap[:, :nn], pa.ap[:, :nn], cos2.ap[:, n0:n0 + nn], ALU.mult,
                         r=[pa, cos2], w=[t1])
                    k.tt("dve", t2.ap[:, :nn], pb.ap[:, :nn], sin2.ap[:, n0:n0 + nn], ALU.mult,
                         r=[pb, sin2], w=[t2])
                    k.tt("pool", st.ap[:, n0:n0 + nn], t1.ap[:, :nn], t2.ap[:, :nn], ALU.add,
                         r=[t1, t2], w=[st])
                row = (cb - 2) * 512 + mblk * 128
                k.dma("sp", G.QK.ap[row:row + 128, :], st.ap, r=[st], w=[G.QK])
        elif cb == 4:
            for i in range(NT):
                ps = nextps()
                for kc in range(KC):
                    k.mm(ps.ap, hT_t[:, kc, i * 128:(i + 1) * 128], wb.ap[:, kc, :],
                         start=(kc == 0), stop=(kc == KC - 1), r=[wb, hT[i]], w=[ps])
                vs = vst[i % 2]
                k.copy("act" if i % 2 == 0 else "dve", vs.ap, ps.ap, r=[ps], w=[vs])
                k.dma("sp", G.V.ap[i * 128:(i + 1) * 128, :], vs.ap, r=[vs], w=[G.V])
        else:
            for mblk in range(4):
                st = stage_f[nst % 2]
                nst += 1
                for (n0, nn) in chunks:
                    ps = proj_fm(wb, mblk, n0, nn)
                    k.copy("act" if evi[0] % 2 == 0 else "dve", st.ap[:, n0:n0 + nn], ps.ap[:, :nn],
                           r=[ps], w=[st])
                    evi[0] += 1
                k.dma("sp", G.UF.ap[uf_row:uf_row + 128, :], st.ap, r=[st], w=[G.UF])
                uf_row += 128
    assert uf_row == 3584
    k.release(m)


def build_program(upto="all", dbg=(), with_moe=True):
    nc = bass.Bass("TRN2", target_bir_lowering=False)
    k = KB(nc)
    G = Prog()
    G.with_moe = with_moe
    declare_io(k, G)
    phase_consts(k, G)
    done = False
    for l in range(DEPTH):
        phase_mod(k, G, l)
    if upto == "mod":
        done = True
    for l in range(DEPTH):
        if done:
            break
        phase_inproj(k, G, l)
        if upto == f"inproj{l}":
            break
        precast_outproj(k, G, l)
        if l == 0 and G.with_moe:
            G.bg = precast_list(k, G, 0)
            pump(G, len(G.bg))
        phase_mixers(k, G, l, need_ctx=(l < DEPTH - 1))
        if upto == f"mix{l}":
            break
        phase_outproj(k, G, l, need_ctx=(l < DEPTH - 1))
        if upto == f"outproj{l}":
            break
        if l + 1 < DEPTH and G.with_moe:
            G.bg = precast_list(k, G, l + 1)
        phase_moe(k, G, l, need_ctx=(l < DEPTH - 1), last=(l == DEPTH - 1))
        if upto == f"moe{l}":
            break
    for name in dbg:
        src = getattr(G, name) if not name.startswith("modv") else G.modv[int(name[4:])]
        o = k.dram("dbg_" + name, list(src.ap.shape), src.ap.dtype, kind="ExternalOutput")
        k.dma("sp", o.ap, src.ap, r=[src], w=[o])
    k.barrier(engines=("sp",))
    G.k = k
    return nc, G


def make_in_maps(inp, cores):
    rope = _rope_tables()
    shared = {
        "rope": rope,
        "ada_w": np.ascontiguousarray(inp["ada_w"], dtype=np.float32),
        "ada_b": np.ascontiguousarray(inp["ada_b"], dtype=np.float32),
        "norm1_g": np.ascontiguousarray(inp["norm1_g"], dtype=np.float32),
        "norm2_g": np.ascontiguousarray(inp["norm2_g"], dtype=np.float32),
        "final_g": np.ascontiguousarray(inp["final_g"], dtype=np.float32).reshape(1, D),
        "w_in": np.ascontiguousarray(inp["w_in"], dtype=np.float32),
        "w_out": np.ascontiguousarray(inp["w_out"], dtype=np.float32),
        "diff_lambda": np.ascontiguousarray(inp["diff_lambda"], dtype=np.float32).reshape(DEPTH, 256),
        "lru_wa": np.ascontiguousarray(inp["lru_wa"], dtype=np.float32),
        "lru_wx": np.ascontiguousarray(inp["lru_wx"], dtype=np.float32),
        "router_w": np.ascontiguousarray(inp["router_w"], dtype=np.float32),
        "router_b": np.ascontiguousarray(inp["router_b"], dtype=np.float32),
        "exp_w1": np.ascontiguousarray(inp["exp_w1"], dtype=np.float32),
        "exp_w2": np.ascontiguousarray(inp["exp_w2"], dtype=np.float32),
        "exp_b2": np.ascontiguousarray(inp["exp_b2"], dtype=np.float32),
    }
    for l in range(DEPTH):
        shared[f"b1t{l}"] = _pack_b1t(inp, l)
    maps = []
    for b in cores:
        mp = dict(shared)
        mp["x"] = np.ascontiguousarray(inp["x"][b], dtype=np.float32)
        mp["ctx"] = np.ascontiguousarray(inp["ctx"][b], dtype=np.float32)
        mp["pp"] = _pack_pp(inp, b)
        maps.append(mp)
    return maps


def useg(gap, step=512):
    out = [(0, CTX, 0)]
    for (t0, n) in tok_chunks(CTX, T, step):
        out.append((t0, n, t0 + gap))
    return out


def load_gapped(k, G, dst, dst_off_ctx, dst_off_lat, src_buf, row0, q="sp"):
    k.dma(q, dst.ap[:, dst_off_ctx:dst_off_ctx + CTX], src_buf.ap[row0:row0 + 128, 0:CTX], r=[src_buf], w=[dst])
    k.dma(q, dst.ap[:, dst_off_lat:dst_off_lat + SEQ], src_buf.ap[row0:row0 + 128, CTX:T], r=[src_buf], w=[dst])


def finish_norm(k, G, ys, gap, kind, l, yc_row0, gname, bname=None):
    m = k.mark()
    ps_sq = [k.ps([128, 512], F32, "pssq") for _ in range(2)]
    ps_su = [k.ps([128, 512], F32, "pssu") for _ in range(2)] if kind == "ln_silu" else None
    sq = [k.sb([128, 512], F32, "sq") for _ in range(2)]
    rstd = [k.sb([128, 512], F32, "rstd") for _ in range(2)]
    mean = [k.sb([128, 512], F32, "mean") for _ in range(2)]
    msq = k.sb([128, 512], F32, "msq")
    t1 = [k.sb([128, 512], F32, "t1") for _ in range(2)]
    stage = [k.sb([128, T], BF16, "ystage") for _ in range(4)]
    for ci, (t0, n, u0) in enumerate(useg(gap)):
        pq = ps_sq[ci % 2]
        rs = rstd[ci % 2]
        mn = mean[ci % 2]
        for c in range(4):
            s = sq[c % 2]
            k.act(s.ap[:, :n], ys[c].ap[:, u0:u0 + n], AF.Square, r=[ys[c]], w=[s])
            k.mm(pq.ap[:, :n], G.ones_f.ap, s.ap[:, :n], start=(c == 0), stop=(c == 3), r=[G.ones_f, s], w=[pq])
        if kind == "ln_silu":
            pu = ps_su[ci % 2]
            for c in range(4):
                k.mm(pu.ap[:, :n], G.ones_f.ap, ys[c].ap[:, u0:u0 + n], start=(c == 0), stop=(c == 3),
                     r=[G.ones_f, ys[c]], w=[pu])
            k.ts("dve", mn.ap[:, :n], pu.ap[:, :n], 1.0 / GW, ALU.mult, r=[pu], w=[mn])
            k.tt("dve", msq.ap[:, :n], mn.ap[:, :n], mn.ap[:, :n], ALU.mult, r=[mn], w=[msq])
            k.stt(rs.ap[:, :n], pq.ap[:, :n], 1.0 / GW, msq.ap[:, :n], ALU.mult, ALU.subtract, r=[pq, msq], w=[rs])
            k.act(rs.ap[:, :n], rs.ap[:, :n], AF.Sqrt, r=[rs, G.eps], w=[rs], bias=G.eps.ap)
        else:
            k.act(rs.ap[:, :n], pq.ap[:, :n], AF.Sqrt, r=[pq, G.eps], w=[rs], scale=1.0 / GW, bias=G.eps.ap)
        k.op("dve", lambda e: e.reciprocal(out=rs.ap[:, :n], in_=rs.ap[:, :n]), r=[rs], w=[rs])
        for c in range(4):
            gcol = ppcol(G, f"{gname}{l}", c)
            if kind == "ln_silu":
                bcol = ppcol(G, f"{bname}{l}", c)
                t = t1[c % 2]
                k.tt("dve", t.ap[:, :n], ys[c].ap[:, u0:u0 + n], mn.ap[:, :n], ALU.subtract, r=[ys[c], mn], w=[t])
                k.tt("pool", t.ap[:, :n], t.ap[:, :n], rs.ap[:, :n], ALU.mult, r=[t, rs], w=[t])
                k.act(stage[c].ap[:, t0:t0 + n], t.ap[:, :n], AF.Silu, r=[t, G.ppt], w=[stage[c]],
                      scale=gcol, bias=bcol)
            else:
                k.stt(stage[c].ap[:, t0:t0 + n], ys[c].ap[:, u0:u0 + n], gcol, rs.ap[:, :n], ALU.mult, ALU.mult,
                      r=[ys[c], rs, G.ppt], w=[stage[c]])
    for c in range(4):
        k.dma("sp", G.YC.ap[yc_row0 + c * 128:yc_row0 + (c + 1) * 128, :], stage[c].ap, r=[stage[c]], w=[G.YC])
    k.release(m)


def mixer_conformer(k, G, l):
    m = k.mark()
    GAP = 30
    NU = T + GAP
    ZW = NU + 30
    ys = [k.sb([128, NU], F32, "convA") for _ in range(4)]
    zps = [k.sb([128, ZW], F32, "zpA") for _ in range(2)]
    vals = [k.sb([128, T], F32, "valA") for _ in range(2)]
    gates = [k.sb([128, T], F32, "gateA") for _ in range(2)]
    for zp in zps:
        k.op("pool", lambda e: e.memset(zp.ap, 0.0), w=[zp])
    wo, _ = PP_COLS[f"conv_a_w{l}"]
    for c in range(4):
        zp = zps[c % 2]
        va = vals[c % 2]
        ga = gates[c % 2]
        k.dma("sp", va.ap, G.UF.ap[c * 128:(c + 1) * 128, :], r=[G.UF], w=[va])
        k.dma("act", ga.ap, G.UF.ap[512 + c * 128:512 + (c + 1) * 128, :], r=[G.UF], w=[ga])
        k.act(ga.ap, ga.ap, AF.Sigmoid, r=[ga], w=[ga])
        k.tt("pool", zp.ap[:, 15:15 + CTX], va.ap[:, 0:CTX], ga.ap[:, 0:CTX], ALU.mult, r=[va, ga], w=[zp])
        k.tt("pool", zp.ap[:, 45 + CTX:45 + CTX + SEQ], va.ap[:, CTX:T], ga.ap[:, CTX:T], ALU.mult,
             r=[va, ga], w=[zp])
        y = ys[c]
        wcol = lambda kk: G.ppt.ap[:, wo + c * 31 + kk:wo + c * 31 + kk + 1]
        k.ts("dve", y.ap, zp.ap[:, 0:NU], wcol(0), ALU.mult, ppcol(G, f"conv_a_b{l}", c), ALU.add,
             r=[zp, G.ppt], w=[y])
        for kk in range(1, 31):
            k.stt(y.ap, zp.ap[:, kk:kk + NU], wcol(kk), y.ap, ALU.mult, ALU.add, r=[zp, y, G.ppt], w=[y])
    finish_norm(k, G, ys, GAP, "ln_silu", l, 0, "ln_a_g", "ln_a_b")
    k.release(m)


def mixer_sconv(k, G, l):
    m = k.mark()
    GAP = 2
    NU = T + GAP
    ZW = NU + 2
    ys = [k.sb([128, NU], F32, "convC") for _ in range(4)]
    zps = [k.sb([128, ZW], F32, "zpC") for _ in range(2)]
    bgs = [k.sb([128, NU], F32, "bgC") for _ in range(2)]
    cgs = [k.sb([128, T], F32, "cgC") for _ in range(2)]
    vs = [k.sb([128, T], F32, "vC") for _ in range(2)]
    for zp in zps:
        k.op("pool", lambda e: e.memset(zp.ap, 0.0), w=[zp])
    for bg in bgs:
        k.op("pool", lambda e: e.memset(bg.ap, 0.0), w=[bg])
    wo, _ = PP_COLS[f"conv_c_w{l}"]
    for c in range(4):
        zp, bg, cg, v = zps[c % 2], bgs[c % 2], cgs[c % 2], vs[c % 2]
        load_gapped(k, G, bg, 0, CTX + GAP, G.UF, 1024 + c * 128, q="sp")
        k.dma("act", cg.ap, G.UF.ap[1536 + c * 128:1536 + (c + 1) * 128, :], r=[G.UF], w=[cg])
        k.dma("sp", v.ap, G.UF.ap[2048 + c * 128:2048 + (c + 1) * 128, :], r=[G.UF], w=[v])
        k.tt("pool", zp.ap[:, 1:1 + CTX], cg.ap[:, 0:CTX], v.ap[:, 0:CTX], ALU.mult, r=[cg, v], w=[zp])
        k.tt("pool", zp.ap[:, 3 + CTX:3 + CTX + SEQ], cg.ap[:, CTX:T], v.ap[:, CTX:T], ALU.mult, r=[cg, v], w=[zp])
        y = ys[c]
        wcol = lambda kk: G.ppt.ap[:, wo + c * 3 + kk:wo + c * 3 + kk + 1]
        k.ts("dve", y.ap, zp.ap[:, 0:NU], wcol(0), ALU.mult, r=[zp, G.ppt], w=[y])
        for kk in range(1, 3):
            k.stt(y.ap, zp.ap[:, kk:kk + NU], wcol(kk), y.ap, ALU.mult, ALU.add, r=[zp, y, G.ppt], w=[y])
        k.tt("dve", y.ap, y.ap, bg.ap, ALU.mult, r=[y, bg], w=[y])
    finish_norm(k, G, ys, GAP, "rms", l, 1024, "norm_c_g")
    k.release(m)


def mixer_lru(k, G, l):
    m = k.mark()
    GAP = 6
    NU = T + GAP
    XW = NU + 6
    ys = [k.sb([128, NU], F32, "yD") for _ in range(4)]
    xp = k.sb([128, XW], F32, "xpD")
    xcv = k.sb([128, NU], F32, "xcvD")
    ra = k.sb([128, NU], F32, "raD")
    gx = k.sb([128, NU], F32, "gxD")
    sq = k.sb([128, NU], F32, "sqD")
    hd = [k.sb([128, NU], F32, "hD") for _ in range(2)]
    gt = k.sb([128, NU], F32, "gtD")
    gt2 = k.sb([128, NU], F32, "gt2D")
    bd_a = k.sb([128, 128], F32, "bdA")
    bd_x = k.sb([128, 128], F32, "bdX")
    negsp = k.sb([128, 8], F32, "negsp")
    one_c = k.sb([128, 1], F32, "onec")
    pss = [k.ps([128, 512], F32, "psD") for _ in range(4)]
    k.op("pool", lambda e: e.memset(one_c.ap, 1.0), w=[one_c])
    k.op("pool", lambda e: e.memset(xp.ap, 0.0), w=[xp])
    k.op("pool", lambda e: e.memset(gt.ap, 0.0), w=[gt])
    for hh in hd:
        k.op("pool", lambda e: e.memset(hh.ap, 0.0), w=[hh])
    k.op("pool", lambda e: e.memset(bd_a.ap, 0.0), w=[bd_a])
    k.op("pool", lambda e: e.memset(bd_x.ap, 0.0), w=[bd_x])
    lo, _ = PP_COLS[f"lru_lam{l}"]
    k.act(negsp.ap, G.ppt.ap[:, lo:lo + 8], AF.Exp, r=[G.ppt], w=[negsp], scale=-1.0)
    k.act(negsp.ap, negsp.ap, AF.Ln, r=[negsp, one_c], w=[negsp], bias=one_c.ap)
    k.ts("dve", negsp.ap, negsp.ap, -8.0, ALU.mult, r=[negsp], w=[negsp])
    wo, _ = PP_COLS[f"lru_conv_w{l}"]
    uchunks = tok_chunks(0, NU, 512)
    pi = 0
    for c in range(4):
        load_gapped(k, G, xp, 3, 9 + CTX, G.UF, 3072 + c * 128, q="sp")
        load_gapped(k, G, gt, 0, CTX + GAP, G.UF, 2560 + c * 128, q="act")
        for d in range(2):
            col = d * 4 + c
            sh = 0 if d == 0 else 3
            wcol = lambda kk: G.ppt.ap[:, wo + col * 4 + kk:wo + col * 4 + kk + 1]
            k.ts("dve", xcv.ap, xp.ap[:, sh:sh + NU], wcol(0), ALU.mult, ppcol(G, f"lru_conv_b{l}", col), ALU.add,
                 r=[xp, G.ppt], w=[xcv])
            for kk in range(1, 4):
                k.stt(xcv.ap, xp.ap[:, sh + kk:sh + kk + NU], wcol(kk), xcv.ap, ALU.mult, ALU.add,
                      r=[xp, xcv, G.ppt], w=[xcv])
            for (bd, wsrc) in ((bd_a, G.lru_wa), (bd_x, G.lru_wx)):
                k.dma("sp", bd.ap[0:64, 0:64], wsrc.ap[l, d, 2 * c], r=[wsrc], w=[bd])
                k.dma("sp", bd.ap[64:128, 64:128], wsrc.ap[l, d, 2 * c + 1], r=[wsrc], w=[bd])
            for (u0, n) in uchunks:
                pa = pss[pi % 4]
                px = pss[(pi + 1) % 4]
                pi += 2
                k.mm(pa.ap[:, :n], bd_a.ap, xcv.ap[:, u0:u0 + n], start=True, stop=True, r=[bd_a, xcv], w=[pa])
                k.mm(px.ap[:, :n], bd_x.ap, xcv.ap[:, u0:u0 + n], start=True, stop=True, r=[bd_x, xcv], w=[px])
                k.act(ra.ap[:, u0:u0 + n], pa.ap[:, :n], AF.Sigmoid, r=[pa, G.ppt], w=[ra],
                      bias=ppcol(G, f"lru_ba{l}", col))
                k.act(gx.ap[:, u0:u0 + n], px.ap[:, :n], AF.Sigmoid, r=[px, G.ppt], w=[gx],
                      bias=ppcol(G, f"lru_bx{l}", col))
            k.act(ra.ap, ra.ap, AF.Exp, r=[ra, negsp], w=[ra], scale=negsp.ap[:, col:col + 1])
            k.tt("pool", gx.ap, gx.ap, xcv.ap, ALU.mult, r=[gx, xcv], w=[gx])
            k.tt("dve", sq.ap, ra.ap, ra.ap, ALU.mult, r=[ra], w=[sq])
            k.act(sq.ap, sq.ap, AF.Sqrt, r=[sq, one_c], w=[sq], scale=-1.0, bias=one_c.ap)
            k.tt("dve", gx.ap, gx.ap, sq.ap, ALU.mult, r=[gx, sq], w=[gx])
            h = hd[d]
            if d == 0:
                k.op("dve", lambda e: e.tensor_tensor_scan(out=h.ap[:, 0:CTX], data0=ra.ap[:, 0:CTX],
                                                           data1=gx.ap[:, 0:CTX], initial=0.0,
                                                           op0=ALU.mult, op1=ALU.add), r=[ra, gx], w=[h])
                k.op("dve", lambda e: e.tensor_tensor_scan(out=h.ap[:, CTX + GAP:NU], data0=ra.ap[:, CTX + GAP:NU],
                                                           data1=gx.ap[:, CTX + GAP:NU],
                                                           initial=h.ap[:, CTX - 1:CTX],
                                                           op0=ALU.mult, op1=ALU.add), r=[ra, gx, h], w=[h])
            else:
                k.op("dve", lambda e: e.tensor_tensor_scan(out=h.ap[:, CTX - 1::-1], data0=ra.ap[:, CTX - 1::-1],
                                                           data1=gx.ap[:, CTX - 1::-1], initial=0.0,
                                                           op0=ALU.mult, op1=ALU.add), r=[ra, gx], w=[h])
                lo_ = CTX + GAP - 1
                k.op("dve", lambda e: e.tensor_tensor_scan(out=h.ap[:, NU - 1:lo_:-1], data0=ra.ap[:, NU - 1:lo_:-1],
                                                           data1=gx.ap[:, NU - 1:lo_:-1],
                                                           initial=h.ap[:, 0:1],
                                                           op0=ALU.mult, op1=ALU.add), r=[ra, gx, h], w=[h])
        y = ys[c]
        k.act(gt2.ap, gt.ap, AF.Square, r=[gt], w=[gt2])
        k.ts("dve", gt2.ap, gt2.ap, 0.044715, ALU.mult, 1.0, ALU.add, r=[gt2], w=[gt2])
        k.tt("dve", gt2.ap, gt2.ap, gt.ap, ALU.mult, r=[gt2, gt], w=[gt2])
        k.act(gt2.ap, gt2.ap, AF.Sigmoid, r=[gt2], w=[gt2], scale=1.5957691216057308)
        k.tt("dve", gt2.ap, gt2.ap, gt.ap, ALU.mult, r=[gt2, gt], w=[gt2])
        k.tt("pool", y.ap[:, 0:CTX], hd[0].ap[:, 0:CTX], hd[1].ap[:, 0:CTX], ALU.add, r=[hd[0], hd[1]], w=[y])
        k.tt("pool", y.ap[:, CTX:NU], hd[0].ap[:, CTX:NU], hd[1].ap[:, CTX:NU], ALU.add, r=[hd[0], hd[1]], w=[y])
        k.tt("dve", y.ap, y.ap, gt2.ap, ALU.mult, r=[y, gt2], w=[y])
    finish_norm(k, G, ys, GAP, "rms", l, 1536, "norm_d_g")
    k.release(m)


def mixer_attn(k, G, l, need_ctx):
    m = k.mark()
    lam_init = 0.8 - 0.6 * math.exp(-0.3 * l)
    dl = k.sb([128, 256], F32, "dlam")
    k.dma("sp", dl.ap, G.diff_lambda.ap[l:l + 1, :].to_broadcast([128, 256]), r=[G.diff_lambda], w=[dl])
    pr = k.sb([128, 128], F32, "dlpr")
    s2 = k.sb([128, 2], F32, "dls")
    dlv = dl.ap.rearrange("p (a b) -> p a b", a=4)
    k.tt("dve", pr.ap[:, 0:64], dlv[:, 0, :], dlv[:, 1, :], ALU.mult, r=[dl], w=[pr])
    k.tt("dve", pr.ap[:, 64:128], dlv[:, 2, :], dlv[:, 3, :], ALU.mult, r=[dl], w=[pr])
    k.op("dve", lambda e: e.reduce_sum(out=s2.ap, in_=pr.ap.rearrange("p (a b) -> p a b", a=2), axis=AX.X),
         r=[pr], w=[s2])
    k.act(s2.ap, s2.ap, AF.Exp, r=[s2], w=[s2])
    neg_lam = k.sb([128, 1], F32, "neglam")
    k.tt("dve", neg_lam.ap, s2.ap[:, 1:2], s2.ap[:, 0:1], ALU.subtract, r=[s2], w=[neg_lam])
    k.ts("dve", neg_lam.ap, neg_lam.ap, -lam_init, ALU.add, r=[neg_lam], w=[neg_lam])
    gsc = k.sb([128, 1], F32, "gsc")
    k.ts("dve", gsc.ap, ppcol(G, f"diff_norm_g{l}"), 1.0 - lam_init, ALU.mult, r=[G.ppt], w=[gsc])
    ones_b = k.sb([128, 128], BF16, "onesb")
    k.copy("dve", ones_b.ap, G.ones_f.ap, r=[G.ones_f], w=[ones_b])
    vt = k.sb([128, NT, 512], BF16, "vtm")
    k.dma("sp", vt.ap, G.V.ap.rearrange("(t p) e -> p t e", p=128), r=[G.V], w=[vt])
    qT = [k.sb([64, T], BF16, "qT") for _ in range(2)]
    kT = [k.sb([64, T], BF16, "kT") for _ in range(2)]
    pts = [k.sb([128, 512], BF16, "pexp") for _ in range(3)]
    ps_s = [k.ps([128, 512], F32, "ps_s") for _ in range(2)]
    ps_acc = [k.ps([128, 512], F32, "ps_acc") for _ in range(2)]
    ps_den = [k.ps([128, 512], F32, "ps_den") for _ in range(2)]
    ps_n = k.ps([128, 512], F32, "ps_n")
    rden = [k.sb([128, 512], F32, "rden") for _ in range(2)]
    tnum = [k.sb([128, 512], F32, "tnum") for _ in range(2)]
    osb = k.sb([128, 512], F32, "osb")
    osq = k.sb([128, 512], F32, "osq")
    rs = k.sb([128, 512], F32, "rsb")
    stage = [k.sb([128, T], BF16, "ystB") for _ in range(2)]
    si = 0
    pi = 0
    for h in range(4):
        for mi in range(2):
            j = 2 * h + mi
            k.dma("sp", qT[mi].ap, G.QK.ap[j * 64:(j + 1) * 64, :], r=[G.QK], w=[qT[mi]])
            k.dma("act", kT[mi].ap, G.QK.ap[512 + j * 64:512 + (j + 1) * 64, :], r=[G.QK], w=[kT[mi]])
        st = stage[h % 2]
        qchunks = [(t0, n, NT) for (t0, n) in tok_chunks(CTX, T, 512)]
        if need_ctx:
            qchunks = [(0, CTX, 2)] + qchunks
        for (t0, n, nkt) in qchunks:
            for mi in range(2):
                for kt in range(nkt):
                    pss = ps_s[si % 2]
                    si += 1
                    k.mm(pss.ap[:, :n], kT[mi].ap[:, kt * 128:(kt + 1) * 128], qT[mi].ap[:, t0:t0 + n],
                         start=True, stop=True, r=[kT[mi], qT[mi]], w=[pss])
                    pt = pts[pi % 3]
                    pi += 1
                    k.act(pt.ap[:, :n], pss.ap[:, :n], AF.Exp, r=[pss], w=[pt], scale=0.125)
                    k.mm(ps_acc[mi].ap[:, :n], vt.ap[:, kt, h * 128:(h + 1) * 128], pt.ap[:, :n],
                         start=(kt == 0), stop=(kt == nkt - 1), r=[vt, pt], w=[ps_acc[mi]])
                    k.mm(ps_den[mi].ap[:, :n], ones_b.ap, pt.ap[:, :n],
                         start=(kt == 0), stop=(kt == nkt - 1), r=[ones_b, pt], w=[ps_den[mi]])
            for mi in range(2):
                k.op("dve", lambda e: e.reciprocal(out=rden[mi].ap[:, :n], in_=ps_den[mi].ap[:, :n]),
                     r=[ps_den[mi]], w=[rden[mi]])
                k.tt("dve", tnum[mi].ap[:, :n], ps_acc[mi].ap[:, :n], rden[mi].ap[:, :n], ALU.mult,
                     r=[ps_acc[mi], rden[mi]], w=[tnum[mi]])
            k.stt(osb.ap[:, :n], tnum[1].ap[:, :n], neg_lam.ap, tnum[0].ap[:, :n], ALU.mult, ALU.add,
                  r=[tnum[0], tnum[1], neg_lam], w=[osb])
            k.act(osq.ap[:, :n], osb.ap[:, :n], AF.Square, r=[osb], w=[osq])
            k.mm(ps_n.ap[:, :n], G.ones_f.ap, osq.ap[:, :n], start=True, stop=True, r=[G.ones_f, osq], w=[ps_n])
            k.act(rs.ap[:, :n], ps_n.ap[:, :n], AF.Sqrt, r=[ps_n, G.eps], w=[rs], scale=1.0 / 128, bias=G.eps.ap)
            k.op("dve", lambda e: e.reciprocal(out=rs.ap[:, :n], in_=rs.ap[:, :n]), r=[rs], w=[rs])
            k.stt(st.ap[:, t0:t0 + n], osb.ap[:, :n], gsc.ap, rs.ap[:, :n], ALU.mult, ALU.mult,
                  r=[osb, gsc, rs], w=[st])
        if need_ctx:
            k.dma("sp", G.YC.ap[512 + h * 128:512 + (h + 1) * 128, :], st.ap, r=[st], w=[G.YC])
        else:
            k.dma("sp", G.YC.ap[512 + h * 128:512 + (h + 1) * 128, CTX:T], st.ap[:, CTX:T], r=[st], w=[G.YC])
    k.release(m)


MIXSEL = "bacd"


def phase_mixers(k, G, l, need_ctx):
    if "b" in MIXSEL:
        mixer_attn(k, G, l, need_ctx)
    if "a" in MIXSEL:
        mixer_conformer(k, G, l)
    if "c" in MIXSEL:
        mixer_sconv(k, G, l)
    if "d" in MIXSEL:
        mixer_lru(k, G, l)


def phase_outproj(k, G, l, need_ctx):
    m = k.mark()
    wo = k.sb([128, KC, D], BF16, "wout")
    wsrc = G.WOB[l].ap.rearrange("p (kc n) -> p kc n", kc=KC)
    for j in range(4):
        k.dma("act", wo.ap[:, j * 4:(j + 1) * 4, :], wsrc[:, j * 4:(j + 1) * 4, :], r=[G.WOB[l]], w=[wo])
    rw = k.sb([128, KC, NE], BF16, "rw")
    k.dma("act", rw.ap, G.RWB[l].ap.rearrange("p (kc e) -> p kc e", kc=KC), r=[G.RWB[l]], w=[rw])
    rb = k.sb([128, NE], F32, "rb")
    k.dma("sp", rb.ap, G.router_b.ap[l:l + 1, :].to_broadcast([128, NE]), r=[G.router_b], w=[rb])
    g1 = k.sb([128, D], F32, "g1b")
    gs2 = k.sb([128, D], F32, "gs2b")
    sh2 = k.sb([128, D], F32, "sh2b")
    ycs = [k.sb([128, KC, 512], BF16, "ycblk") for _ in range(2)]
    xts = [k.sb([128, D], F32, "xt") for _ in range(2)]
    tmp = k.sb([128, D], F32, "tmp")
    scr = k.sb([128, D], BF16, "scr")
    hbs = [k.sb([128, D], BF16, "hb") for _ in range(2)]
    h2s = [k.sb([128, KC, 128], BF16, "h2s") for _ in range(2)]
    ssq = k.sb([128, 1], F32, "ssq")
    rstd = k.sb([128, 1], F32, "rstd")
    lg = k.sb([128, NE], F32, "lg")
    ex = k.sb([128, NE], F32, "ex")
    msk = k.sb([128, NE], F32, "msk")
    top8 = k.sb([128, 8], F32, "top8")
    nmx = k.sb([128, 1], F32, "nmx")
    ssum = k.sb([128, 1], F32, "ssum")
    cmb = [k.sb([128, NE], F32, "cmb") for _ in range(2)]
    ps_y = [k.ps([128, 512], F32, "psy") for _ in range(4)]
    pts = [k.ps([128, 1024], BF16, "pt") for _ in range(2)]
    ps_l = k.ps([128, NE], F32, "psl")
    ps_r = k.ps([128, 2 * NE], F32, "psr")
    rks = [k.sb([128, NE], F32, "rk") for _ in range(2)]
    lgs_ = [k.sb([128, NE], F32, "lgc") for _ in range(2)]
    t8s = [k.sb([128, 8], F32, "t8c") for _ in range(2)]
    cnt = k.sb([128, NE], F32, "cnt")
    k.op("dve", lambda e: e.memset(cnt.ap, 0.0), w=[cnt])
    lto, _ = PP_COLS["ltri"]
    ltri = G.ppt.ap[:, lto:lto + 128]
    mv = G.modv[l]
    tiles = list(range(NT)) if need_ctx else list(range(2, NT))
    cur_which = None
    cur_blk = None
    nblk = 0
    for i in tiles:
        which = 1 if i < 2 else 0
        if which != cur_which:
            for (t, sec) in ((g1, 2), (gs2, 3), (sh2, 4)):
                k.dma("sp", t.ap, mv.ap[which:which + 1, sec, :].to_broadcast([128, D]), r=[mv], w=[t])
            cur_which = which
        blk = i // 4
        if blk != cur_blk:
            yc = ycs[nblk % 2]
            nblk += 1
            nt_ = min(512, T - blk * 512)
            for kc in range(KC):
                k.dma("sp" if kc % 2 == 0 else "act", yc.ap[:, kc, :nt_],
                      G.YC.ap[kc * 128:(kc + 1) * 128, blk * 512:blk * 512 + nt_], r=[G.YC], w=[yc])
            cur_blk = blk
        xt = xts[i % 2]
        hb = hbs[i % 2]
        h2 = h2s[i % 2]
        cb_ = cmb[i % 2]
        k.dma("sp", xt.ap, G.xres.ap[i * 128:(i + 1) * 128, :], r=[G.xres], w=[xt])
        off = (i % 4) * 128
        for dblk in range(4):
            ps = ps_y[dblk]
            for kc in range(KC):
                k.mm(ps.ap, yc.ap[:, kc, off:off + 128], wo.ap[:, kc, dblk * 512:(dblk + 1) * 512],
                     start=(kc == 0), stop=(kc == KC - 1), r=[yc, wo], w=[ps])
            sl = slice(dblk * 512, (dblk + 1) * 512)
            k.tt("dve", tmp.ap[:, sl], ps.ap, g1.ap[:, sl], ALU.mult, r=[ps, g1], w=[tmp])
            k.tt("pool", xt.ap[:, sl], xt.ap[:, sl], tmp.ap[:, sl], ALU.add, r=[xt, tmp], w=[xt])
        k.dma("sp", G.xres.ap[i * 128:(i + 1) * 128, :], xt.ap, r=[xt], w=[G.xres])
        norm_mod_tile(k, G, xt, gs2, sh2, hb, scr, ssq, rstd, tmp)
        for half in range(2):
            pt = pts[half]
            for j in range(8):
                kc = half * 8 + j
                k.op("pe", lambda e: e.transpose(out=pt.ap[:, j * 128:(j + 1) * 128],
                                                 in_=hb.ap[:, kc * 128:(kc + 1) * 128], identity=G.ident_b.ap),
                     r=[hb, G.ident_b], w=[pt])
            k.copy("act" if half == 0 else "dve", h2.ap[:, half * 8:(half + 1) * 8, :],
                   pt.ap.rearrange("p (a b) -> p a b", a=8), r=[pt], w=[h2])
        k.dma("sp", G.H2TM.ap[i * 128:(i + 1) * 128, :], hb.ap, r=[hb], w=[G.H2TM])
        for kc in range(KC):
            k.mm(ps_l.ap, h2.ap[:, kc, :], rw.ap[:, kc, :], start=(kc == 0), stop=(kc == KC - 1),
                 r=[h2, rw], w=[ps_l])
        k.tt("dve", lg.ap, ps_l.ap, rb.ap, ALU.add, r=[ps_l, rb], w=[lg])
        k.op("dve", lambda e: e.max(out=top8.ap, in_=lg.ap), r=[lg], w=[top8])
        k.ts("dve", msk.ap, lg.ap, top8.ap[:, 3:4], ALU.is_ge, r=[lg, top8], w=[msk])
        k.ts("dve", nmx.ap, top8.ap[:, 0:1], -1.0, ALU.mult, r=[top8], w=[nmx])
        k.act(ex.ap, lg.ap, AF.Exp, r=[lg, nmx], w=[ex], bias=nmx.ap)
        k.tt("dve", ex.ap, ex.ap, msk.ap, ALU.mult, r=[ex, msk], w=[ex])
        k.op("dve", lambda e: e.reduce_sum(out=ssum.ap, in_=ex.ap, axis=AX.X), r=[ex], w=[ssum])
        k.op("dve", lambda e: e.reciprocal(out=ssum.ap, in_=ssum.ap), r=[ssum], w=[ssum])
        k.ts("dve", cb_.ap, ex.ap, ssum.ap, ALU.mult, r=[ex, ssum], w=[cb_])
        k.dma("sp", G.COMB.ap[i * 128:(i + 1) * 128, :], cb_.ap, r=[cb_], w=[G.COMB])
        rk, lgc, t8c = rks[i % 2], lgs_[i % 2], t8s[i % 2]
        k.mm(ps_r.ap[:, 0:NE], ltri, msk.ap, start=True, stop=True, r=[G.ppt, msk], w=[ps_r])
        k.mm(ps_r.ap[:, NE:2 * NE], G.ones_f.ap, msk.ap, start=True, stop=True, r=[G.ones_f, msk], w=[ps_r])
        k.tt("dve", rk.ap, ps_r.ap[:, 0:NE], cnt.ap, ALU.add, r=[ps_r, cnt], w=[rk])
        k.tt("dve", cnt.ap, ps_r.ap[:, NE:2 * NE], cnt.ap, ALU.add, r=[ps_r, cnt], w=[cnt])
        k.copy("dve", lgc.ap, lg.ap, r=[lg], w=[lgc])
        k.copy("dve", t8c.ap, top8.ap, r=[top8], w=[t8c])
        k.dma("sp", G.RANKD.ap[i * 128:(i + 1) * 128, :], rk.ap, r=[rk], w=[G.RANKD])
        k.dma("sp", G.LGD.ap[i * 128:(i + 1) * 128, :], lgc.ap, r=[lgc], w=[G.LGD])
        k.dma("sp", G.TOPD.ap[i * 128:(i + 1) * 128, :], t8c.ap, r=[t8c], w=[G.TOPD])
    k.dma("sp", G.CNTD.ap, cnt.ap, r=[cnt], w=[G.CNTD])
    k.release(m)


class WStream:
    def __init__(self, k, bufs, srcs, q="pool", hold=1):
        self.k, self.bufs, self.srcs, self.q, self.hold = k, bufs, srcs, q, hold
        self.nxt = 0

    def get(self, n):
        nb = len(self.bufs)
        while self.nxt < len(self.srcs) and self.nxt <= n + nb - self.hold:
            j = self.nxt
            out_fn, in_ap, srcbuf = self.srcs[j]
            b = self.bufs[j % nb]
            self.k.dma(self.q, out_fn(b), in_ap, r=[srcbuf], w=[b])
            self.nxt += 1
        return self.bufs[n % nb]


def precast_outproj(k, G, l):
    wov = G.w_out.ap[l].rearrange("(kc p) n -> p kc n", p=128)
    dst = G.WOB[l].ap.rearrange("p (kc n) -> p kc n", kc=KC)
    for j in range(4):
        k.dma("pool", dst[:, :, j * 512:(j + 1) * 512], wov[:, :, j * 512:(j + 1) * 512], r=[G.w_out], w=[G.WOB[l]])
    k.dma("pool", G.RWB[l].ap.rearrange("p (kc e) -> p kc e", kc=KC),
          G.router_w.ap[l].rearrange("(kc p) e -> p kc e", p=128), r=[G.router_w], w=[G.RWB[l]])


def precast_list(k, G, l):
    w1v = G.exp_w1.ap[l].rearrange("e (kc p) n -> e p kc n", p=128)
    w2v = G.exp_w2.ap[l].rearrange("e (fc p) n -> e p fc n", p=128)
    out = []
    for e in range(NE):
        for cbk in range(4):
            out.append(lambda e=e, cbk=cbk: k.dma(
                "pool", G.W1B[l][cbk].ap[e * 128:(e + 1) * 128, :].rearrange("p (kc c) -> p kc c", kc=KC),
                w1v[e][:, :, cbk * 512:(cbk + 1) * 512], r=[G.exp_w1], w=[]))
        for hf in range(2):
            out.append(lambda e=e, hf=hf: k.dma(
                "pool", G.W2B[l][hf].ap[e * 128:(e + 1) * 128, :].rearrange("p (fc c) -> p fc c", fc=4),
                w2v[e][:, hf * 4:(hf + 1) * 4, :], r=[G.exp_w2], w=[]))
    return out


def pump(G, n):
    for _ in range(n):
        if G.bg:
            G.bg.pop(0)()


def phase_moe(k, G, l, need_ctx, last):
    m0 = k.mark()
    tiles = list(range(NT)) if need_ctx else list(range(2, NT))
    tb, ntt = tiles[0], len(tiles)
    NTL = ntt * 4 + NE
    NSL = NTL * 128
    tsl = slice(tb * 128, (tb + ntt) * 128)

    def tload(src, width, name):
        t = k.sb([128, ntt, width], F32, name)
        k.dma("sp", t.ap, src.ap[tsl, :].rearrange("(t p) e -> p t e", p=128), r=[src], w=[t])
        return t

    lgs = tload(G.LGD, NE, "lgs")
    cmbs = tload(G.COMB, NE, "cmbs")
    rks = tload(G.RANKD, NE, "rks")
    tops = tload(G.TOPD, 8, "tops")
    cnt = k.sb([128, NE], F32, "cntm")
    k.dma("sp", cnt.ap, G.CNTD.ap, r=[G.CNTD], w=[cnt])
    io_, _ = PP_COLS["iota"]
    iota = G.ppt.ap[:, io_:io_ + NTL]
    pidx = ppcol(G, "pidx")
    ntile = k.sb([128, NE], F32, "ntile")
    ones32 = k.sb([128, NE], F32, "ones32")
    incl = k.sb([128, NE], F32, "incl")
    base = k.sb([128, NE], F32, "base")
    k.op("dve", lambda e: e.memset(ntile.ap, 0.0), w=[ntile])
    k.op("dve", lambda e: e.memset(ones32.ap, 1.0), w=[ones32])
    for mth in range(ntt):
        k.stt(ntile.ap, cnt.ap, 128.0 * mth, ntile.ap, ALU.is_gt, ALU.add, r=[cnt, ntile], w=[ntile])
    k.op("dve", lambda e: e.tensor_tensor_scan(out=incl.ap, data0=ones32.ap, data1=ntile.ap, initial=0.0,
                                               op0=ALU.mult, op1=ALU.add), r=[ones32, ntile], w=[incl])
    k.tt("dve", base.ap, incl.ap, ntile.ap, ALU.subtract, r=[incl, ntile], w=[base])
    k.ts("dve", base.ap, base.ap, 128.0, ALU.mult, r=[base], w=[base])
    posf = k.sb([128, ntt, 4], F32, "posf")
    gat = k.sb([128, ntt, 4], F32, "gat")
    posi = k.sb([128, ntt, 4], I32, "posi")
    pf = k.sb([128, NE], F32, "pf")
    ohs = [k.sb([128, NE], F32, "oh") for _ in range(2)]
    sc1 = [k.sb([128, NE], F32, "sc1") for _ in range(2)]
    sc2 = [k.sb([128, NE], F32, "sc2") for _ in range(2)]
    n = 0
    for ti in range(ntt):
        k.tt("dve", pf.ap, rks.ap[:, ti, :], base.ap, ALU.add, r=[rks, base], w=[pf])
        for kk in range(4):
            oh, s1_, s2_ = ohs[n % 2], sc1[n % 2], sc2[n % 2]
            n += 1
            k.ts("dve", oh.ap, lgs.ap[:, ti, :], tops.ap[:, ti, kk:kk + 1], ALU.is_equal, r=[lgs, tops], w=[oh])
            k.tt("dve", s1_.ap, oh.ap, pf.ap, ALU.mult, r=[oh, pf], w=[s1_])
            k.op("dve", lambda e: e.reduce_sum(out=posf.ap[:, ti, kk:kk + 1], in_=s1_.ap, axis=AX.X),
                 r=[s1_], w=[posf])
            k.tt("dve", s2_.ap, oh.ap, cmbs.ap[:, ti, :], ALU.mult, r=[oh, cmbs], w=[s2_])
            k.op("dve", lambda e: e.reduce_sum(out=gat.ap[:, ti, kk:kk + 1], in_=s2_.ap, axis=AX.X),
                 r=[s2_], w=[gat])
    k.copy("dve", posi.ap, posf.ap, r=[posf], w=[posi])
    texp = k.sb([128, NTLMAX], F32, "texp")
    widf = k.sb([128, NTLMAX], F32, "widf")
    bidf = k.sb([128, NTLMAX], F32, "bidf")
    eq = k.sb([128, NTLMAX], F32, "eqt")
    widx = k.sb([128, NTLMAX], I32, "widx")
    bidx = k.sb([128, NTLMAX], I32, "bidx")
    k.op("dve", lambda e: e.memset(texp.ap, 0.0), w=[texp])
    k.op("dve", lambda e: e.memset(eq.ap, 0.0), w=[eq])
    for e_ in range(NE):
        k.stt(texp.ap[:, :NTL], iota, incl.ap[:, e_:e_ + 1], texp.ap[:, :NTL], ALU.is_ge, ALU.add,
              r=[G.ppt, incl, texp], w=[texp])
    k.ts("dve", widf.ap, texp.ap, 128.0, ALU.mult, pidx, ALU.add, r=[texp, G.ppt], w=[widf])
    k.tt("dve", eq.ap[:, 1:NTL], texp.ap[:, 1:NTL], texp.ap[:, 0:NTL - 1], ALU.is_equal, r=[texp], w=[eq])
    k.stt(widf.ap, eq.ap, BIG, widf.ap, ALU.mult, ALU.add, r=[eq, widf], w=[widf])
    k.copy("dve", widx.ap, widf.ap, r=[widf], w=[widx])
    k.ts("dve", bidf.ap, texp.ap, float(NE - 1), ALU.min, 128.0, ALU.mult, r=[texp], w=[bidf])
    k.ts("dve", bidf.ap, bidf.ap, pidx, ALU.add, r=[bidf, G.ppt], w=[bidf])
    k.copy("dve", bidx.ap, bidf.ap, r=[bidf], w=[bidx])
    ms = k.mark()
    xsc = [k.sb([128, D], BF16, "xsc") for _ in range(3)]
    for ti, i in enumerate(tiles):
        xt = xsc[ti % 3]
        k.dma("sp", xt.ap, G.H2TM.ap[i * 128:(i + 1) * 128, :], r=[G.H2TM], w=[xt])
        for kk in range(4):
            k.idma(out=G.XS.ap, in_=xt.ap, out_off=posi.ap[:, ti, kk:kk + 1], bounds=NSL - 1,
                   r=[xt, posi], w=[G.XS])
    k.release(ms)
    me = k.mark()
    w1 = [k.sb([128, KC, 512], BF16, "w1res") for _ in range(4)]
    w2 = [k.sb([128, 4, D], BF16, "w2res") for _ in range(2)]
    b1s = [k.sb([128, 16], F32, "b1s") for _ in range(2)]
    xs = [k.sb([128, D], BF16, "xs") for _ in range(3)]
    h2s = [k.sb([128, KC, 128], BF16, "h2s") for _ in range(2)]
    ats = [k.sb([128, 8, 128], BF16, "actT") for _ in range(2)]
    yts = [k.sb([128, D], F32, "yt") for _ in range(2)]
    gt = [k.sb([128, 128], F32, "g") for _ in range(2)]
    sg = [k.sb([128, 128], F32, "sig") for _ in range(2)]
    u1 = [k.sb([128, 128], F32, "u1") for _ in range(2)]
    pts = [k.ps([128, 1024], BF16, "pt") for _ in range(2)]
    ps_gu = [k.ps([128, 512], F32, "psgu") for _ in range(2)]
    ps_o = [k.ps([128, 512], F32, "pso") for _ in range(4)]
    NROW = NE * 128

    def load_x(j):
        xt = xs[j % 3]
        k.dma("sp", xt.ap, G.XS.ap[j * 128:(j + 1) * 128, :], r=[G.XS], w=[xt])

    def transp(j):
        xt, h2 = xs[j % 3], h2s[j % 2]
        for half in range(2):
            pt = pts[half]
            for jj in range(8):
                kc = half * 8 + jj
                k.op("pe", lambda e: e.transpose(out=pt.ap[:, jj * 128:(jj + 1) * 128],
                                                 in_=xt.ap[:, kc * 128:(kc + 1) * 128], identity=G.ident_b.ap),
                     r=[xt, G.ident_b], w=[pt])
            k.copy("act" if half == 0 else "dve", h2.ap[:, half * 8:(half + 1) * 8, :],
                   pt.ap.rearrange("p (a b) -> p a b", a=8), r=[pt], w=[h2])

    def gather_w1(j):
        for cbk in range(4):
            k.idma(out=w1[cbk].ap.rearrange("p kc c -> p (kc c)"), in_=G.W1B[l][cbk].ap, in_off=widx.ap[:, j:j + 1],
                   bounds=NROW - 1, r=[G.W1B[l][cbk], widx], w=[w1[cbk]])

    def gather_w2(j):
        for hf in range(2):
            k.idma(out=w2[hf].ap.rearrange("p fc c -> p (fc c)"), in_=G.W2B[l][hf].ap, in_off=widx.ap[:, j:j + 1],
                   bounds=NROW - 1, r=[G.W2B[l][hf], widx], w=[w2[hf]])

    def gather_b(j):
        bb = b1s[j % 2]
        k.idma(out=bb.ap, in_=G.b1t[l].ap, in_off=bidx.ap[:, j:j + 1], bounds=NROW - 1, r=[G.b1t[l], bidx], w=[bb])
        k.ts("dve", bb.ap[:, 8:16], bb.ap[:, 8:16], 1.0, ALU.add, r=[bb], w=[bb])

    fi = [0]

    def first(j):
        at, h2, bb = ats[j % 2], h2s[j % 2], b1s[j % 2]
        for cbk in range(4):
            wb = w1[cbk]
            for s in range(2):
                fc = cbk * 2 + s
                pgu = ps_gu[fi[0] % 2]
                g, sig, uu = gt[fi[0] % 2], sg[fi[0] % 2], u1[fi[0] % 2]
                fi[0] += 1
                wsl = wb.ap[:, :, s * 256:(s + 1) * 256].rearrange("p kc (f two) -> p kc f two", two=2)
                for kc in range(KC):
                    k.mm(pgu.ap[:, 0:128], wsl[:, kc, :, 0], h2.ap[:, kc, :], start=(kc == 0),
                         stop=(kc == KC - 1), r=[wb, h2], w=[pgu])
                for kc in range(KC):
                    k.mm(pgu.ap[:, 128:256], wsl[:, kc, :, 1], h2.ap[:, kc, :], start=(kc == 0),
                         stop=(kc == KC - 1), r=[wb, h2], w=[pgu])
                k.ts("dve", g.ap, pgu.ap[:, 0:128], bb.ap[:, fc:fc + 1], ALU.add, 7.0, ALU.min, r=[pgu, bb], w=[g])
                k.act(sig.ap, g.ap, AF.Sigmoid, r=[g], w=[sig], scale=1.702)
                k.ts("dve", uu.ap, pgu.ap[:, 128:256], bb.ap[:, 8 + fc:9 + fc], ALU.add, -6.0, ALU.max,
                     r=[pgu, bb], w=[uu])
                k.tt("dve", g.ap, g.ap, sig.ap, ALU.mult, r=[g, sig], w=[g])
                k.stt(at.ap[:, fc, :], uu.ap, 8.0, g.ap, ALU.min, ALU.mult, r=[uu, g], w=[at])

    oi = [0]

    def second(j):
        at, yt = ats[j % 2], yts[j % 2]
        for dblk in range(4):
            po = ps_o[oi[0] % 4]
            oi[0] += 1
            for fc in range(8):
                wb = w2[fc // 4]
                k.mm(po.ap, at.ap[:, fc, :], wb.ap[:, fc % 4, dblk * 512:(dblk + 1) * 512], start=(fc == 0),
                     stop=(fc == 7), r=[at, wb], w=[po])
            k.copy("act" if dblk % 2 == 0 else "dve", yt.ap[:, dblk * 512:(dblk + 1) * 512], po.ap, r=[po], w=[yt])
        k.dma("sp", G.YS.ap[j * 128:(j + 1) * 128, :], yt.ap, r=[yt], w=[G.YS])

    load_x(0)
    load_x(1)
    gather_w1(0)
    gather_w2(0)
    gather_b(0)
    transp(0)
    for j in range(NTL):
        if j + 2 < NTL:
            load_x(j + 2)
        if j + 1 < NTL:
            gather_b(j + 1)
            transp(j + 1)
        first(j)
        if j + 1 < NTL:
            gather_w1(j + 1)
        if j > 0:
            second(j - 1)
            gather_w2(j)
        pump(G, 2)
    second(NTL - 1)
    pump(G, len(G.bg))
    k.release(me)
    b2 = k.sb([NE, D], F32, "b2")
    k.dma("sp", b2.ap, G.exp_b2.ap[l], r=[G.exp_b2], w=[b2])
    mv = G.modv[l]
    ygs = [k.sb([128, D], F32, "yg") for _ in range(8)]
    accs = [k.sb([128, D], F32, "acc") for _ in range(2)]
    xts = [k.sb([128, D], F32, "xt") for _ in range(2)]
    combT = k.sb([NE, 128], F32, "combT")
    ps_o = [k.ps([128, 512], F32, "pso") for _ in range(4)]
    g2 = [None, None]
    if last:
        fg = k.sb([128, D], F32, "fgb")
        k.dma("sp", fg.ap, G.final_g.ap.to_broadcast([128, D]), r=[G.final_g], w=[fg])
        scr = k.sb([128, D], BF16, "scr")
        ssq = k.sb([128, 1], F32, "ssq")
        rstd = k.sb([128, 1], F32, "rstd")
        ots = [k.sb([128, D], F32, "ot") for _ in range(2)]
    oi = 0
    for ti, i in enumerate(tiles):
        which = 1 if i < 2 else 0
        if g2[which] is None:
            g2[which] = k.sb([128, D], F32, "g2b")
            k.dma("sp", g2[which].ap, mv.ap[which:which + 1, 5, :].to_broadcast([128, D]), r=[mv], w=[g2[which]])
        yg = [ygs[(ti % 2) * 4 + kk] for kk in range(4)]
        for kk in range(4):
            k.idma(out=yg[kk].ap, in_=G.YS.ap, in_off=posi.ap[:, ti, kk:kk + 1], bounds=NSL - 1,
                   r=[G.YS, posi], w=[yg[kk]])
        xt = xts[ti % 2]
        acc = accs[ti % 2]
        k.dma("sp", xt.ap, G.xres.ap[i * 128:(i + 1) * 128, :], r=[G.xres], w=[xt])
        pT = ps_o[oi % 4]
        oi += 1
        k.op("pe", lambda e: e.transpose(out=pT.ap[:NE, :128], in_=cmbs.ap[:, ti, :], identity=G.ident_f.ap),
             r=[cmbs, G.ident_f], w=[pT])
        k.copy("dve", combT.ap, pT.ap[:NE, :128], r=[pT], w=[combT])
        for dblk in range(4):
            po = ps_o[oi % 4]
            oi += 1
            sl = slice(dblk * 512, (dblk + 1) * 512)
            k.mm(po.ap, combT.ap, b2.ap[:, sl], start=True, stop=True, r=[combT, b2], w=[po])
            k.stt(acc.ap[:, sl], yg[0].ap[:, sl], gat.ap[:, ti, 0:1], po.ap, ALU.mult, ALU.add,
                  r=[yg[0], gat, po], w=[acc])
        for kk in range(1, 4):
            k.stt(acc.ap, yg[kk].ap, gat.ap[:, ti, kk:kk + 1], acc.ap, ALU.mult, ALU.add,
                  r=[yg[kk], gat, acc], w=[acc])
        k.tt("dve", acc.ap, acc.ap, g2[which].ap, ALU.mult, r=[acc, g2[which]], w=[acc])
        k.tt("dve", xt.ap, xt.ap, acc.ap, ALU.add, r=[xt, acc], w=[xt])
        if not last:
            k.dma("sp", G.xres.ap[i * 128:(i + 1) * 128, :], xt.ap, r=[xt], w=[G.xres])
        else:
            ot = ots[ti % 2]
            k.act(scr.ap, xt.ap, AF.Square, r=[xt], w=[scr, ssq], accum_out=ssq.ap)
            k.act(rstd.ap, ssq.ap, AF.Sqrt, r=[ssq, G.eps], w=[rstd], scale=1.0 / D, bias=G.eps.ap)
            k.op("dve", lambda e: e.reciprocal(out=rstd.ap, in_=rstd.ap), r=[rstd], w=[rstd])
            k.stt(ot.ap, xt.ap, rstd.ap, fg.ap, ALU.mult, ALU.mult, r=[xt, rstd, fg], w=[ot])
            k.dma("sp", G.out.ap[(i - 2) * 128:(i - 1) * 128, :], ot.ap, r=[ot], w=[G.out])
    k.release(m0)


_CACHE = {}


def kernel(**inputs):
    n = 8
    if "nc" not in _CACHE:
        _CACHE["nc"] = build_program(upto="all")[0]
    nc = _CACHE["nc"]
    maps = make_in_maps(inputs, list(range(n)))
    res = run_bass_kernel_spmd(nc, maps, core_ids=list(range(n)))
    out = np.stack([np.asarray(r["out"], dtype=np.float32) for r in res.results], axis=0)
    return out
```

```python
import math
import numpy as np
import concourse.bass as bass
import concourse.mybir as mybir
from concourse.bass_utils import run_bass_kernel_spmd

F32 = mybir.dt.float32
BF16 = mybir.dt.bfloat16
I32 = mybir.dt.int32
AF = mybir.ActivationFunctionType
ALU = mybir.AluOpType
AX = mybir.AxisListType

D = 2048
SEQ = 2048
CTX = 256
T = SEQ + CTX
NT = T // 128
DEPTH = 2
GW = 512
N_IN = 5120
NE = 32
DFF = 1024
EPS = 1e-6
KC = D // 128
NTLMAX = NT * 4 + NE
BIG = 1.0e6


class Buf:
    __slots__ = ("ap", "w", "r", "name")

    def __init__(self, ap, name=""):
        self.ap = ap
        self.w = None
        self.r = {}
        self.name = name

    def __getitem__(self, idx):
        return self.ap[idx]


class KB:
    RING = 8

    def __init__(self, nc):
        self.nc = nc
        self.eng = {"pe": nc.tensor, "act": nc.scalar, "dve": nc.vector, "pool": nc.gpsimd, "sp": nc.sync}
        self.csem = {}
        self.cnt = {}
        for e in ("pe", "act", "dve", "pool"):
            self.csem[e] = nc.alloc_semaphore("c_" + e)
            self.cnt[e] = 0
        self.pending = {e: False for e in self.cnt}
        self.rings = {}
        self.dcount = {}
        for q in ("sp", "act", "pool"):
            self.rings[q] = [nc.alloc_semaphore(f"r_{q}{i}") for i in range(self.RING)]
            self.dcount[q] = 0
        self.waited = {}
        self.n_ins = 0
        self.n_wait = 0
        self._uid = 0

    def sb(self, shape, dtype, name=None):
        self._uid += 1
        name = f"{name or 't'}_{self._uid}"
        return Buf(self.nc.alloc_sbuf_tensor(name, list(shape), dtype).ap(), name)

    def ps(self, shape, dtype=F32, name=None):
        self._uid += 1
        name = f"{name or 'p'}_{self._uid}"
        return Buf(self.nc.alloc_psum_tensor(name, list(shape), dtype).ap(), name)

    def dram(self, name, shape, dtype, kind="Internal"):
        return Buf(self.nc.dram_tensor(name, list(shape), dtype, kind=kind).ap(), name)

    def mark(self):
        nc = self.nc
        return (nc.sbuf_base, nc.sbuf_top, nc.psum_base, nc.psum_top)

    def release(self, m):
        self.barrier()
        nc = self.nc
        nc.sbuf_base, nc.sbuf_top, nc.psum_base, nc.psum_top = m

    def _wait(self, ename, ev):
        sem, val = ev
        key = (ename, sem.num if hasattr(sem, "num") else id(sem))
        if self.waited.get(key, 0) >= val:
            return
        self.eng[ename].wait_ge(sem, val)
        self.waited[key] = val
        self.n_wait += 1

    def _deps(self, ename, r, w):
        evs = []
        for b in r:
            if b.w is not None:
                evs.append(b.w)
        for b in w:
            if b.w is not None:
                evs.append(b.w)
            evs.extend(b.r.values())
        own = self.csem.get(ename)
        for ev in evs:
            if ename == "pe" and ev[0] is own:
                continue
            self._wait(ename, ev)

    def _record(self, ev, r, w):
        sid = id(ev[0])
        for b in r:
            cur = b.r.get(sid)
            if cur is None or cur[1] < ev[1]:
                b.r[sid] = ev
        for b in w:
            b.w = ev
            b.r = {}

    def op(self, ename, fn, r=(), w=(), signal=True):
        self._deps(ename, r, w)
        ins = fn(self.eng[ename])
        self.n_ins += 1
        if signal:
            self.cnt[ename] += 1
            ins.then_inc(self.csem[ename], 1)
            ev = (self.csem[ename], self.cnt[ename])
        else:
            ev = (self.csem[ename], self.cnt[ename] + 1)
        self._record(ev, r, w)
        return ins

    def dma(self, q, out, in_, r=(), w=(), **kw):
        self._deps(q, r, w)
        i = self.dcount[q]
        slot = i % self.RING
        sem = self.rings[q][slot]
        if i >= self.RING:
            self._wait(q, (sem, 16 * (i // self.RING)))
        ins = self.eng[q].dma_start(out=out, in_=in_, **kw)
        ins.then_inc(sem, 16)
        self.n_ins += 1
        self.dcount[q] = i + 1
        ev = (sem, 16 * (i // self.RING + 1))
        self._record(ev, r, w)
        return ev

    def bound_reg(self, val):
        if not hasattr(self, "_bregs"):
            self._bregs = {}
        if val not in self._bregs:
            reg = self.nc.alloc_register(mybir.EngineType.Pool, f"bnd{val}")
            self.nc.reg_mov(reg, val)
            self._bregs[val] = reg
        return self._bregs[val]

    def idma(self, out, in_, out_off=None, in_off=None, bounds=None, r=(), w=()):
        q = "pool"
        self._deps(q, r, w)
        i = self.dcount[q]
        slot = i % self.RING
        sem = self.rings[q][slot]
        if i >= self.RING:
            self._wait(q, (sem, 16 * (i // self.RING)))
        oo = bass.IndirectOffsetOnAxis(ap=out_off, axis=0) if out_off is not None else None
        io = bass.IndirectOffsetOnAxis(ap=in_off, axis=0) if in_off is not None else None
        ins = self.nc.gpsimd.indirect_dma_start(out=out, out_offset=oo, in_=in_, in_offset=io,
                                                bounds_check=self.bound_reg(bounds), oob_is_err=False)
        ins.then_inc(sem, 16)
        self.n_ins += 1
        self.dcount[q] = i + 1
        ev = (sem, 16 * (i // self.RING + 1))
        self._record(ev, r, w)
        return ev

    def all_events(self):
        evs = [(self.csem[e], self.cnt[e]) for e in self.cnt if self.cnt[e] > 0]
        for q in self.rings:
            n = self.dcount[q]
            for slot in range(self.RING):
                k = (n - slot + self.RING - 1) // self.RING
                if k > 0:
                    evs.append((self.rings[q][slot], 16 * k))
        return evs

    def barrier(self, engines=("pe", "act", "dve", "pool", "sp")):
        evs = self.all_events()
        for e in engines:
            for ev in evs:
                self._wait(e, ev)

    def mm(self, out_ap, lhsT, rhs, start, stop, r=(), w=(), signal=None, **kw):
        if signal is None:
            signal = True
        return self.op("pe", lambda e: e.matmul(out_ap, lhsT, rhs, start=start, stop=stop, **kw),
                       r=r, w=w, signal=signal)

    def act(self, out, in_, func, r=(), w=(), eng="act", **kw):
        return self.op(eng, lambda e: e.activation(out=out, in_=in_, func=func, **kw), r=r, w=w)

    def tt(self, eng, out, in0, in1, op, r=(), w=()):
        return self.op(eng, lambda e: e.tensor_tensor(out=out, in0=in0, in1=in1, op=op), r=r, w=w)

    def ts(self, eng, out, in0, s1, op0, s2=None, op1=None, r=(), w=(), **kw):
        if op1 is None:
            return self.op(eng, lambda e: e.tensor_scalar(out=out, in0=in0, scalar1=s1, scalar2=None,
                                                          op0=op0, **kw), r=r, w=w)
        return self.op(eng, lambda e: e.tensor_scalar(out=out, in0=in0, scalar1=s1, scalar2=s2,
                                                      op0=op0, op1=op1, **kw), r=r, w=w)

    def stt(self, out, in0, scalar, in1, op0, op1, r=(), w=(), **kw):
        return self.op("dve", lambda e: e.scalar_tensor_tensor(out=out, in0=in0, scalar=scalar, in1=in1,
                                                               op0=op0, op1=op1, **kw), r=r, w=w)

    def copy(self, eng, out, in_, r=(), w=()):
        if eng == "act":
            return self.op("act", lambda e: e.activation(out=out, in_=in_, func=AF.Copy), r=r, w=w)
        return self.op(eng, lambda e: e.tensor_copy(out=out, in_=in_), r=r, w=w)


def _pp_layout():
    cols = {}
    off = 0

    def add(name, n):
        nonlocal off
        cols[name] = (off, n)
        off += n

    add("cvec", 32)
    add("iota", NTLMAX)
    add("pidx", 1)
    add("ltri", 128)
    for l in range(DEPTH):
        add(f"conv_a_w{l}", 4 * 31)
        add(f"conv_a_b{l}", 4)
        add(f"ln_a_g{l}", 4)
        add(f"ln_a_b{l}", 4)
        add(f"diff_norm_g{l}", 1)
        add(f"conv_c_w{l}", 4 * 3)
        add(f"norm_c_g{l}", 4)
        add(f"lru_conv_w{l}", 2 * 4 * 4)
        add(f"lru_conv_b{l}", 8)
        add(f"lru_ba{l}", 8)
        add(f"lru_bx{l}", 8)
        add(f"lru_lam{l}", 8)
        add(f"norm_d_g{l}", 4)
    return cols, off


PP_COLS, NPP = _pp_layout()


def _pack_pp(inp, b):
    pp = np.zeros((128, NPP), np.float32)

    def put(name, arr):
        o, n = PP_COLS[name]
        pp[:, o:o + n] = np.ascontiguousarray(arr, dtype=np.float32).reshape(128, n)

    def chp(v):
        return np.asarray(v).reshape(4, 128).T

    cv = np.concatenate([np.asarray(inp["c"][b]).reshape(16, 128).T,
                         np.asarray(inp["c_ctx"]).reshape(16, 128).T], axis=1)
    put("cvec", cv)
    put("iota", np.tile(np.arange(NTLMAX, dtype=np.float32), (128, 1)))
    put("pidx", np.arange(128, dtype=np.float32).reshape(128, 1))
    put("ltri", (np.arange(128)[:, None] < np.arange(128)[None, :]).astype(np.float32))
    for l in range(DEPTH):
        put(f"conv_a_w{l}", np.asarray(inp["conv_a_w"][l]).reshape(31, 4, 128).transpose(2, 1, 0))
        put(f"conv_a_b{l}", chp(inp["conv_a_b"][l]))
        put(f"ln_a_g{l}", chp(inp["ln_a_g"][l]))
        put(f"ln_a_b{l}", chp(inp["ln_a_b"][l]))
        put(f"diff_norm_g{l}", np.asarray(inp["diff_norm_g"][l]).reshape(128, 1))
        put(f"conv_c_w{l}", np.asarray(inp["conv_c_w"][l]).reshape(3, 4, 128).transpose(2, 1, 0))
        put(f"norm_c_g{l}", chp(inp["norm_c_g"][l]))
        put(f"lru_conv_w{l}", np.asarray(inp["lru_conv_w"][l]).reshape(2, 4, 4, 128).transpose(3, 0, 2, 1))
        for nm in ("lru_conv_b", "lru_ba", "lru_bx", "lru_lam"):
            put(f"{nm}{l}", np.asarray(inp[nm][l]).reshape(2, 4, 128).transpose(2, 0, 1))
        put(f"norm_d_g{l}", chp(inp["norm_d_g"][l]))
    return pp


def _pack_b1t(inp, l):
    b1 = np.asarray(inp["exp_b1"][l], dtype=np.float32)
    g = b1[:, 0::2].reshape(NE, 8, 128).transpose(0, 2, 1).reshape(NE * 128, 8)
    u = b1[:, 1::2].reshape(NE, 8, 128).transpose(0, 2, 1).reshape(NE * 128, 8)
    return np.ascontiguousarray(np.concatenate([g, u], axis=1))


def _rope_tables():
    GRID_W = 64
    rows = SEQ // GRID_W
    row = np.repeat(np.arange(rows, dtype=np.float32), GRID_W)
    col = np.tile(np.arange(GRID_W, dtype=np.float32), rows)
    half = 32
    inv = (np.float32(10000.0) ** (-np.arange(0, half, 2, dtype=np.float32) / np.float32(half))).astype(np.float32)
    ar = row[:, None] * inv
    ac = col[:, None] * inv
    ang = np.concatenate([ar, ar, ac, ac], axis=-1)
    cos = np.cos(ang).astype(np.float32)
    sin = np.sin(ang).astype(np.float32)
    sgn = np.concatenate([-np.ones(16), np.ones(16), -np.ones(16), np.ones(16)]).astype(np.float32)
    sin = sin * sgn[None, :]
    tab = np.zeros((2, 128, T), np.float32)
    tab[0, :, :CTX] = 1.0
    tab[0, 0:64, CTX:] = cos.T
    tab[0, 64:128, CTX:] = cos.T
    tab[1, 0:64, CTX:] = sin.T
    tab[1, 64:128, CTX:] = sin.T
    return tab


class Prog:
    pass


def tok_chunks(t0=0, t1=T, step=512):
    out = []
    t = t0
    while t < t1:
        n = min(step, t1 - t)
        out.append((t, n))
        t += n
    return out


def declare_io(k, G):
    nc = k.nc

    def inp(name, shape, dt=F32):
        return k.dram(name, shape, dt, kind="ExternalInput")

    G.x = inp("x", [SEQ, D])
    G.ctx = inp("ctx", [CTX, D])
    G.pp = inp("pp", [128, NPP])
    G.rope = inp("rope", [2, 128, T])
    G.ada_w = inp("ada_w", [DEPTH, D, 6 * D])
    G.ada_b = inp("ada_b", [DEPTH, 6 * D])
    G.norm1_g = inp("norm1_g", [DEPTH, D])
    G.norm2_g = inp("norm2_g", [DEPTH, D])
    G.final_g = inp("final_g", [1, D])
    G.w_in = inp("w_in", [DEPTH, D, N_IN])
    G.w_out = inp("w_out", [DEPTH, D, D])
    G.diff_lambda = inp("diff_lambda", [DEPTH, 256])
    G.lru_wa = inp("lru_wa", [DEPTH, 2, 8, 64, 64])
    G.lru_wx = inp("lru_wx", [DEPTH, 2, 8, 64, 64])
    G.router_w = inp("router_w", [DEPTH, D, NE])
    G.router_b = inp("router_b", [DEPTH, NE])
    if G.with_moe:
        G.exp_w1 = inp("exp_w1", [DEPTH, NE, D, 2 * DFF])
        G.exp_w2 = inp("exp_w2", [DEPTH, NE, DFF, D])
        G.exp_b2 = inp("exp_b2", [DEPTH, NE, D])
        G.b1t = [inp(f"b1t{l}", [NE * 128, 16]) for l in range(DEPTH)]
    G.out = k.dram("out", [SEQ, D], F32, kind="ExternalOutput")
    G.xres = k.dram("xres", [T, D], F32)
    G.modv = [k.dram(f"modv{l}", [2, 6, D], F32) for l in range(DEPTH)]
    G.UF = k.dram("UF", [3584, T], F32)
    G.QK = k.dram("QK", [1024, T], BF16)
    G.V = k.dram("Vtm", [T, 512], BF16)
    G.YC = k.dram("YC", [D, T], BF16)
    G.H2TM = k.dram("H2TM", [T, D], BF16)
    G.COMB = k.dram("COMB", [T, NE], F32)
    G.LGD = k.dram("LGD", [T, NE], F32)
    G.TOPD = k.dram("TOPD", [T, 8], F32)
    G.RANKD = k.dram("RANKD", [T, NE], F32)
    G.CNTD = k.dram("CNTD", [128, NE], F32)
    G.XS = k.dram("XS", [NTLMAX * 128, D], BF16)
    G.YS = k.dram("YS", [NTLMAX * 128, D], F32)
    G.WOB = [k.dram(f"wob{l}", [128, KC * D], BF16) for l in range(DEPTH)]
    G.RWB = [k.dram(f"rwb{l}", [128, KC * NE], BF16) for l in range(DEPTH)]
    if G.with_moe:
        G.W1B = [[k.dram(f"w1b{l}_{c}", [NE * 128, KC * 512], BF16) for c in range(4)] for l in range(DEPTH)]
        G.W2B = [[k.dram(f"w2b{l}_{h}", [NE * 128, 4 * D], BF16) for h in range(2)] for l in range(DEPTH)]


def phase_consts(k, G):
    G.ppt = k.sb([128, NPP], F32, "pp")
    k.dma("sp", G.ppt.ap, G.pp.ap, r=[G.pp], w=[G.ppt])
    G.ident_f = k.sb([128, 128], F32, "identf")
    G.ident_b = k.sb([128, 128], BF16, "identb")
    G.ones_f = k.sb([128, 128], F32, "onesf")
    k.op("pool", lambda e: e.memset(G.ident_f.ap, 0.0), w=[G.ident_f])
    k.op("pool", lambda e: e.memset(G.ones_f.ap, 1.0), w=[G.ones_f])
    k.op("pool", lambda e: e.affine_select(out=G.ident_f.ap, in_=G.ones_f.ap, pattern=[[-1, 128]],
                                           compare_op=ALU.is_equal, fill=0.0, base=0, channel_multiplier=1),
         r=[G.ones_f], w=[G.ident_f])
    k.copy("dve", G.ident_b.ap, G.ident_f.ap, r=[G.ident_f], w=[G.ident_b])
    G.eps = k.sb([128, 1], F32, "eps")
    k.op("pool", lambda e: e.memset(G.eps.ap, EPS), w=[G.eps])
    k.dma("sp", G.xres.ap[0:CTX, :], G.ctx.ap, r=[G.ctx], w=[G.xres])
    k.dma("sp", G.xres.ap[CTX:T, :], G.x.ap, r=[G.x], w=[G.xres])


def ppcol(G, name, i=0, n=1):
    o, _ = PP_COLS[name]
    return G.ppt.ap[:, o + i:o + i + n]


def phase_mod(k, G, l):
    m = k.mark()
    s = k.sb([128, 16, 2], F32, "silu_c")
    o, _ = PP_COLS["cvec"]
    k.act(s.ap[:, :, 0], G.ppt.ap[:, o:o + 16], AF.Silu, r=[G.ppt], w=[s])
    k.act(s.ap[:, :, 1], G.ppt.ap[:, o + 16:o + 32], AF.Silu, r=[G.ppt], w=[s])
    mod = k.sb([2, 6 * D], F32, "mod")
    adab = k.sb([2, 6 * D], F32, "adab")
    k.dma("sp", adab.ap, G.ada_b.ap[l:l + 1, :].to_broadcast([2, 6 * D]), r=[G.ada_b], w=[adab])
    wbufs = [k.sb([128, KC, 512], F32, f"adaw{i}") for i in range(2)]
    pss = [k.ps([2, 512], F32, f"modps{i}") for i in range(2)]
    awv = G.ada_w.ap[l].rearrange("(kc p) n -> p kc n", p=128)
    for cb in range(24):
        wb = wbufs[cb % 2]
        ps = pss[cb % 2]
        k.dma("sp" if cb % 2 == 0 else "act", wb.ap, awv[:, :, cb * 512:(cb + 1) * 512], r=[G.ada_w], w=[wb])
        for kc in range(KC):
            k.mm(ps.ap, s.ap[:, kc, :], wb.ap[:, kc, :], start=(kc == 0), stop=(kc == KC - 1),
                 r=[s, wb], w=[ps])
        k.tt("dve", mod.ap[:, cb * 512:(cb + 1) * 512], ps.ap, adab.ap[:, cb * 512:(cb + 1) * 512], ALU.add,
             r=[ps, adab], w=[mod])
    n1 = k.sb([2, D], F32, "n1")
    n2 = k.sb([2, D], F32, "n2")
    k.dma("sp", n1.ap, G.norm1_g.ap[l:l + 1, :].to_broadcast([2, D]), r=[G.norm1_g], w=[n1])
    k.dma("sp", n2.ap, G.norm2_g.ap[l:l + 1, :].to_broadcast([2, D]), r=[G.norm2_g], w=[n2])
    gs1 = k.sb([2, D], F32, "gs1")
    gs2 = k.sb([2, D], F32, "gs2")
    k.stt(gs1.ap, mod.ap[:, D:2 * D], 1.0, n1.ap, ALU.add, ALU.mult, r=[mod, n1], w=[gs1])
    k.stt(gs2.ap, mod.ap[:, 4 * D:5 * D], 1.0, n2.ap, ALU.add, ALU.mult, r=[mod, n2], w=[gs2])
    mv = G.modv[l]
    k.dma("sp", mv.ap[:, 0, :], gs1.ap, r=[gs1], w=[mv])
    k.dma("sp", mv.ap[:, 1, :], mod.ap[:, 0:D], r=[mod], w=[mv])
    k.dma("sp", mv.ap[:, 2, :], mod.ap[:, 2 * D:3 * D], r=[mod], w=[mv])
    k.dma("sp", mv.ap[:, 3, :], gs2.ap, r=[gs2], w=[mv])
    k.dma("sp", mv.ap[:, 4, :], mod.ap[:, 3 * D:4 * D], r=[mod], w=[mv])
    k.dma("sp", mv.ap[:, 5, :], mod.ap[:, 5 * D:6 * D], r=[mod], w=[mv])
    k.release(m)


def load_bcast(k, G, l, sec, which, name):
    t = k.sb([128, D], F32, name)
    mv = G.modv[l]
    k.dma("sp", t.ap, mv.ap[which:which + 1, sec, :].to_broadcast([128, D]), r=[mv], w=[t])
    return t


def norm_mod_tile(k, G, xt, gs, sh, hb, scr, ssq, rstd, tmp):
    k.act(scr.ap, xt.ap, AF.Square, r=[xt], w=[scr, ssq], accum_out=ssq.ap)
    k.act(rstd.ap, ssq.ap, AF.Sqrt, r=[ssq, G.eps], w=[rstd], scale=1.0 / D, bias=G.eps.ap)
    k.op("dve", lambda e: e.reciprocal(out=rstd.ap, in_=rstd.ap), r=[rstd], w=[rstd])
    k.stt(tmp.ap, xt.ap, rstd.ap, gs.ap, ALU.mult, ALU.mult, r=[xt, rstd, gs], w=[tmp])
    k.tt("pool", hb.ap, tmp.ap, sh.ap, ALU.add, r=[tmp, sh], w=[hb])


def phase_inproj(k, G, l):
    m = k.mark()
    hT_t = k.nc.alloc_sbuf_tensor(f"hT{l}", [128, KC, T], BF16).ap()
    hT = [Buf(hT_t, f"hT{i}") for i in range(NT)]
    m1 = k.mark()
    gs = [load_bcast(k, G, l, 0, w, "gs1") for w in range(2)]
    sh = [load_bcast(k, G, l, 1, w, "sh1") for w in range(2)]
    xts = [k.sb([128, D], F32, "xt") for _ in range(2)]
    tmp = k.sb([128, D], F32, "tmp")
    scr = k.sb([128, D], BF16, "scr")
    hbs = [k.sb([128, D], BF16, "hb") for _ in range(2)]
    ssq = k.sb([128, 1], F32, "ssq")
    rstd = k.sb([128, 1], F32, "rstd")
    pts = [k.ps([128, 1024], BF16, "pt") for _ in range(2)]
    for i in range(NT):
        which = 1 if i < 2 else 0
        xt = xts[i % 2]
        hb = hbs[i % 2]
        k.dma("sp", xt.ap, G.xres.ap[i * 128:(i + 1) * 128, :], r=[G.xres], w=[xt])
        norm_mod_tile(k, G, xt, gs[which], sh[which], hb, scr, ssq, rstd, tmp)
        for half in range(2):
            pt = pts[half]
            for j in range(8):
                kc = half * 8 + j
                k.op("pe", lambda e: e.transpose(out=pt.ap[:, j * 128:(j + 1) * 128],
                                                 in_=hb.ap[:, kc * 128:(kc + 1) * 128], identity=G.ident_b.ap),
                     r=[hb, G.ident_b], w=[pt])
            k.copy("act" if half == 0 else "dve",
                   hT_t[:, half * 8:(half + 1) * 8, i * 128:(i + 1) * 128],
                   pt.ap.rearrange("p (a b) -> p a b", a=8), r=[pt], w=[hT[i]])
    k.release(m1)
    cos2 = k.sb([128, T], F32, "cos2")
    sin2 = k.sb([128, T], F32, "sin2")
    k.dma("sp", cos2.ap, G.rope.ap[0], r=[G.rope], w=[cos2])
    k.dma("sp", sin2.ap, G.rope.ap[1], r=[G.rope], w=[sin2])
    wbufs = [k.sb([128, KC, 512], BF16, "wblk") for _ in range(2)]
    wperm = k.sb([128, KC, 512], BF16, "wperm")
    pss = [k.ps([128, 512], F32, "ps") for _ in range(6)]
    stage_f = [k.sb([128, T], F32, "stf") for _ in range(2)]
    stage_b = [k.sb([128, T], BF16, "stb") for _ in range(2)]
    t1 = k.sb([128, 512], F32, "ropet1")
    t2 = k.sb([128, 512], F32, "ropet2")
    vst = [k.sb([128, 512], BF16, "vst") for _ in range(2)]
    wv = G.w_in.ap[l].rearrange("(kc p) n -> p kc n", p=128)
    chunks = tok_chunks()
    psi = [0]
    evi = [0]

    def nextps():
        p = pss[psi[0] % len(pss)]
        psi[0] += 1
        return p

    def proj_fm(wb, mblk, n0, nn):
        ps = nextps()
        tiles = [hT[i] for i in range(n0 // 128, (n0 + nn) // 128)]
        for kc in range(KC):
            k.mm(ps.ap[:, :nn], wb.ap[:, kc, mblk * 128:(mblk + 1) * 128], hT_t[:, kc, n0:n0 + nn],
                 start=(kc == 0), stop=(kc == KC - 1), r=[wb] + tiles, w=[ps])
        return ps

    uf_row = 0
    nst = 0
    for cb in range(10):
        wb = wbufs[cb % 2]
        k.dma("pool", wb.ap, wv[:, :, cb * 512:(cb + 1) * 512], r=[G.w_in], w=[wb])
        if cb in (2, 3):
            src = wb.ap.rearrange("p kc (g s j) -> p (kc g) s j", s=2, j=16)
            dst = wperm.ap.rearrange("p kc (g s j) -> p (kc g) s j", s=2, j=16)
            k.copy("dve", dst[:, :, 0, :], src[:, :, 1, :], r=[wb], w=[wperm])
            k.copy("dve", dst[:, :, 1, :], src[:, :, 0, :], r=[wb], w=[wperm])
            for mblk in range(4):
                st = stage_b[nst % 2]
                nst += 1
                for (n0, nn) in chunks:
                    pa = proj_fm(wb, mblk, n0, nn)
                    pb = proj_fm(wperm, mblk, n0, nn)
                    k.tt("dve", t1.ap[:, :nn], pa.ap[:, :nn], cos2.ap[:, n0:n0 + nn], ALU.mult,
                         r=[pa, cos2], w=[t1])
                    k.tt("dve", t2.ap[:, :nn], pb.ap[:, :nn], sin2.ap[:, n0:n0 + nn], ALU.mult,
                         r=[pb, sin2], w=[t2])
                    k.tt("pool", st.ap[:, n0:n0 + nn], t1.ap[:, :nn], t2.ap[:, :nn], ALU.add,
                         r=[t1, t2], w=[st])
                row = (cb - 2) * 512 + mblk * 128
                k.dma("sp", G.QK.ap[row:row + 128, :], st.ap, r=[st], w=[G.QK])
        elif cb == 4:
            for i in range(NT):
                ps = nextps()
                for kc in range(KC):
                    k.mm(ps.ap, hT_t[:, kc, i * 128:(i + 1) * 128], wb.ap[:, kc, :],
                         start=(kc == 0), stop=(kc == KC - 1), r=[wb, hT[i]], w=[ps])
                vs = vst[i % 2]
                k.copy("act" if i % 2 == 0 else "dve", vs.ap, ps.ap, r=[ps], w=[vs])
                k.dma("sp", G.V.ap[i * 128:(i + 1) * 128, :], vs.ap, r=[vs], w=[G.V])
        else:
            for mblk in range(4):
                st = stage_f[nst % 2]
                nst += 1
                for (n0, nn) in chunks:
                    ps = proj_fm(wb, mblk, n0, nn)
                    k.copy("act" if evi[0] % 2 == 0 else "dve", st.ap[:, n0:n0 + nn], ps.ap[:, :nn],
                           r=[ps], w=[st])
                    evi[0] += 1
                k.dma("sp", G.UF.ap[uf_row:uf_row + 128, :], st.ap, r=[st], w=[G.UF])
                uf_row += 128
    assert uf_row == 3584
    k.release(m)


def build_program(upto="all", dbg=(), with_moe=True):
    nc = bass.Bass("TRN2", target_bir_lowering=False)
    k = KB(nc)
    G = Prog()
    G.with_moe = with_moe
    declare_io(k, G)
    phase_consts(k, G)
    done = False
    for l in range(DEPTH):
        phase_mod(k, G, l)
    if upto == "mod":
        done = True
    for l in range(DEPTH):
        if done:
            break
        phase_inproj(k, G, l)
        if upto == f"inproj{l}":
            break
        precast_outproj(k, G, l)
        if G.with_moe:
            G.bg = precast_list(k, G, l)
            pump(G, len(G.bg))
        phase_mixers(k, G, l, need_ctx=(l < DEPTH - 1))
        if upto == f"mix{l}":
            break
        phase_outproj(k, G, l, need_ctx=(l < DEPTH - 1))
        if upto == f"outproj{l}":
            break
        phase_moe(k, G, l, need_ctx=(l < DEPTH - 1), last=(l == DEPTH - 1))
        if upto == f"moe{l}":
            break
    for name in dbg:
        src = getattr(G, name) if not name.startswith("modv") else G.modv[int(name[4:])]
        o = k.dram("dbg_" + name, list(src.ap.shape), src.ap.dtype, kind="ExternalOutput")
        k.dma("sp", o.ap, src.ap, r=[src], w=[o])
    k.barrier(engines=("sp",))
    G.k = k
    return nc, G


def make_in_maps(inp, cores):
    rope = _rope_tables()
    shared = {
        "rope": rope,
        "ada_w": np.ascontiguousarray(inp["ada_w"], dtype=np.float32),
        "ada_b": np.ascontiguousarray(inp["ada_b"], dtype=np.float32),
        "norm1_g": np.ascontiguousarray(inp["norm1_g"], dtype=np.float32),
        "norm2_g": np.ascontiguousarray(inp["norm2_g"], dtype=np.float32),
        "final_g": np.ascontiguousarray(inp["final_g"], dtype=np.float32).reshape(1, D),
        "w_in": np.ascontiguousarray(inp["w_in"], dtype=np.float32),
        "w_out": np.ascontiguousarray(inp["w_out"], dtype=np.float32),
        "diff_lambda": np.ascontiguousarray(inp["diff_lambda"], dtype=np.float32).reshape(DEPTH, 256),
        "lru_wa": np.ascontiguousarray(inp["lru_wa"], dtype=np.float32),
        "lru_wx": np.ascontiguousarray(inp["lru_wx"], dtype=np.float32),
        "router_w": np.ascontiguousarray(inp["router_w"], dtype=np.float32),
        "router_b": np.ascontiguousarray(inp["router_b"], dtype=np.float32),
        "exp_w1": np.ascontiguousarray(inp["exp_w1"], dtype=np.float32),
        "exp_w2": np.ascontiguousarray(inp["exp_w2"], dtype=np.float32),
        "exp_b2": np.ascontiguousarray(inp["exp_b2"], dtype=np.float32),
    }
    for l in range(DEPTH):
        shared[f"b1t{l}"] = _pack_b1t(inp, l)
    maps = []
    for b in cores:
        mp = dict(shared)
        mp["x"] = np.ascontiguousarray(inp["x"][b], dtype=np.float32)
        mp["ctx"] = np.ascontiguousarray(inp["ctx"][b], dtype=np.float32)
        mp["pp"] = _pack_pp(inp, b)
        maps.append(mp)
    return maps


def useg(gap, step=512):
    out = [(0, CTX, 0)]
    for (t0, n) in tok_chunks(CTX, T, step):
        out.append((t0, n, t0 + gap))
    return out


def load_gapped(k, G, dst, dst_off_ctx, dst_off_lat, src_buf, row0, q="sp"):
    k.dma(q, dst.ap[:, dst_off_ctx:dst_off_ctx + CTX], src_buf.ap[row0:row0 + 128, 0:CTX], r=[src_buf], w=[dst])
    k.dma(q, dst.ap[:, dst_off_lat:dst_off_lat + SEQ], src_buf.ap[row0:row0 + 128, CTX:T], r=[src_buf], w=[dst])


def finish_norm(k, G, ys, gap, kind, l, yc_row0, gname, bname=None):
    m = k.mark()
    ps_sq = [k.ps([128, 512], F32, "pssq") for _ in range(2)]
    ps_su = [k.ps([128, 512], F32, "pssu") for _ in range(2)] if kind == "ln_silu" else None
    sq = [k.sb([128, 512], F32, "sq") for _ in range(2)]
    rstd = [k.sb([128, 512], F32, "rstd") for _ in range(2)]
    mean = [k.sb([128, 512], F32, "mean") for _ in range(2)]
    msq = k.sb([128, 512], F32, "msq")
    t1 = [k.sb([128, 512], F32, "t1") for _ in range(2)]
    stage = [k.sb([128, T], BF16, "ystage") for _ in range(4)]
    for ci, (t0, n, u0) in enumerate(useg(gap)):
        pq = ps_sq[ci % 2]
        rs = rstd[ci % 2]
        mn = mean[ci % 2]
        for c in range(4):
            s = sq[c % 2]
            k.act(s.ap[:, :n], ys[c].ap[:, u0:u0 + n], AF.Square, r=[ys[c]], w=[s])
            k.mm(pq.ap[:, :n], G.ones_f.ap, s.ap[:, :n], start=(c == 0), stop=(c == 3), r=[G.ones_f, s], w=[pq])
        if kind == "ln_silu":
            pu = ps_su[ci % 2]
            for c in range(4):
                k.mm(pu.ap[:, :n], G.ones_f.ap, ys[c].ap[:, u0:u0 + n], start=(c == 0), stop=(c == 3),
                     r=[G.ones_f, ys[c]], w=[pu])
            k.ts("dve", mn.ap[:, :n], pu.ap[:, :n], 1.0 / GW, ALU.mult, r=[pu], w=[mn])
            k.tt("dve", msq.ap[:, :n], mn.ap[:, :n], mn.ap[:, :n], ALU.mult, r=[mn], w=[msq])
            k.stt(rs.ap[:, :n], pq.ap[:, :n], 1.0 / GW, msq.ap[:, :n], ALU.mult, ALU.subtract, r=[pq, msq], w=[rs])
            k.act(rs.ap[:, :n], rs.ap[:, :n], AF.Sqrt, r=[rs, G.eps], w=[rs], bias=G.eps.ap)
        else:
            k.act(rs.ap[:, :n], pq.ap[:, :n], AF.Sqrt, r=[pq, G.eps], w=[rs], scale=1.0 / GW, bias=G.eps.ap)
        k.op("dve", lambda e: e.reciprocal(out=rs.ap[:, :n], in_=rs.ap[:, :n]), r=[rs], w=[rs])
        for c in range(4):
            gcol = ppcol(G, f"{gname}{l}", c)
            if kind == "ln_silu":
                bcol = ppcol(G, f"{bname}{l}", c)
                t = t1[c % 2]
                k.tt("dve", t.ap[:, :n], ys[c].ap[:, u0:u0 + n], mn.ap[:, :n], ALU.subtract, r=[ys[c], mn], w=[t])
                k.tt("pool", t.ap[:, :n], t.ap[:, :n], rs.ap[:, :n], ALU.mult, r=[t, rs], w=[t])
                k.act(stage[c].ap[:, t0:t0 + n], t.ap[:, :n], AF.Silu, r=[t, G.ppt], w=[stage[c]],
                      scale=gcol, bias=bcol)
            else:
                k.stt(stage[c].ap[:, t0:t0 + n], ys[c].ap[:, u0:u0 + n], gcol, rs.ap[:, :n], ALU.mult, ALU.mult,
                      r=[ys[c], rs, G.ppt], w=[stage[c]])
    for c in range(4):
        k.dma("sp", G.YC.ap[yc_row0 + c * 128:yc_row0 + (c + 1) * 128, :], stage[c].ap, r=[stage[c]], w=[G.YC])
    k.release(m)


def mixer_conformer(k, G, l):
    m = k.mark()
    GAP = 30
    NU = T + GAP
    ZW = NU + 30
    ys = [k.sb([128, NU], F32, "convA") for _ in range(4)]
    zps = [k.sb([128, ZW], F32, "zpA") for _ in range(2)]
    vals = [k.sb([128, T], F32, "valA") for _ in range(2)]
    gates = [k.sb([128, T], F32, "gateA") for _ in range(2)]
    for zp in zps:
        k.op("pool", lambda e: e.memset(zp.ap, 0.0), w=[zp])
    wo, _ = PP_COLS[f"conv_a_w{l}"]
    for c in range(4):
        zp = zps[c % 2]
        va = vals[c % 2]
        ga = gates[c % 2]
        k.dma("sp", va.ap, G.UF.ap[c * 128:(c + 1) * 128, :], r=[G.UF], w=[va])
        k.dma("act", ga.ap, G.UF.ap[512 + c * 128:512 + (c + 1) * 128, :], r=[G.UF], w=[ga])
        k.act(ga.ap, ga.ap, AF.Sigmoid, r=[ga], w=[ga])
        k.tt("pool", zp.ap[:, 15:15 + CTX], va.ap[:, 0:CTX], ga.ap[:, 0:CTX], ALU.mult, r=[va, ga], w=[zp])
        k.tt("pool", zp.ap[:, 45 + CTX:45 + CTX + SEQ], va.ap[:, CTX:T], ga.ap[:, CTX:T], ALU.mult,
             r=[va, ga], w=[zp])
        y = ys[c]
        wcol = lambda kk: G.ppt.ap[:, wo + c * 31 + kk:wo + c * 31 + kk + 1]
        k.ts("dve", y.ap, zp.ap[:, 0:NU], wcol(0), ALU.mult, ppcol(G, f"conv_a_b{l}", c), ALU.add,
             r=[zp, G.ppt], w=[y])
        for kk in range(1, 31):
            k.stt(y.ap, zp.ap[:, kk:kk + NU], wcol(kk), y.ap, ALU.mult, ALU.add, r=[zp, y, G.ppt], w=[y])
    finish_norm(k, G, ys, GAP, "ln_silu", l, 0, "ln_a_g", "ln_a_b")
    k.release(m)


def mixer_sconv(k, G, l):
    m = k.mark()
    GAP = 2
    NU = T + GAP
    ZW = NU + 2
    ys = [k.sb([128, NU], F32, "convC") for _ in range(4)]
    zps = [k.sb([128, ZW], F32, "zpC") for _ in range(2)]
    bgs = [k.sb([128, NU], F32, "bgC") for _ in range(2)]
    cgs = [k.sb([128, T], F32, "cgC") for _ in range(2)]
    vs = [k.sb([128, T], F32, "vC") for _ in range(2)]
    for zp in zps:
        k.op("pool", lambda e: e.memset(zp.ap, 0.0), w=[zp])
    for bg in bgs:
        k.op("pool", lambda e: e.memset(bg.ap, 0.0), w=[bg])
    wo, _ = PP_COLS[f"conv_c_w{l}"]
    for c in range(4):
        zp, bg, cg, v = zps[c % 2], bgs[c % 2], cgs[c % 2], vs[c % 2]
        load_gapped(k, G, bg, 0, CTX + GAP, G.UF, 1024 + c * 128, q="sp")
        k.dma("act", cg.ap, G.UF.ap[1536 + c * 128:1536 + (c + 1) * 128, :], r=[G.UF], w=[cg])
        k.dma("sp", v.ap, G.UF.ap[2048 + c * 128:2048 + (c + 1) * 128, :], r=[G.UF], w=[v])
        k.tt("pool", zp.ap[:, 1:1 + CTX], cg.ap[:, 0:CTX], v.ap[:, 0:CTX], ALU.mult, r=[cg, v], w=[zp])
        k.tt("pool", zp.ap[:, 3 + CTX:3 + CTX + SEQ], cg.ap[:, CTX:T], v.ap[:, CTX:T], ALU.mult, r=[cg, v], w=[zp])
        y = ys[c]
        wcol = lambda kk: G.ppt.ap[:, wo + c * 3 + kk:wo + c * 3 + kk + 1]
        k.ts("dve", y.ap, zp.ap[:, 0:NU], wcol(0), ALU.mult, r=[zp, G.ppt], w=[y])
        for kk in range(1, 3):
            k.stt(y.ap, zp.ap[:, kk:kk + NU], wcol(kk), y.ap, ALU.mult, ALU.add, r=[zp, y, G.ppt], w=[y])
        k.tt("dve", y.ap, y.ap, bg.ap, ALU.mult, r=[y, bg], w=[y])
    finish_norm(k, G, ys, GAP, "rms", l, 1024, "norm_c_g")
    k.release(m)


def mixer_lru(k, G, l):
    m = k.mark()
    GAP = 6
    NU = T + GAP
    XW = NU + 6
    ys = [k.sb([128, NU], F32, "yD") for _ in range(4)]
    xp = k.sb([128, XW], F32, "xpD")
    xcv = k.sb([128, NU], F32, "xcvD")
    ra = k.sb([128, NU], F32, "raD")
    gx = k.sb([128, NU], F32, "gxD")
    sq = k.sb([128, NU], F32, "sqD")
    hd = [k.sb([128, NU], F32, "hD") for _ in range(2)]
    gt = k.sb([128, NU], F32, "gtD")
    gt2 = k.sb([128, NU], F32, "gt2D")
    bd_a = k.sb([128, 128], F32, "bdA")
    bd_x = k.sb([128, 128], F32, "bdX")
    negsp = k.sb([128, 8], F32, "negsp")
    one_c = k.sb([128, 1], F32, "onec")
    pss = [k.ps([128, 512], F32, "psD") for _ in range(4)]
    k.op("pool", lambda e: e.memset(one_c.ap, 1.0), w=[one_c])
    k.op("pool", lambda e: e.memset(xp.ap, 0.0), w=[xp])
    k.op("pool", lambda e: e.memset(gt.ap, 0.0), w=[gt])
    for hh in hd:
        k.op("pool", lambda e: e.memset(hh.ap, 0.0), w=[hh])
    k.op("pool", lambda e: e.memset(bd_a.ap, 0.0), w=[bd_a])
    k.op("pool", lambda e: e.memset(bd_x.ap, 0.0), w=[bd_x])
    lo, _ = PP_COLS[f"lru_lam{l}"]
    k.act(negsp.ap, G.ppt.ap[:, lo:lo + 8], AF.Exp, r=[G.ppt], w=[negsp], scale=-1.0)
    k.act(negsp.ap, negsp.ap, AF.Ln, r=[negsp, one_c], w=[negsp], bias=one_c.ap)
    k.ts("dve", negsp.ap, negsp.ap, -8.0, ALU.mult, r=[negsp], w=[negsp])
    wo, _ = PP_COLS[f"lru_conv_w{l}"]
    uchunks = tok_chunks(0, NU, 512)
    pi = 0
    for c in range(4):
        load_gapped(k, G, xp, 3, 9 + CTX, G.UF, 3072 + c * 128, q="sp")
        load_gapped(k, G, gt, 0, CTX + GAP, G.UF, 2560 + c * 128, q="act")
        for d in range(2):
            col = d * 4 + c
            sh = 0 if d == 0 else 3
            wcol = lambda kk: G.ppt.ap[:, wo + col * 4 + kk:wo + col * 4 + kk + 1]
            k.ts("dve", xcv.ap, xp.ap[:, sh:sh + NU], wcol(0), ALU.mult, ppcol(G, f"lru_conv_b{l}", col), ALU.add,
                 r=[xp, G.ppt], w=[xcv])
            for kk in range(1, 4):
                k.stt(xcv.ap, xp.ap[:, sh + kk:sh + kk + NU], wcol(kk), xcv.ap, ALU.mult, ALU.add,
                      r=[xp, xcv, G.ppt], w=[xcv])
            for (bd, wsrc) in ((bd_a, G.lru_wa), (bd_x, G.lru_wx)):
                k.dma("sp", bd.ap[0:64, 0:64], wsrc.ap[l, d, 2 * c], r=[wsrc], w=[bd])
                k.dma("sp", bd.ap[64:128, 64:128], wsrc.ap[l, d, 2 * c + 1], r=[wsrc], w=[bd])
            for (u0, n) in uchunks:
                pa = pss[pi % 4]
                px = pss[(pi + 1) % 4]
                pi += 2
                k.mm(pa.ap[:, :n], bd_a.ap, xcv.ap[:, u0:u0 + n], start=True, stop=True, r=[bd_a, xcv], w=[pa])
                k.mm(px.ap[:, :n], bd_x.ap, xcv.ap[:, u0:u0 + n], start=True, stop=True, r=[bd_x, xcv], w=[px])
                k.act(ra.ap[:, u0:u0 + n], pa.ap[:, :n], AF.Sigmoid, r=[pa, G.ppt], w=[ra],
                      bias=ppcol(G, f"lru_ba{l}", col))
                k.act(gx.ap[:, u0:u0 + n], px.ap[:, :n], AF.Sigmoid, r=[px, G.ppt], w=[gx],
                      bias=ppcol(G, f"lru_bx{l}", col))
            k.act(ra.ap, ra.ap, AF.Exp, r=[ra, negsp], w=[ra], scale=negsp.ap[:, col:col + 1])
            k.tt("pool", gx.ap, gx.ap, xcv.ap, ALU.mult, r=[gx, xcv], w=[gx])
            k.tt("dve", sq.ap, ra.ap, ra.ap, ALU.mult, r=[ra], w=[sq])
            k.act(sq.ap, sq.ap, AF.Sqrt, r=[sq, one_c], w=[sq], scale=-1.0, bias=one_c.ap)
            k.tt("dve", gx.ap, gx.ap, sq.ap, ALU.mult, r=[gx, sq], w=[gx])
            h = hd[d]
            if d == 0:
                k.op("dve", lambda e: e.tensor_tensor_scan(out=h.ap[:, 0:CTX], data0=ra.ap[:, 0:CTX],
                                                           data1=gx.ap[:, 0:CTX], initial=0.0,
                                                           op0=ALU.mult, op1=ALU.add), r=[ra, gx], w=[h])
                k.op("dve", lambda e: e.tensor_tensor_scan(out=h.ap[:, CTX + GAP:NU], data0=ra.ap[:, CTX + GAP:NU],
                                                           data1=gx.ap[:, CTX + GAP:NU],
                                                           initial=h.ap[:, CTX - 1:CTX],
                                                           op0=ALU.mult, op1=ALU.add), r=[ra, gx, h], w=[h])
            else:
                k.op("dve", lambda e: e.tensor_tensor_scan(out=h.ap[:, CTX - 1::-1], data0=ra.ap[:, CTX - 1::-1],
                                                           data1=gx.ap[:, CTX - 1::-1], initial=0.0,
                                                           op0=ALU.mult, op1=ALU.add), r=[ra, gx], w=[h])
                lo_ = CTX + GAP - 1
                k.op("dve", lambda e: e.tensor_tensor_scan(out=h.ap[:, NU - 1:lo_:-1], data0=ra.ap[:, NU - 1:lo_:-1],
                                                           data1=gx.ap[:, NU - 1:lo_:-1],
                                                           initial=h.ap[:, 0:1],
                                                           op0=ALU.mult, op1=ALU.add), r=[ra, gx, h], w=[h])
        y = ys[c]
        k.act(gt2.ap, gt.ap, AF.Square, r=[gt], w=[gt2])
        k.ts("dve", gt2.ap, gt2.ap, 0.044715, ALU.mult, 1.0, ALU.add, r=[gt2], w=[gt2])
        k.tt("dve", gt2.ap, gt2.ap, gt.ap, ALU.mult, r=[gt2, gt], w=[gt2])
        k.act(gt2.ap, gt2.ap, AF.Sigmoid, r=[gt2], w=[gt2], scale=1.5957691216057308)
        k.tt("dve", gt2.ap, gt2.ap, gt.ap, ALU.mult, r=[gt2, gt], w=[gt2])
        k.tt("pool", y.ap[:, 0:CTX], hd[0].ap[:, 0:CTX], hd[1].ap[:, 0:CTX], ALU.add, r=[hd[0], hd[1]], w=[y])
        k.tt("pool", y.ap[:, CTX:NU], hd[0].ap[:, CTX:NU], hd[1].ap[:, CTX:NU], ALU.add, r=[hd[0], hd[1]], w=[y])
        k.tt("dve", y.ap, y.ap, gt2.ap, ALU.mult, r=[y, gt2], w=[y])
    finish_norm(k, G, ys, GAP, "rms", l, 1536, "norm_d_g")
    k.release(m)


def mixer_attn(k, G, l, need_ctx):
    m = k.mark()
    lam_init = 0.8 - 0.6 * math.exp(-0.3 * l)
    dl = k.sb([128, 256], F32, "dlam")
    k.dma("sp", dl.ap, G.diff_lambda.ap[l:l + 1, :].to_broadcast([128, 256]), r=[G.diff_lambda], w=[dl])
    pr = k.sb([128, 128], F32, "dlpr")
    s2 = k.sb([128, 2], F32, "dls")
    dlv = dl.ap.rearrange("p (a b) -> p a b", a=4)
    k.tt("dve", pr.ap[:, 0:64], dlv[:, 0, :], dlv[:, 1, :], ALU.mult, r=[dl], w=[pr])
    k.tt("dve", pr.ap[:, 64:128], dlv[:, 2, :], dlv[:, 3, :], ALU.mult, r=[dl], w=[pr])
    k.op("dve", lambda e: e.reduce_sum(out=s2.ap, in_=pr.ap.rearrange("p (a b) -> p a b", a=2), axis=AX.X),
         r=[pr], w=[s2])
    k.act(s2.ap, s2.ap, AF.Exp, r=[s2], w=[s2])
    neg_lam = k.sb([128, 1], F32, "neglam")
    k.tt("dve", neg_lam.ap, s2.ap[:, 1:2], s2.ap[:, 0:1], ALU.subtract, r=[s2], w=[neg_lam])
    k.ts("dve", neg_lam.ap, neg_lam.ap, -lam_init, ALU.add, r=[neg_lam], w=[neg_lam])
    gsc = k.sb([128, 1], F32, "gsc")
    k.ts("dve", gsc.ap, ppcol(G, f"diff_norm_g{l}"), 1.0 - lam_init, ALU.mult, r=[G.ppt], w=[gsc])
    ones_b = k.sb([128, 128], BF16, "onesb")
    k.copy("dve", ones_b.ap, G.ones_f.ap, r=[G.ones_f], w=[ones_b])
    vt = k.sb([128, NT, 512], BF16, "vtm")
    k.dma("sp", vt.ap, G.V.ap.rearrange("(t p) e -> p t e", p=128), r=[G.V], w=[vt])
    qT = [k.sb([64, T], BF16, "qT") for _ in range(2)]
    kT = [k.sb([64, T], BF16, "kT") for _ in range(2)]
    pts = [k.sb([128, 512], BF16, "pexp") for _ in range(3)]
    ps_s = [k.ps([128, 512], F32, "ps_s") for _ in range(2)]
    ps_acc = [k.ps([128, 512], F32, "ps_acc") for _ in range(2)]
    ps_den = [k.ps([128, 512], F32, "ps_den") for _ in range(2)]
    ps_n = k.ps([128, 512], F32, "ps_n")
    rden = [k.sb([128, 512], F32, "rden") for _ in range(2)]
    tnum = [k.sb([128, 512], F32, "tnum") for _ in range(2)]
    osb = k.sb([128, 512], F32, "osb")
    osq = k.sb([128, 512], F32, "osq")
    rs = k.sb([128, 512], F32, "rsb")
    stage = [k.sb([128, T], BF16, "ystB") for _ in range(2)]
    si = 0
    pi = 0
    for h in range(4):
        for mi in range(2):
            j = 2 * h + mi
            k.dma("sp", qT[mi].ap, G.QK.ap[j * 64:(j + 1) * 64, :], r=[G.QK], w=[qT[mi]])
            k.dma("act", kT[mi].ap, G.QK.ap[512 + j * 64:512 + (j + 1) * 64, :], r=[G.QK], w=[kT[mi]])
        st = stage[h % 2]
        qchunks = [(t0, n, NT) for (t0, n) in tok_chunks(CTX, T, 512)]
        if need_ctx:
            qchunks = [(0, CTX, 2)] + qchunks
        for (t0, n, nkt) in qchunks:
            for mi in range(2):
                for kt in range(nkt):
                    pss = ps_s[si % 2]
                    si += 1
                    k.mm(pss.ap[:, :n], kT[mi].ap[:, kt * 128:(kt + 1) * 128], qT[mi].ap[:, t0:t0 + n],
                         start=True, stop=True, r=[kT[mi], qT[mi]], w=[pss])
                    pt = pts[pi % 3]
                    pi += 1
                    k.act(pt.ap[:, :n], pss.ap[:, :n], AF.Exp, r=[pss], w=[pt], scale=0.125)
                    k.mm(ps_acc[mi].ap[:, :n], vt.ap[:, kt, h * 128:(h + 1) * 128], pt.ap[:, :n],
                         start=(kt == 0), stop=(kt == nkt - 1), r=[vt, pt], w=[ps_acc[mi]])
                    k.mm(ps_den[mi].ap[:, :n], ones_b.ap, pt.ap[:, :n],
                         start=(kt == 0), stop=(kt == nkt - 1), r=[ones_b, pt], w=[ps_den[mi]])
            for mi in range(2):
                k.op("dve", lambda e: e.reciprocal(out=rden[mi].ap[:, :n], in_=ps_den[mi].ap[:, :n]),
                     r=[ps_den[mi]], w=[rden[mi]])
                k.tt("dve", tnum[mi].ap[:, :n], ps_acc[mi].ap[:, :n], rden[mi].ap[:, :n], ALU.mult,
                     r=[ps_acc[mi], rden[mi]], w=[tnum[mi]])
            k.stt(osb.ap[:, :n], tnum[1].ap[:, :n], neg_lam.ap, tnum[0].ap[:, :n], ALU.mult, ALU.add,
                  r=[tnum[0], tnum[1], neg_lam], w=[osb])
            k.act(osq.ap[:, :n], osb.ap[:, :n], AF.Square, r=[osb], w=[osq])
            k.mm(ps_n.ap[:, :n], G.ones_f.ap, osq.ap[:, :n], start=True, stop=True, r=[G.ones_f, osq], w=[ps_n])
            k.act(rs.ap[:, :n], ps_n.ap[:, :n], AF.Sqrt, r=[ps_n, G.eps], w=[rs], scale=1.0 / 128, bias=G.eps.ap)
            k.op("dve", lambda e: e.reciprocal(out=rs.ap[:, :n], in_=rs.ap[:, :n]), r=[rs], w=[rs])
            k.stt(st.ap[:, t0:t0 + n], osb.ap[:, :n], gsc.ap, rs.ap[:, :n], ALU.mult, ALU.mult,
                  r=[osb, gsc, rs], w=[st])
        if need_ctx:
            k.dma("sp", G.YC.ap[512 + h * 128:512 + (h + 1) * 128, :], st.ap, r=[st], w=[G.YC])
        else:
            k.dma("sp", G.YC.ap[512 + h * 128:512 + (h + 1) * 128, CTX:T], st.ap[:, CTX:T], r=[st], w=[G.YC])
    k.release(m)


MIXSEL = "bacd"


def phase_mixers(k, G, l, need_ctx):
    if "b" in MIXSEL:
        mixer_attn(k, G, l, need_ctx)
    if "a" in MIXSEL:
        mixer_conformer(k, G, l)
    if "c" in MIXSEL:
        mixer_sconv(k, G, l)
    if "d" in MIXSEL:
        mixer_lru(k, G, l)


def phase_outproj(k, G, l, need_ctx):
    m = k.mark()
    wo = k.sb([128, KC, D], BF16, "wout")
    wsrc = G.WOB[l].ap.rearrange("p (kc n) -> p kc n", kc=KC)
    for j in range(4):
        k.dma("act", wo.ap[:, j * 4:(j + 1) * 4, :], wsrc[:, j * 4:(j + 1) * 4, :], r=[G.WOB[l]], w=[wo])
    rw = k.sb([128, KC, NE], BF16, "rw")
    k.dma("act", rw.ap, G.RWB[l].ap.rearrange("p (kc e) -> p kc e", kc=KC), r=[G.RWB[l]], w=[rw])
    rb = k.sb([128, NE], F32, "rb")
    k.dma("sp", rb.ap, G.router_b.ap[l:l + 1, :].to_broadcast([128, NE]), r=[G.router_b], w=[rb])
    g1 = k.sb([128, D], F32, "g1b")
    gs2 = k.sb([128, D], F32, "gs2b")
    sh2 = k.sb([128, D], F32, "sh2b")
    ycs = [k.sb([128, KC, 512], BF16, "ycblk") for _ in range(2)]
    xts = [k.sb([128, D], F32, "xt") for _ in range(2)]
    tmp = k.sb([128, D], F32, "tmp")
    scr = k.sb([128, D], BF16, "scr")
    hbs = [k.sb([128, D], BF16, "hb") for _ in range(2)]
    h2s = [k.sb([128, KC, 128], BF16, "h2s") for _ in range(2)]
    ssq = k.sb([128, 1], F32, "ssq")
    rstd = k.sb([128, 1], F32, "rstd")
    lg = k.sb([128, NE], F32, "lg")
    ex = k.sb([128, NE], F32, "ex")
    msk = k.sb([128, NE], F32, "msk")
    top8 = k.sb([128, 8], F32, "top8")
    nmx = k.sb([128, 1], F32, "nmx")
    ssum = k.sb([128, 1], F32, "ssum")
    cmb = [k.sb([128, NE], F32, "cmb") for _ in range(2)]
    ps_y = [k.ps([128, 512], F32, "psy") for _ in range(4)]
    pts = [k.ps([128, 1024], BF16, "pt") for _ in range(2)]
    ps_l = k.ps([128, NE], F32, "psl")
    ps_r = k.ps([128, 2 * NE], F32, "psr")
    rks = [k.sb([128, NE], F32, "rk") for _ in range(2)]
    lgs_ = [k.sb([128, NE], F32, "lgc") for _ in range(2)]
    t8s = [k.sb([128, 8], F32, "t8c") for _ in range(2)]
    cnt = k.sb([128, NE], F32, "cnt")
    k.op("dve", lambda e: e.memset(cnt.ap, 0.0), w=[cnt])
    lto, _ = PP_COLS["ltri"]
    ltri = G.ppt.ap[:, lto:lto + 128]
    mv = G.modv[l]
    tiles = list(range(NT)) if need_ctx else list(range(2, NT))
    cur_which = None
    cur_blk = None
    nblk = 0
    for i in tiles:
        which = 1 if i < 2 else 0
        if which != cur_which:
            for (t, sec) in ((g1, 2), (gs2, 3), (sh2, 4)):
                k.dma("sp", t.ap, mv.ap[which:which + 1, sec, :].to_broadcast([128, D]), r=[mv], w=[t])
            cur_which = which
        blk = i // 4
        if blk != cur_blk:
            yc = ycs[nblk % 2]
            nblk += 1
            nt_ = min(512, T - blk * 512)
            for kc in range(KC):
                k.dma("sp" if kc % 2 == 0 else "act", yc.ap[:, kc, :nt_],
                      G.YC.ap[kc * 128:(kc + 1) * 128, blk * 512:blk * 512 + nt_], r=[G.YC], w=[yc])
            cur_blk = blk
        xt = xts[i % 2]
        hb = hbs[i % 2]
        h2 = h2s[i % 2]
        cb_ = cmb[i % 2]
        k.dma("sp", xt.ap, G.xres.ap[i * 128:(i + 1) * 128, :], r=[G.xres], w=[xt])
        off = (i % 4) * 128
        for dblk in range(4):
            ps = ps_y[dblk]
            for kc in range(KC):
                k.mm(ps.ap, yc.ap[:, kc, off:off + 128], wo.ap[:, kc, dblk * 512:(dblk + 1) * 512],
                     start=(kc == 0), stop=(kc == KC - 1), r=[yc, wo], w=[ps])
            sl = slice(dblk * 512, (dblk + 1) * 512)
            k.tt("dve", tmp.ap[:, sl], ps.ap, g1.ap[:, sl], ALU.mult, r=[ps, g1], w=[tmp])
            k.tt("pool", xt.ap[:, sl], xt.ap[:, sl], tmp.ap[:, sl], ALU.add, r=[xt, tmp], w=[xt])
        k.dma("sp", G.xres.ap[i * 128:(i + 1) * 128, :], xt.ap, r=[xt], w=[G.xres])
        norm_mod_tile(k, G, xt, gs2, sh2, hb, scr, ssq, rstd, tmp)
        for half in range(2):
            pt = pts[half]
            for j in range(8):
                kc = half * 8 + j
                k.op("pe", lambda e: e.transpose(out=pt.ap[:, j * 128:(j + 1) * 128],
                                                 in_=hb.ap[:, kc * 128:(kc + 1) * 128], identity=G.ident_b.ap),
                     r=[hb, G.ident_b], w=[pt])
            k.copy("act" if half == 0 else "dve", h2.ap[:, half * 8:(half + 1) * 8, :],
                   pt.ap.rearrange("p (a b) -> p a b", a=8), r=[pt], w=[h2])
        k.dma("sp", G.H2TM.ap[i * 128:(i + 1) * 128, :], hb.ap, r=[hb], w=[G.H2TM])
        for kc in range(KC):
            k.mm(ps_l.ap, h2.ap[:, kc, :], rw.ap[:, kc, :], start=(kc == 0), stop=(kc == KC - 1),
                 r=[h2, rw], w=[ps_l])
        k.tt("dve", lg.ap, ps_l.ap, rb.ap, ALU.add, r=[ps_l, rb], w=[lg])
        k.op("dve", lambda e: e.max(out=top8.ap, in_=lg.ap), r=[lg], w=[top8])
        k.ts("dve", msk.ap, lg.ap, top8.ap[:, 3:4], ALU.is_ge, r=[lg, top8], w=[msk])
        k.ts("dve", nmx.ap, top8.ap[:, 0:1], -1.0, ALU.mult, r=[top8], w=[nmx])
        k.act(ex.ap, lg.ap, AF.Exp, r=[lg, nmx], w=[ex], bias=nmx.ap)
        k.tt("dve", ex.ap, ex.ap, msk.ap, ALU.mult, r=[ex, msk], w=[ex])
        k.op("dve", lambda e: e.reduce_sum(out=ssum.ap, in_=ex.ap, axis=AX.X), r=[ex], w=[ssum])
        k.op("dve", lambda e: e.reciprocal(out=ssum.ap, in_=ssum.ap), r=[ssum], w=[ssum])
        k.ts("dve", cb_.ap, ex.ap, ssum.ap, ALU.mult, r=[ex, ssum], w=[cb_])
        k.dma("sp", G.COMB.ap[i * 128:(i + 1) * 128, :], cb_.ap, r=[cb_], w=[G.COMB])
        rk, lgc, t8c = rks[i % 2], lgs_[i % 2], t8s[i % 2]
        k.mm(ps_r.ap[:, 0:NE], ltri, msk.ap, start=True, stop=True, r=[G.ppt, msk], w=[ps_r])
        k.mm(ps_r.ap[:, NE:2 * NE], G.ones_f.ap, msk.ap, start=True, stop=True, r=[G.ones_f, msk], w=[ps_r])
        k.tt("dve", rk.ap, ps_r.ap[:, 0:NE], cnt.ap, ALU.add, r=[ps_r, cnt], w=[rk])
        k.tt("dve", cnt.ap, ps_r.ap[:, NE:2 * NE], cnt.ap, ALU.add, r=[ps_r, cnt], w=[cnt])
        k.copy("dve", lgc.ap, lg.ap, r=[lg], w=[lgc])
        k.copy("dve", t8c.ap, top8.ap, r=[top8], w=[t8c])
        k.dma("sp", G.RANKD.ap[i * 128:(i + 1) * 128, :], rk.ap, r=[rk], w=[G.RANKD])
        k.dma("sp", G.LGD.ap[i * 128:(i + 1) * 128, :], lgc.ap, r=[lgc], w=[G.LGD])
        k.dma("sp", G.TOPD.ap[i * 128:(i + 1) * 128, :], t8c.ap, r=[t8c], w=[G.TOPD])
    k.dma("sp", G.CNTD.ap, cnt.ap, r=[cnt], w=[G.CNTD])
    k.release(m)


class WStream:
    def __init__(self, k, bufs, srcs, q="pool", hold=1):
        self.k, self.bufs, self.srcs, self.q, self.hold = k, bufs, srcs, q, hold
        self.nxt = 0

    def get(self, n):
        nb = len(self.bufs)
        while self.nxt < len(self.srcs) and self.nxt <= n + nb - self.hold:
            j = self.nxt
            out_fn, in_ap, srcbuf = self.srcs[j]
            b = self.bufs[j % nb]
            self.k.dma(self.q, out_fn(b), in_ap, r=[srcbuf], w=[b])
            self.nxt += 1
        return self.bufs[n % nb]


def precast_outproj(k, G, l):
    wov = G.w_out.ap[l].rearrange("(kc p) n -> p kc n", p=128)
    dst = G.WOB[l].ap.rearrange("p (kc n) -> p kc n", kc=KC)
    for j in range(4):
        k.dma("pool", dst[:, :, j * 512:(j + 1) * 512], wov[:, :, j * 512:(j + 1) * 512], r=[G.w_out], w=[G.WOB[l]])
    k.dma("pool", G.RWB[l].ap.rearrange("p (kc e) -> p kc e", kc=KC),
          G.router_w.ap[l].rearrange("(kc p) e -> p kc e", p=128), r=[G.router_w], w=[G.RWB[l]])


def precast_list(k, G, l):
    w1v = G.exp_w1.ap[l].rearrange("e (kc p) n -> e p kc n", p=128)
    w2v = G.exp_w2.ap[l].rearrange("e (fc p) n -> e p fc n", p=128)
    out = []
    for e in range(NE):
        for cbk in range(4):
            out.append(lambda e=e, cbk=cbk: k.dma(
                "pool", G.W1B[l][cbk].ap[e * 128:(e + 1) * 128, :].rearrange("p (kc c) -> p kc c", kc=KC),
                w1v[e][:, :, cbk * 512:(cbk + 1) * 512], r=[G.exp_w1], w=[]))
        for hf in range(2):
            out.append(lambda e=e, hf=hf: k.dma(
                "pool", G.W2B[l][hf].ap[e * 128:(e + 1) * 128, :].rearrange("p (fc c) -> p fc c", fc=4),
                w2v[e][:, hf * 4:(hf + 1) * 4, :], r=[G.exp_w2], w=[]))
    return out


def pump(G, n):
    for _ in range(n):
        if G.bg:
            G.bg.pop(0)()


def phase_moe(k, G, l, need_ctx, last):
    m0 = k.mark()
    tiles = list(range(NT)) if need_ctx else list(range(2, NT))
    tb, ntt = tiles[0], len(tiles)
    NTL = ntt * 4 + NE
    NSL = NTL * 128
    tsl = slice(tb * 128, (tb + ntt) * 128)

    def tload(src, width, name):
        t = k.sb([128, ntt, width], F32, name)
        k.dma("sp", t.ap, src.ap[tsl, :].rearrange("(t p) e -> p t e", p=128), r=[src], w=[t])
        return t

    lgs = tload(G.LGD, NE, "lgs")
    cmbs = tload(G.COMB, NE, "cmbs")
    rks = tload(G.RANKD, NE, "rks")
    tops = tload(G.TOPD, 8, "tops")
    cnt = k.sb([128, NE], F32, "cntm")
    k.dma("sp", cnt.ap, G.CNTD.ap, r=[G.CNTD], w=[cnt])
    io_, _ = PP_COLS["iota"]
    iota = G.ppt.ap[:, io_:io_ + NTL]
    pidx = ppcol(G, "pidx")
    ntile = k.sb([128, NE], F32, "ntile")
    ones32 = k.sb([128, NE], F32, "ones32")
    incl = k.sb([128, NE], F32, "incl")
    base = k.sb([128, NE], F32, "base")
    k.op("dve", lambda e: e.memset(ntile.ap, 0.0), w=[ntile])
    k.op("dve", lambda e: e.memset(ones32.ap, 1.0), w=[ones32])
    for mth in range(ntt):
        k.stt(ntile.ap, cnt.ap, 128.0 * mth, ntile.ap, ALU.is_gt, ALU.add, r=[cnt, ntile], w=[ntile])
    k.op("dve", lambda e: e.tensor_tensor_scan(out=incl.ap, data0=ones32.ap, data1=ntile.ap, initial=0.0,
                                               op0=ALU.mult, op1=ALU.add), r=[ones32, ntile], w=[incl])
    k.tt("dve", base.ap, incl.ap, ntile.ap, ALU.subtract, r=[incl, ntile], w=[base])
    k.ts("dve", base.ap, base.ap, 128.0, ALU.mult, r=[base], w=[base])
    posf = k.sb([128, ntt, 4], F32, "posf")
    gat = k.sb([128, ntt, 4], F32, "gat")
    posi = k.sb([128, ntt, 4], I32, "posi")
    pf = k.sb([128, NE], F32, "pf")
    ohs = [k.sb([128, NE], F32, "oh") for _ in range(2)]
    sc1 = [k.sb([128, NE], F32, "sc1") for _ in range(2)]
    sc2 = [k.sb([128, NE], F32, "sc2") for _ in range(2)]
    n = 0
    for ti in range(ntt):
        k.tt("dve", pf.ap, rks.ap[:, ti, :], base.ap, ALU.add, r=[rks, base], w=[pf])
        for kk in range(4):
            oh, s1_, s2_ = ohs[n % 2], sc1[n % 2], sc2[n % 2]
            n += 1
            k.ts("dve", oh.ap, lgs.ap[:, ti, :], tops.ap[:, ti, kk:kk + 1], ALU.is_equal, r=[lgs, tops], w=[oh])
            k.tt("dve", s1_.ap, oh.ap, pf.ap, ALU.mult, r=[oh, pf], w=[s1_])
            k.op("dve", lambda e: e.reduce_sum(out=posf.ap[:, ti, kk:kk + 1], in_=s1_.ap, axis=AX.X),
                 r=[s1_], w=[posf])
            k.tt("dve", s2_.ap, oh.ap, cmbs.ap[:, ti, :], ALU.mult, r=[oh, cmbs], w=[s2_])
            k.op("dve", lambda e: e.reduce_sum(out=gat.ap[:, ti, kk:kk + 1], in_=s2_.ap, axis=AX.X),
                 r=[s2_], w=[gat])
    k.copy("dve", posi.ap, posf.ap, r=[posf], w=[posi])
    texp = k.sb([128, NTLMAX], F32, "texp")
    widf = k.sb([128, NTLMAX], F32, "widf")
    bidf = k.sb([128, NTLMAX], F32, "bidf")
    eq = k.sb([128, NTLMAX], F32, "eqt")
    widx = k.sb([128, NTLMAX], I32, "widx")
    bidx = k.sb([128, NTLMAX], I32, "bidx")
    k.op("dve", lambda e: e.memset(texp.ap, 0.0), w=[texp])
    k.op("dve", lambda e: e.memset(eq.ap, 0.0), w=[eq])
    for e_ in range(NE):
        k.stt(texp.ap[:, :NTL], iota, incl.ap[:, e_:e_ + 1], texp.ap[:, :NTL], ALU.is_ge, ALU.add,
              r=[G.ppt, incl, texp], w=[texp])
    k.ts("dve", widf.ap, texp.ap, 128.0, ALU.mult, pidx, ALU.add, r=[texp, G.ppt], w=[widf])
    k.tt("dve", eq.ap[:, 1:NTL], texp.ap[:, 1:NTL], texp.ap[:, 0:NTL - 1], ALU.is_equal, r=[texp], w=[eq])
    k.stt(widf.ap, eq.ap, BIG, widf.ap, ALU.mult, ALU.add, r=[eq, widf], w=[widf])
    k.copy("dve", widx.ap, widf.ap, r=[widf], w=[widx])
    k.ts("dve", bidf.ap, texp.ap, float(NE - 1), ALU.min, 128.0, ALU.mult, r=[texp], w=[bidf])
    k.ts("dve", bidf.ap, bidf.ap, pidx, ALU.add, r=[bidf, G.ppt], w=[bidf])
    k.copy("dve", bidx.ap, bidf.ap, r=[bidf], w=[bidx])
    ms = k.mark()
    xsc = [k.sb([128, D], BF16, "xsc") for _ in range(3)]
    for ti, i in enumerate(tiles):
        xt = xsc[ti % 3]
        k.dma("sp", xt.ap, G.H2TM.ap[i * 128:(i + 1) * 128, :], r=[G.H2TM], w=[xt])
        for kk in range(4):
            k.idma(out=G.XS.ap, in_=xt.ap, out_off=posi.ap[:, ti, kk:kk + 1], bounds=NSL - 1,
                   r=[xt, posi], w=[G.XS])
    k.release(ms)
    me = k.mark()
    w1 = [k.sb([128, KC, 512], BF16, "w1res") for _ in range(4)]
    w2 = [k.sb([128, 4, D], BF16, "w2res") for _ in range(2)]
    b1s = [k.sb([128, 16], F32, "b1s") for _ in range(2)]
    xs = [k.sb([128, D], BF16, "xs") for _ in range(3)]
    h2s = [k.sb([128, KC, 128], BF16, "h2s") for _ in range(2)]
    ats = [k.sb([128, 8, 128], BF16, "actT") for _ in range(2)]
    yts = [k.sb([128, D], F32, "yt") for _ in range(2)]
    gt = [k.sb([128, 128], F32, "g") for _ in range(2)]
    sg = [k.sb([128, 128], F32, "sig") for _ in range(2)]
    u1 = [k.sb([128, 128], F32, "u1") for _ in range(2)]
    pts = [k.ps([128, 1024], BF16, "pt") for _ in range(2)]
    ps_gu = [k.ps([128, 512], F32, "psgu") for _ in range(2)]
    ps_o = [k.ps([128, 512], F32, "pso") for _ in range(4)]
    NROW = NE * 128

    def load_x(j):
        xt = xs[j % 3]
        k.dma("sp", xt.ap, G.XS.ap[j * 128:(j + 1) * 128, :], r=[G.XS], w=[xt])

    def transp(j):
        xt, h2 = xs[j % 3], h2s[j % 2]
        for half in range(2):
            pt = pts[half]
            for jj in range(8):
                kc = half * 8 + jj
                k.op("pe", lambda e: e.transpose(out=pt.ap[:, jj * 128:(jj + 1) * 128],
                                                 in_=xt.ap[:, kc * 128:(kc + 1) * 128], identity=G.ident_b.ap),
                     r=[xt, G.ident_b], w=[pt])
            k.copy("act" if half == 0 else "dve", h2.ap[:, half * 8:(half + 1) * 8, :],
                   pt.ap.rearrange("p (a b) -> p a b", a=8), r=[pt], w=[h2])

    def gather_w1(j):
        for cbk in range(4):
            k.idma(out=w1[cbk].ap.rearrange("p kc c -> p (kc c)"), in_=G.W1B[l][cbk].ap, in_off=widx.ap[:, j:j + 1],
                   bounds=NROW - 1, r=[G.W1B[l][cbk], widx], w=[w1[cbk]])

    def gather_w2(j):
        for hf in range(2):
            k.idma(out=w2[hf].ap.rearrange("p fc c -> p (fc c)"), in_=G.W2B[l][hf].ap, in_off=widx.ap[:, j:j + 1],
                   bounds=NROW - 1, r=[G.W2B[l][hf], widx], w=[w2[hf]])

    def gather_b(j):
        bb = b1s[j % 2]
        k.idma(out=bb.ap, in_=G.b1t[l].ap, in_off=bidx.ap[:, j:j + 1], bounds=NROW - 1, r=[G.b1t[l], bidx], w=[bb])
        k.ts("dve", bb.ap[:, 8:16], bb.ap[:, 8:16], 1.0, ALU.add, r=[bb], w=[bb])

    fi = [0]

    def first(j):
        at, h2, bb = ats[j % 2], h2s[j % 2], b1s[j % 2]
        for cbk in range(4):
            wb = w1[cbk]
            for s in range(2):
                fc = cbk * 2 + s
                pgu = ps_gu[fi[0] % 2]
                g, sig, uu = gt[fi[0] % 2], sg[fi[0] % 2], u1[fi[0] % 2]
                fi[0] += 1
                wsl = wb.ap[:, :, s * 256:(s + 1) * 256].rearrange("p kc (f two) -> p kc f two", two=2)
                for kc in range(KC):
                    k.mm(pgu.ap[:, 0:128], wsl[:, kc, :, 0], h2.ap[:, kc, :], start=(kc == 0),
                         stop=(kc == KC - 1), r=[wb, h2], w=[pgu])
                for kc in range(KC):
                    k.mm(pgu.ap[:, 128:256], wsl[:, kc, :, 1], h2.ap[:, kc, :], start=(kc == 0),
                         stop=(kc == KC - 1), r=[wb, h2], w=[pgu])
                k.ts("dve", g.ap, pgu.ap[:, 0:128], bb.ap[:, fc:fc + 1], ALU.add, 7.0, ALU.min, r=[pgu, bb], w=[g])
                k.act(sig.ap, g.ap, AF.Sigmoid, r=[g], w=[sig], scale=1.702)
                k.ts("dve", uu.ap, pgu.ap[:, 128:256], bb.ap[:, 8 + fc:9 + fc], ALU.add, -6.0, ALU.max,
                     r=[pgu, bb], w=[uu])
                k.tt("dve", g.ap, g.ap, sig.ap, ALU.mult, r=[g, sig], w=[g])
                k.stt(at.ap[:, fc, :], uu.ap, 8.0, g.ap, ALU.min, ALU.mult, r=[uu, g], w=[at])

    oi = [0]

    def second(j):
        at, yt = ats[j % 2], yts[j % 2]
        for dblk in range(4):
            po = ps_o[oi[0] % 4]
            oi[0] += 1
            for fc in range(8):
                wb = w2[fc // 4]
                k.mm(po.ap, at.ap[:, fc, :], wb.ap[:, fc % 4, dblk * 512:(dblk + 1) * 512], start=(fc == 0),
                     stop=(fc == 7), r=[at, wb], w=[po])
            k.copy("act" if dblk % 2 == 0 else "dve", yt.ap[:, dblk * 512:(dblk + 1) * 512], po.ap, r=[po], w=[yt])
        k.dma("sp", G.YS.ap[j * 128:(j + 1) * 128, :], yt.ap, r=[yt], w=[G.YS])

    load_x(0)
    load_x(1)
    gather_w1(0)
    gather_w2(0)
    gather_b(0)
    transp(0)
    for j in range(NTL):
        if j + 2 < NTL:
            load_x(j + 2)
        if j + 1 < NTL:
            gather_b(j + 1)
            transp(j + 1)
        first(j)
        if j + 1 < NTL:
            gather_w1(j + 1)
        if j > 0:
            second(j - 1)
            gather_w2(j)
        pump(G, 2)
    second(NTL - 1)
    pump(G, len(G.bg))
    k.release(me)
    b2 = k.sb([NE, D], F32, "b2")
    k.dma("sp", b2.ap, G.exp_b2.ap[l], r=[G.exp_b2], w=[b2])
    mv = G.modv[l]
    ygs = [k.sb([128, D], F32, "yg") for _ in range(8)]
    accs = [k.sb([128, D], F32, "acc") for _ in range(2)]
    xts = [k.sb([128, D], F32, "xt") for _ in range(2)]
    combT = k.sb([NE, 128], F32, "combT")
    ps_o = [k.ps([128, 512], F32, "pso") for _ in range(4)]
    g2 = [None, None]
    if last:
        fg = k.sb([128, D], F32, "fgb")
        k.dma("sp", fg.ap, G.final_g.ap.to_broadcast([128, D]), r=[G.final_g], w=[fg])
        scr = k.sb([128, D], BF16, "scr")
        ssq = k.sb([128, 1], F32, "ssq")
        rstd = k.sb([128, 1], F32, "rstd")
        ots = [k.sb([128, D], F32, "ot") for _ in range(2)]
    oi = 0
    for ti, i in enumerate(tiles):
        which = 1 if i < 2 else 0
        if g2[which] is None:
            g2[which] = k.sb([128, D], F32, "g2b")
            k.dma("sp", g2[which].ap, mv.ap[which:which + 1, 5, :].to_broadcast([128, D]), r=[mv], w=[g2[which]])
        yg = [ygs[(ti % 2) * 4 + kk] for kk in range(4)]
        for kk in range(4):
            k.idma(out=yg[kk].ap, in_=G.YS.ap, in_off=posi.ap[:, ti, kk:kk + 1], bounds=NSL - 1,
                   r=[G.YS, posi], w=[yg[kk]])
        xt = xts[ti % 2]
        acc = accs[ti % 2]
        k.dma("sp", xt.ap, G.xres.ap[i * 128:(i + 1) * 128, :], r=[G.xres], w=[xt])
        pT = ps_o[oi % 4]
        oi += 1
        k.op("pe", lambda e: e.transpose(out=pT.ap[:NE, :128], in_=cmbs.ap[:, ti, :], identity=G.ident_f.ap),
             r=[cmbs, G.ident_f], w=[pT])
        k.copy("dve", combT.ap, pT.ap[:NE, :128], r=[pT], w=[combT])
        for dblk in range(4):
            po = ps_o[oi % 4]
            oi += 1
            sl = slice(dblk * 512, (dblk + 1) * 512)
            k.mm(po.ap, combT.ap, b2.ap[:, sl], start=True, stop=True, r=[combT, b2], w=[po])
            k.stt(acc.ap[:, sl], yg[0].ap[:, sl], gat.ap[:, ti, 0:1], po.ap, ALU.mult, ALU.add,
                  r=[yg[0], gat, po], w=[acc])
        for kk in range(1, 4):
            k.stt(acc.ap, yg[kk].ap, gat.ap[:, ti, kk:kk + 1], acc.ap, ALU.mult, ALU.add,
                  r=[yg[kk], gat, acc], w=[acc])
        k.tt("dve", acc.ap, acc.ap, g2[which].ap, ALU.mult, r=[acc, g2[which]], w=[acc])
        k.tt("dve", xt.ap, xt.ap, acc.ap, ALU.add, r=[xt, acc], w=[xt])
        if not last:
            k.dma("sp", G.xres.ap[i * 128:(i + 1) * 128, :], xt.ap, r=[xt], w=[G.xres])
        else:
            ot = ots[ti % 2]
            k.act(scr.ap, xt.ap, AF.Square, r=[xt], w=[scr, ssq], accum_out=ssq.ap)
            k.act(rstd.ap, ssq.ap, AF.Sqrt, r=[ssq, G.eps], w=[rstd], scale=1.0 / D, bias=G.eps.ap)
            k.op("dve", lambda e: e.reciprocal(out=rstd.ap, in_=rstd.ap), r=[rstd], w=[rstd])
            k.stt(ot.ap, xt.ap, rstd.ap, fg.ap, ALU.mult, ALU.mult, r=[xt, rstd, fg], w=[ot])
            k.dma("sp", G.out.ap[(i - 2) * 128:(i - 1) * 128, :], ot.ap, r=[ot], w=[G.out])
    k.release(m0)


_CACHE = {}


def kernel(**inputs):
    n = 8
    if "nc" not in _CACHE:
        _CACHE["nc"] = build_program(upto="all")[0]
    nc = _CACHE["nc"]
    maps = make_in_maps(inputs, list(range(n)))
    res = run_bass_kernel_spmd(nc, maps, core_ids=list(range(n)))
    out = np.stack([np.asarray(r["out"], dtype=np.float32) for r in res.results], axis=0)
    return out
```

```python
import math
import numpy as np
import concourse.bass as bass
import concourse.mybir as mybir
from concourse.bass_utils import run_bass_kernel_spmd

F32 = mybir.dt.float32
BF16 = mybir.dt.bfloat16
I32 = mybir.dt.int32
AF = mybir.ActivationFunctionType
ALU = mybir.AluOpType
AX = mybir.AxisListType

D = 2048
SEQ = 2048
CTX = 256
T = SEQ + CTX
NT = T // 128
DEPTH = 2
GW = 512
N_IN = 5120
NE = 32
DFF = 1024
EPS = 1e-6
KC = D // 128
NTLMAX = NT * 4 + NE
BIG = 1.0e6


class Buf:
    __slots__ = ("ap", "w", "r", "name")

    def __init__(self, ap, name=""):
        self.ap = ap
        self.w = None
        self.r = {}
        self.name = name

    def __getitem__(self, idx):
        return self.ap[idx]


class KB:
    RING = 8

    def __init__(self, nc):
        self.nc = nc
        self.eng = {"pe": nc.tensor, "act": nc.scalar, "dve": nc.vector, "pool": nc.gpsimd, "sp": nc.sync}
        self.csem = {}
        self.cnt = {}
        for e in ("pe", "act", "dve", "pool"):
            self.csem[e] = nc.alloc_semaphore("c_" + e)
            self.cnt[e] = 0
        self.pending = {e: False for e in self.cnt}
        self.rings = {}
        self.dcount = {}
        for q in ("sp", "act", "pool"):
            self.rings[q] = [nc.alloc_semaphore(f"r_{q}{i}") for i in range(self.RING)]
            self.dcount[q] = 0
        self.BGRING = 16
        self.bgring = [nc.alloc_semaphore(f"r_bg{i}") for i in range(self.BGRING)]
        self.bgcount = 0
        self.waited = {}
        self.n_ins = 0
        self.n_wait = 0
        self._uid = 0

    def sb(self, shape, dtype, name=None):
        self._uid += 1
        name = f"{name or 't'}_{self._uid}"
        return Buf(self.nc.alloc_sbuf_tensor(name, list(shape), dtype).ap(), name)

    def ps(self, shape, dtype=F32, name=None):
        self._uid += 1
        name = f"{name or 'p'}_{self._uid}"
        return Buf(self.nc.alloc_psum_tensor(name, list(shape), dtype).ap(), name)

    def dram(self, name, shape, dtype, kind="Internal"):
        return Buf(self.nc.dram_tensor(name, list(shape), dtype, kind=kind).ap(), name)

    def mark(self):
        nc = self.nc
        return (nc.sbuf_base, nc.sbuf_top, nc.psum_base, nc.psum_top)

    def release(self, m):
        self.barrier()
        nc = self.nc
        nc.sbuf_base, nc.sbuf_top, nc.psum_base, nc.psum_top = m

    def _wait(self, ename, ev):
        sem, val = ev
        key = (ename, sem.num if hasattr(sem, "num") else id(sem))
        if self.waited.get(key, 0) >= val:
            return
        self.eng[ename].wait_ge(sem, val)
        self.waited[key] = val
        self.n_wait += 1

    def _deps(self, ename, r, w):
        evs = []
        for b in r:
            if b.w is not None:
                evs.append(b.w)
        for b in w:
            if b.w is not None:
                evs.append(b.w)
            evs.extend(b.r.values())
        own = self.csem.get(ename)
        for ev in evs:
            if ename == "pe" and ev[0] is own:
                continue
            self._wait(ename, ev)

    def _record(self, ev, r, w):
        sid = id(ev[0])
        for b in r:
            cur = b.r.get(sid)
            if cur is None or cur[1] < ev[1]:
                b.r[sid] = ev
        for b in w:
            b.w = ev
            b.r = {}

    def op(self, ename, fn, r=(), w=(), signal=True):
        self._deps(ename, r, w)
        ins = fn(self.eng[ename])
        self.n_ins += 1
        if signal:
            self.cnt[ename] += 1
            ins.then_inc(self.csem[ename], 1)
            ev = (self.csem[ename], self.cnt[ename])
        else:
            ev = (self.csem[ename], self.cnt[ename] + 1)
        self._record(ev, r, w)
        return ins

    def dma(self, q, out, in_, r=(), w=(), **kw):
        self._deps(q, r, w)
        i = self.dcount[q]
        slot = i % self.RING
        sem = self.rings[q][slot]
        if i >= self.RING:
            self._wait(q, (sem, 16 * (i // self.RING)))
        ins = self.eng[q].dma_start(out=out, in_=in_, **kw)
        ins.then_inc(sem, 16)
        self.n_ins += 1
        self.dcount[q] = i + 1
        ev = (sem, 16 * (i // self.RING + 1))
        self._record(ev, r, w)
        return ev

    def bgdma(self, out, in_):
        i = self.bgcount
        slot = i % self.BGRING
        sem = self.bgring[slot]
        if i >= self.BGRING:
            self._wait("pool", (sem, 16 * (i // self.BGRING)))
        ins = self.nc.gpsimd.dma_start(out=out, in_=in_)
        ins.then_inc(sem, 16)
        self.n_ins += 1
        self.bgcount = i + 1

    def bg_wait(self, engines=("pool",)):
        for slot in range(self.BGRING):
            kc_ = (self.bgcount - slot + self.BGRING - 1) // self.BGRING
            if kc_ > 0:
                for e in engines:
                    self._wait(e, (self.bgring[slot], 16 * kc_))

    def bound_reg(self, val):
        if not hasattr(self, "_bregs"):
            self._bregs = {}
        if val not in self._bregs:
            reg = self.nc.alloc_register(mybir.EngineType.Pool, f"bnd{val}")
            self.nc.reg_mov(reg, val)
            self._bregs[val] = reg
        return self._bregs[val]

    def idma(self, out, in_, out_off=None, in_off=None, bounds=None, r=(), w=()):
        q = "pool"
        self._deps(q, r, w)
        i = self.dcount[q]
        slot = i % self.RING
        sem = self.rings[q][slot]
        if i >= self.RING:
            self._wait(q, (sem, 16 * (i // self.RING)))
        oo = bass.IndirectOffsetOnAxis(ap=out_off, axis=0) if out_off is not None else None
        io = bass.IndirectOffsetOnAxis(ap=in_off, axis=0) if in_off is not None else None
        ins = self.nc.gpsimd.indirect_dma_start(out=out, out_offset=oo, in_=in_, in_offset=io,
                                                bounds_check=self.bound_reg(bounds), oob_is_err=False)
        ins.then_inc(sem, 16)
        self.n_ins += 1
        self.dcount[q] = i + 1
        ev = (sem, 16 * (i // self.RING + 1))
        self._record(ev, r, w)
        return ev

    def all_events(self):
        evs = [(self.csem[e], self.cnt[e]) for e in self.cnt if self.cnt[e] > 0]
        for q in self.rings:
            n = self.dcount[q]
            for slot in range(self.RING):
                k = (n - slot + self.RING - 1) // self.RING
                if k > 0:
                    evs.append((self.rings[q][slot], 16 * k))
        return evs

    def barrier(self, engines=("pe", "act", "dve", "pool", "sp")):
        evs = self.all_events()
        for e in engines:
            for ev in evs:
                self._wait(e, ev)

    def mm(self, out_ap, lhsT, rhs, start, stop, r=(), w=(), signal=None, **kw):
        if signal is None:
            signal = True
        return self.op("pe", lambda e: e.matmul(out_ap, lhsT, rhs, start=start, stop=stop, **kw),
                       r=r, w=w, signal=signal)

    def act(self, out, in_, func, r=(), w=(), eng="act", **kw):
        return self.op(eng, lambda e: e.activation(out=out, in_=in_, func=func, **kw), r=r, w=w)

    def tt(self, eng, out, in0, in1, op, r=(), w=()):
        return self.op(eng, lambda e: e.tensor_tensor(out=out, in0=in0, in1=in1, op=op), r=r, w=w)

    def ts(self, eng, out, in0, s1, op0, s2=None, op1=None, r=(), w=(), **kw):
        if op1 is None:
            return self.op(eng, lambda e: e.tensor_scalar(out=out, in0=in0, scalar1=s1, scalar2=None,
                                                          op0=op0, **kw), r=r, w=w)
        return self.op(eng, lambda e: e.tensor_scalar(out=out, in0=in0, scalar1=s1, scalar2=s2,
                                                      op0=op0, op1=op1, **kw), r=r, w=w)

    def stt(self, out, in0, scalar, in1, op0, op1, r=(), w=(), **kw):
        return self.op("dve", lambda e: e.scalar_tensor_tensor(out=out, in0=in0, scalar=scalar, in1=in1,
                                                               op0=op0, op1=op1, **kw), r=r, w=w)

    def copy(self, eng, out, in_, r=(), w=()):
        if eng == "act":
            return self.op("act", lambda e: e.activation(out=out, in_=in_, func=AF.Copy), r=r, w=w)
        return self.op(eng, lambda e: e.tensor_copy(out=out, in_=in_), r=r, w=w)


def _pp_layout():
    cols = {}
    off = 0

    def add(name, n):
        nonlocal off
        cols[name] = (off, n)
        off += n

    add("cvec", 32)
    add("iota", NTLMAX)
    add("pidx", 1)
    add("ltri", 128)
    for l in range(DEPTH):
        add(f"conv_a_w{l}", 4 * 31)
        add(f"conv_a_b{l}", 4)
        add(f"ln_a_g{l}", 4)
        add(f"ln_a_b{l}", 4)
        add(f"diff_norm_g{l}", 1)
        add(f"conv_c_w{l}", 4 * 3)
        add(f"norm_c_g{l}", 4)
        add(f"lru_conv_w{l}", 2 * 4 * 4)
        add(f"lru_conv_b{l}", 8)
        add(f"lru_ba{l}", 8)
        add(f"lru_bx{l}", 8)
        add(f"lru_lam{l}", 8)
        add(f"norm_d_g{l}", 4)
    return cols, off


PP_COLS, NPP = _pp_layout()


def _pack_pp(inp, b):
    pp = np.zeros((128, NPP), np.float32)

    def put(name, arr):
        o, n = PP_COLS[name]
        pp[:, o:o + n] = np.ascontiguousarray(arr, dtype=np.float32).reshape(128, n)

    def chp(v):
        return np.asarray(v).reshape(4, 128).T

    cv = np.concatenate([np.asarray(inp["c"][b]).reshape(16, 128).T,
                         np.asarray(inp["c_ctx"]).reshape(16, 128).T], axis=1)
    put("cvec", cv)
    put("iota", np.tile(np.arange(NTLMAX, dtype=np.float32), (128, 1)))
    put("pidx", np.arange(128, dtype=np.float32).reshape(128, 1))
    put("ltri", (np.arange(128)[:, None] < np.arange(128)[None, :]).astype(np.float32))
    for l in range(DEPTH):
        put(f"conv_a_w{l}", np.asarray(inp["conv_a_w"][l]).reshape(31, 4, 128).transpose(2, 1, 0))
        put(f"conv_a_b{l}", chp(inp["conv_a_b"][l]))
        put(f"ln_a_g{l}", chp(inp["ln_a_g"][l]))
        put(f"ln_a_b{l}", chp(inp["ln_a_b"][l]))
        put(f"diff_norm_g{l}", np.asarray(inp["diff_norm_g"][l]).reshape(128, 1))
        put(f"conv_c_w{l}", np.asarray(inp["conv_c_w"][l]).reshape(3, 4, 128).transpose(2, 1, 0))
        put(f"norm_c_g{l}", chp(inp["norm_c_g"][l]))
        put(f"lru_conv_w{l}", np.asarray(inp["lru_conv_w"][l]).reshape(2, 4, 4, 128).transpose(3, 0, 2, 1))
        for nm in ("lru_conv_b", "lru_ba", "lru_bx", "lru_lam"):
            put(f"{nm}{l}", np.asarray(inp[nm][l]).reshape(2, 4, 128).transpose(2, 0, 1))
        put(f"norm_d_g{l}", chp(inp["norm_d_g"][l]))
    return pp


def _pack_b1t(inp, l):
    b1 = np.asarray(inp["exp_b1"][l], dtype=np.float32)
    g = b1[:, 0::2].reshape(NE, 8, 128).transpose(0, 2, 1).reshape(NE * 128, 8)
    u = b1[:, 1::2].reshape(NE, 8, 128).transpose(0, 2, 1).reshape(NE * 128, 8)
    return np.ascontiguousarray(np.concatenate([g, u], axis=1))


def _rope_tables():
    GRID_W = 64
    rows = SEQ // GRID_W
    row = np.repeat(np.arange(rows, dtype=np.float32), GRID_W)
    col = np.tile(np.arange(GRID_W, dtype=np.float32), rows)
    half = 32
    inv = (np.float32(10000.0) ** (-np.arange(0, half, 2, dtype=np.float32) / np.float32(half))).astype(np.float32)
    ar = row[:, None] * inv
    ac = col[:, None] * inv
    ang = np.concatenate([ar, ar, ac, ac], axis=-1)
    cos = np.cos(ang).astype(np.float32)
    sin = np.sin(ang).astype(np.float32)
    sgn = np.concatenate([-np.ones(16), np.ones(16), -np.ones(16), np.ones(16)]).astype(np.float32)
    sin = sin * sgn[None, :]
    tab = np.zeros((2, 128, T), np.float32)
    tab[0, :, :CTX] = 1.0
    tab[0, 0:64, CTX:] = cos.T
    tab[0, 64:128, CTX:] = cos.T
    tab[1, 0:64, CTX:] = sin.T
    tab[1, 64:128, CTX:] = sin.T
    return tab


class Prog:
    pass


def tok_chunks(t0=0, t1=T, step=512):
    out = []
    t = t0
    while t < t1:
        n = min(step, t1 - t)
        out.append((t, n))
        t += n
    return out


def declare_io(k, G):
    nc = k.nc

    def inp(name, shape, dt=F32):
        return k.dram(name, shape, dt, kind="ExternalInput")

    G.x = inp("x", [SEQ, D])
    G.ctx = inp("ctx", [CTX, D])
    G.pp = inp("pp", [128, NPP])
    G.rope = inp("rope", [2, 128, T])
    G.ada_w = inp("ada_w", [DEPTH, D, 6 * D])
    G.ada_b = inp("ada_b", [DEPTH, 6 * D])
    G.norm1_g = inp("norm1_g", [DEPTH, D])
    G.norm2_g = inp("norm2_g", [DEPTH, D])
    G.final_g = inp("final_g", [1, D])
    G.w_in = inp("w_in", [DEPTH, D, N_IN])
    G.w_out = inp("w_out", [DEPTH, D, D])
    G.diff_lambda = inp("diff_lambda", [DEPTH, 256])
    G.lru_wa = inp("lru_wa", [DEPTH, 2, 8, 64, 64])
    G.lru_wx = inp("lru_wx", [DEPTH, 2, 8, 64, 64])
    G.router_w = inp("router_w", [DEPTH, D, NE])
    G.router_b = inp("router_b", [DEPTH, NE])
    if G.with_moe:
        G.exp_w1 = inp("exp_w1", [DEPTH, NE, D, 2 * DFF])
        G.exp_w2 = inp("exp_w2", [DEPTH, NE, DFF, D])
        G.exp_b2 = inp("exp_b2", [DEPTH, NE, D])
        G.b1t = [inp(f"b1t{l}", [NE * 128, 16]) for l in range(DEPTH)]
    G.out = k.dram("out", [SEQ, D], F32, kind="ExternalOutput")
    G.xres = k.dram("xres", [T, D], F32)
    G.modv = [k.dram(f"modv{l}", [2, 6, D], F32) for l in range(DEPTH)]
    G.UF = k.dram("UF", [3584, T], F32)
    G.QK = k.dram("QK", [1024, T], BF16)
    G.V = k.dram("Vtm", [T, 512], BF16)
    G.YC = k.dram("YC", [D, T], BF16)
    G.H2TM = k.dram("H2TM", [T, D], BF16)
    G.COMB = k.dram("COMB", [T, NE], F32)
    G.LGD = k.dram("LGD", [T, NE], F32)
    G.TOPD = k.dram("TOPD", [T, 8], F32)
    G.RANKD = k.dram("RANKD", [T, NE], F32)
    G.CNTD = k.dram("CNTD", [128, NE], F32)
    G.XS = k.dram("XS", [NTLMAX * 128, D], BF16)
    G.YS = k.dram("YS", [NTLMAX * 128, D], F32)
    G.WOB = [k.dram(f"wob{l}", [128, KC * D], BF16) for l in range(DEPTH)]
    G.RWB = [k.dram(f"rwb{l}", [128, KC * NE], BF16) for l in range(DEPTH)]
    G.WIB = [k.dram(f"wib{l}", [10, 128, KC * 512], BF16) for l in range(DEPTH)]
    if G.with_moe:
        G.W1B = [[k.dram(f"w1b{l}_{c}", [NE * 128, KC * 512], BF16) for c in range(4)] for l in range(DEPTH)]
        G.W2B = [[k.dram(f"w2b{l}_{h}", [NE * 128, 4 * D], BF16) for h in range(2)] for l in range(DEPTH)]


def phase_consts(k, G):
    G.ppt = k.sb([128, NPP], F32, "pp")
    k.dma("sp", G.ppt.ap, G.pp.ap, r=[G.pp], w=[G.ppt])
    G.ident_f = k.sb([128, 128], F32, "identf")
    G.ident_b = k.sb([128, 128], BF16, "identb")
    G.ones_f = k.sb([128, 128], F32, "onesf")
    k.op("pool", lambda e: e.memset(G.ident_f.ap, 0.0), w=[G.ident_f])
    k.op("pool", lambda e: e.memset(G.ones_f.ap, 1.0), w=[G.ones_f])
    k.op("pool", lambda e: e.affine_select(out=G.ident_f.ap, in_=G.ones_f.ap, pattern=[[-1, 128]],
                                           compare_op=ALU.is_equal, fill=0.0, base=0, channel_multiplier=1),
         r=[G.ones_f], w=[G.ident_f])
    k.copy("dve", G.ident_b.ap, G.ident_f.ap, r=[G.ident_f], w=[G.ident_b])
    G.eps = k.sb([128, 1], F32, "eps")
    k.op("pool", lambda e: e.memset(G.eps.ap, EPS), w=[G.eps])
    k.dma("sp", G.xres.ap[0:CTX, :], G.ctx.ap, r=[G.ctx], w=[G.xres])
    k.dma("sp", G.xres.ap[CTX:T, :], G.x.ap, r=[G.x], w=[G.xres])


def ppcol(G, name, i=0, n=1):
    o, _ = PP_COLS[name]
    return G.ppt.ap[:, o + i:o + i + n]


def phase_mod(k, G, l):
    m = k.mark()
    s = k.sb([128, 16, 2], F32, "silu_c")
    o, _ = PP_COLS["cvec"]
    k.act(s.ap[:, :, 0], G.ppt.ap[:, o:o + 16], AF.Silu, r=[G.ppt], w=[s])
    k.act(s.ap[:, :, 1], G.ppt.ap[:, o + 16:o + 32], AF.Silu, r=[G.ppt], w=[s])
    mod = k.sb([2, 6 * D], F32, "mod")
    adab = k.sb([2, 6 * D], F32, "adab")
    k.dma("sp", adab.ap, G.ada_b.ap[l:l + 1, :].to_broadcast([2, 6 * D]), r=[G.ada_b], w=[adab])
    wbufs = [k.sb([128, KC, 512], F32, f"adaw{i}") for i in range(2)]
    pss = [k.ps([2, 512], F32, f"modps{i}") for i in range(2)]
    awv = G.ada_w.ap[l].rearrange("(kc p) n -> p kc n", p=128)
    for cb in range(24):
        wb = wbufs[cb % 2]
        ps = pss[cb % 2]
        k.dma("sp" if cb % 2 == 0 else "act", wb.ap, awv[:, :, cb * 512:(cb + 1) * 512], r=[G.ada_w], w=[wb])
        for kc in range(KC):
            k.mm(ps.ap, s.ap[:, kc, :], wb.ap[:, kc, :], start=(kc == 0), stop=(kc == KC - 1),
                 r=[s, wb], w=[ps])
        k.tt("dve", mod.ap[:, cb * 512:(cb + 1) * 512], ps.ap, adab.ap[:, cb * 512:(cb + 1) * 512], ALU.add,
             r=[ps, adab], w=[mod])
    n1 = k.sb([2, D], F32, "n1")
    n2 = k.sb([2, D], F32, "n2")
    k.dma("sp", n1.ap, G.norm1_g.ap[l:l + 1, :].to_broadcast([2, D]), r=[G.norm1_g], w=[n1])
    k.dma("sp", n2.ap, G.norm2_g.ap[l:l + 1, :].to_broadcast([2, D]), r=[G.norm2_g], w=[n2])
    gs1 = k.sb([2, D], F32, "gs1")
    gs2 = k.sb([2, D], F32, "gs2")
    k.stt(gs1.ap, mod.ap[:, D:2 * D], 1.0, n1.ap, ALU.add, ALU.mult, r=[mod, n1], w=[gs1])
    k.stt(gs2.ap, mod.ap[:, 4 * D:5 * D], 1.0, n2.ap, ALU.add, ALU.mult, r=[mod, n2], w=[gs2])
    mv = G.modv[l]
    k.dma("sp", mv.ap[:, 0, :], gs1.ap, r=[gs1], w=[mv])
    k.dma("sp", mv.ap[:, 1, :], mod.ap[:, 0:D], r=[mod], w=[mv])
    k.dma("sp", mv.ap[:, 2, :], mod.ap[:, 2 * D:3 * D], r=[mod], w=[mv])
    k.dma("sp", mv.ap[:, 3, :], gs2.ap, r=[gs2], w=[mv])
    k.dma("sp", mv.ap[:, 4, :], mod.ap[:, 3 * D:4 * D], r=[mod], w=[mv])
    k.dma("sp", mv.ap[:, 5, :], mod.ap[:, 5 * D:6 * D], r=[mod], w=[mv])
    k.release(m)


def load_bcast(k, G, l, sec, which, name):
    t = k.sb([128, D], F32, name)
    mv = G.modv[l]
    k.dma("sp", t.ap, mv.ap[which:which + 1, sec, :].to_broadcast([128, D]), r=[mv], w=[t])
    return t


def norm_mod_tile(k, G, xt, gs, sh, hb, scr, ssq, rstd, tmp, addeng="pool"):
    k.act(scr.ap, xt.ap, AF.Square, r=[xt], w=[scr, ssq], accum_out=ssq.ap)
    k.act(rstd.ap, ssq.ap, AF.Sqrt, r=[ssq, G.eps], w=[rstd], scale=1.0 / D, bias=G.eps.ap)
    k.op("dve", lambda e: e.reciprocal(out=rstd.ap, in_=rstd.ap), r=[rstd], w=[rstd])
    k.stt(tmp.ap, xt.ap, rstd.ap, gs.ap, ALU.mult, ALU.mult, r=[xt, rstd, gs], w=[tmp])
    k.tt(addeng, hb.ap, tmp.ap, sh.ap, ALU.add, r=[tmp, sh], w=[hb])


def phase_inproj(k, G, l):
    m = k.mark()
    hT_t = k.nc.alloc_sbuf_tensor(f"hT{l}", [128, KC, T], BF16).ap()
    hT = [Buf(hT_t, f"hT{i}") for i in range(NT)]
    m1 = k.mark()
    gs = [load_bcast(k, G, l, 0, w, "gs1") for w in range(2)]
    sh = [load_bcast(k, G, l, 1, w, "sh1") for w in range(2)]
    xts = [k.sb([128, D], F32, "xt") for _ in range(2)]
    tmp = k.sb([128, D], F32, "tmp")
    scr = k.sb([128, D], BF16, "scr")
    hbs = [k.sb([128, D], BF16, "hb") for _ in range(2)]
    ssq = k.sb([128, 1], F32, "ssq")
    rstd = k.sb([128, 1], F32, "rstd")
    pts = [k.ps([128, 1024], BF16, "pt") for _ in range(2)]
    for i in range(NT):
        which = 1 if i < 2 else 0
        xt = xts[i % 2]
        hb = hbs[i % 2]
        k.dma("sp", xt.ap, G.xres.ap[i * 128:(i + 1) * 128, :], r=[G.xres], w=[xt])
        norm_mod_tile(k, G, xt, gs[which], sh[which], hb, scr, ssq, rstd, tmp, addeng="dve")
        for half in range(2):
            pt = pts[half]
            for j in range(8):
                kc = half * 8 + j
                k.op("pe", lambda e: e.transpose(out=pt.ap[:, j * 128:(j + 1) * 128],
                                                 in_=hb.ap[:, kc * 128:(kc + 1) * 128], identity=G.ident_b.ap),
                     r=[hb, G.ident_b], w=[pt])
            k.copy("act" if half == 0 else "dve",
                   hT_t[:, half * 8:(half + 1) * 8, i * 128:(i + 1) * 128],
                   pt.ap.rearrange("p (a b) -> p a b", a=8), r=[pt], w=[hT[i]])
    k.release(m1)
    cos2 = k.sb([128, T], F32, "cos2")
    sin2 = k.sb([128, T], F32, "sin2")
    k.dma("sp", cos2.ap, G.rope.ap[0], r=[G.rope], w=[cos2])
    k.dma("sp", sin2.ap, G.rope.ap[1], r=[G.rope], w=[sin2])
    wbufs = [k.sb([128, KC, 512], BF16, "wblk") for _ in range(2)]
    wperm = k.sb([128, KC, 512], BF16, "wperm")
    pss = [k.ps([128, 512], F32, "ps") for _ in range(6)]
    stage_f = [k.sb([128, T], F32, "stf") for _ in range(2)]
    stage_b = [k.sb([128, T], BF16, "stb") for _ in range(2)]
    t1 = k.sb([128, 512], F32, "ropet1")
    t2 = k.sb([128, 512], F32, "ropet2")
    vst = [k.sb([128, 512], BF16, "vst") for _ in range(2)]
    wv = G.w_in.ap[l].rearrange("(kc p) n -> p kc n", p=128)
    chunks = tok_chunks()
    psi = [0]
    evi = [0]

    def nextps():
        p = pss[psi[0] % len(pss)]
        psi[0] += 1
        return p

    def proj_fm(wb, mblk, n0, nn):
        ps = nextps()
        tiles = [hT[i] for i in range(n0 // 128, (n0 + nn) // 128)]
        for kc in range(KC):
            k.mm(ps.ap[:, :nn], wb.ap[:, kc, mblk * 128:(mblk + 1) * 128], hT_t[:, kc, n0:n0 + nn],
                 start=(kc == 0), stop=(kc == KC - 1), r=[wb] + tiles, w=[ps])
        return ps

    uf_row = 0
    nst = 0
    def load_w(cb):
        k.dma("sp", wbufs[cb % 2].ap, G.WIB[l].ap[cb].rearrange("p (kc c) -> p kc c", kc=KC),
              r=[G.WIB[l]], w=[wbufs[cb % 2]])

    load_w(0)
    for cb in range(10):
        wb = wbufs[cb % 2]
        if cb + 1 < 10:
            load_w(cb + 1)
        if cb in (2, 3):
            src = wb.ap.rearrange("p kc (g s j) -> p (kc g) s j", s=2, j=16)
            dst = wperm.ap.rearrange("p kc (g s j) -> p (kc g) s j", s=2, j=16)
            k.copy("dve", dst[:, :, 0, :], src[:, :, 1, :], r=[wb], w=[wperm])
            k.copy("dve", dst[:, :, 1, :], src[:, :, 0, :], r=[wb], w=[wperm])
            for mblk in range(4):
                st = stage_b[nst % 2]
                nst += 1
                for (n0, nn) in chunks:
                    pa = proj_fm(wb, mblk, n0, nn)
                    pb = proj_fm(wperm, mblk, n0, nn)
                    k.tt("dve", t1.ap[:, :nn], pa.ap[:, :nn], cos2.ap[:, n0:n0 + nn], ALU.mult,
                         r=[pa, cos2], w=[t1])
                    k.tt("dve", t2.ap[:, :nn], pb.ap[:, :nn], sin2.ap[:, n0:n0 + nn], ALU.mult,
                         r=[pb, sin2], w=[t2])
                    k.tt("dve", st.ap[:, n0:n0 + nn], t1.ap[:, :nn], t2.ap[:, :nn], ALU.add,
                         r=[t1, t2], w=[st])
                row = (cb - 2) * 512 + mblk * 128
                k.dma("sp", G.QK.ap[row:row + 128, :], st.ap, r=[st], w=[G.QK])
        elif cb == 4:
            for i in range(NT):
                ps = nextps()
                for kc in range(KC):
                    k.mm(ps.ap, hT_t[:, kc, i * 128:(i + 1) * 128], wb.ap[:, kc, :],
                         start=(kc == 0), stop=(kc == KC - 1), r=[wb, hT[i]], w=[ps])
                vs = vst[i % 2]
                k.copy("act" if i % 2 == 0 else "dve", vs.ap, ps.ap, r=[ps], w=[vs])
                k.dma("sp", G.V.ap[i * 128:(i + 1) * 128, :], vs.ap, r=[vs], w=[G.V])
        else:
            for mblk in range(4):
                st = stage_f[nst % 2]
                nst += 1
                for (n0, nn) in chunks:
                    ps = proj_fm(wb, mblk, n0, nn)
                    k.copy("act" if evi[0] % 2 == 0 else "dve", st.ap[:, n0:n0 + nn], ps.ap[:, :nn],
                           r=[ps], w=[st])
                    evi[0] += 1
                k.dma("sp", G.UF.ap[uf_row:uf_row + 128, :], st.ap, r=[st], w=[G.UF])
                uf_row += 128
    assert uf_row == 3584
    k.release(m)


def build_program(upto="all", dbg=(), with_moe=True):
    nc = bass.Bass("TRN2", target_bir_lowering=False)
    k = KB(nc)
    G = Prog()
    G.with_moe = with_moe
    declare_io(k, G)
    phase_consts(k, G)
    G.bg = []
    for l in range(DEPTH):
        precast_win(k, G, l)
        precast_outproj(k, G, l)
    done = False
    for l in range(DEPTH):
        phase_mod(k, G, l)
    if with_moe:
        G.bg = precast_list(k, G, 0)
        pump(G, len(G.bg))
    if upto == "mod":
        done = True
    for l in range(DEPTH):
        if done:
            break
        if l > 0 and G.with_moe:
            G.bg = precast_list(k, G, l)
            pump(G, len(G.bg))
        phase_inproj(k, G, l)
        if upto == f"inproj{l}":
            break
        phase_mixers(k, G, l, need_ctx=(l < DEPTH - 1))
        if upto == f"mix{l}":
            break
        phase_outproj(k, G, l, need_ctx=(l < DEPTH - 1))
        if upto == f"outproj{l}":
            break
        phase_moe(k, G, l, need_ctx=(l < DEPTH - 1), last=(l == DEPTH - 1))
        if upto == f"moe{l}":
            break
    for name in dbg:
        src = getattr(G, name) if not name.startswith("modv") else G.modv[int(name[4:])]
        o = k.dram("dbg_" + name, list(src.ap.shape), src.ap.dtype, kind="ExternalOutput")
        k.dma("sp", o.ap, src.ap, r=[src], w=[o])
    k.barrier(engines=("sp",))
    G.k = k
    return nc, G


def make_in_maps(inp, cores):
    rope = _rope_tables()
    shared = {
        "rope": rope,
        "ada_w": np.ascontiguousarray(inp["ada_w"], dtype=np.float32),
        "ada_b": np.ascontiguousarray(inp["ada_b"], dtype=np.float32),
        "norm1_g": np.ascontiguousarray(inp["norm1_g"], dtype=np.float32),
        "norm2_g": np.ascontiguousarray(inp["norm2_g"], dtype=np.float32),
        "final_g": np.ascontiguousarray(inp["final_g"], dtype=np.float32).reshape(1, D),
        "w_in": np.ascontiguousarray(inp["w_in"], dtype=np.float32),
        "w_out": np.ascontiguousarray(inp["w_out"], dtype=np.float32),
        "diff_lambda": np.ascontiguousarray(inp["diff_lambda"], dtype=np.float32).reshape(DEPTH, 256),
        "lru_wa": np.ascontiguousarray(inp["lru_wa"], dtype=np.float32),
        "lru_wx": np.ascontiguousarray(inp["lru_wx"], dtype=np.float32),
        "router_w": np.ascontiguousarray(inp["router_w"], dtype=np.float32),
        "router_b": np.ascontiguousarray(inp["router_b"], dtype=np.float32),
        "exp_w1": np.ascontiguousarray(inp["exp_w1"], dtype=np.float32),
        "exp_w2": np.ascontiguousarray(inp["exp_w2"], dtype=np.float32),
        "exp_b2": np.ascontiguousarray(inp["exp_b2"], dtype=np.float32),
    }
    for l in range(DEPTH):
        shared[f"b1t{l}"] = _pack_b1t(inp, l)
    maps = []
    for b in cores:
        mp = dict(shared)
        mp["x"] = np.ascontiguousarray(inp["x"][b], dtype=np.float32)
        mp["ctx"] = np.ascontiguousarray(inp["ctx"][b], dtype=np.float32)
        mp["pp"] = _pack_pp(inp, b)
        maps.append(mp)
    return maps


def useg(gap, step=512):
    out = [(0, CTX, 0)]
    for (t0, n) in tok_chunks(CTX, T, step):
        out.append((t0, n, t0 + gap))
    return out


def load_gapped(k, G, dst, dst_off_ctx, dst_off_lat, src_buf, row0, q="sp"):
    k.dma(q, dst.ap[:, dst_off_ctx:dst_off_ctx + CTX], src_buf.ap[row0:row0 + 128, 0:CTX], r=[src_buf], w=[dst])
    k.dma(q, dst.ap[:, dst_off_lat:dst_off_lat + SEQ], src_buf.ap[row0:row0 + 128, CTX:T], r=[src_buf], w=[dst])


def finish_norm(k, G, ys, gap, kind, l, yc_row0, gname, bname=None):
    m = k.mark()
    ps_sq = [k.ps([128, 512], F32, "pssq") for _ in range(2)]
    ps_su = [k.ps([128, 512], F32, "pssu") for _ in range(2)] if kind == "ln_silu" else None
    sq = [k.sb([128, 512], F32, "sq") for _ in range(2)]
    rstd = [k.sb([128, 512], F32, "rstd") for _ in range(2)]
    mean = [k.sb([128, 512], F32, "mean") for _ in range(2)]
    msq = k.sb([128, 512], F32, "msq")
    t1 = [k.sb([128, 512], F32, "t1") for _ in range(2)]
    stage = [k.sb([128, T], BF16, "ystage") for _ in range(4)]
    for ci, (t0, n, u0) in enumerate(useg(gap)):
        pq = ps_sq[ci % 2]
        rs = rstd[ci % 2]
        mn = mean[ci % 2]
        for c in range(4):
            s = sq[c % 2]
            k.act(s.ap[:, :n], ys[c].ap[:, u0:u0 + n], AF.Square, r=[ys[c]], w=[s])
            k.mm(pq.ap[:, :n], G.ones_f.ap, s.ap[:, :n], start=(c == 0), stop=(c == 3), r=[G.ones_f, s], w=[pq])
        if kind == "ln_silu":
            pu = ps_su[ci % 2]
            for c in range(4):
                k.mm(pu.ap[:, :n], G.ones_f.ap, ys[c].ap[:, u0:u0 + n], start=(c == 0), stop=(c == 3),
                     r=[G.ones_f, ys[c]], w=[pu])
            k.ts("dve", mn.ap[:, :n], pu.ap[:, :n], 1.0 / GW, ALU.mult, r=[pu], w=[mn])
            k.tt("dve", msq.ap[:, :n], mn.ap[:, :n], mn.ap[:, :n], ALU.mult, r=[mn], w=[msq])
            k.stt(rs.ap[:, :n], pq.ap[:, :n], 1.0 / GW, msq.ap[:, :n], ALU.mult, ALU.subtract, r=[pq, msq], w=[rs])
            k.act(rs.ap[:, :n], rs.ap[:, :n], AF.Sqrt, r=[rs, G.eps], w=[rs], bias=G.eps.ap)
        else:
            k.act(rs.ap[:, :n], pq.ap[:, :n], AF.Sqrt, r=[pq, G.eps], w=[rs], scale=1.0 / GW, bias=G.eps.ap)
        k.op("dve", lambda e: e.reciprocal(out=rs.ap[:, :n], in_=rs.ap[:, :n]), r=[rs], w=[rs])
        for c in range(4):
            gcol = ppcol(G, f"{gname}{l}", c)
            if kind == "ln_silu":
                bcol = ppcol(G, f"{bname}{l}", c)
                t = t1[c % 2]
                k.tt("dve", t.ap[:, :n], ys[c].ap[:, u0:u0 + n], mn.ap[:, :n], ALU.subtract, r=[ys[c], mn], w=[t])
                k.tt("pool", t.ap[:, :n], t.ap[:, :n], rs.ap[:, :n], ALU.mult, r=[t, rs], w=[t])
                k.act(stage[c].ap[:, t0:t0 + n], t.ap[:, :n], AF.Silu, r=[t, G.ppt], w=[stage[c]],
                      scale=gcol, bias=bcol)
            else:
                k.stt(stage[c].ap[:, t0:t0 + n], ys[c].ap[:, u0:u0 + n], gcol, rs.ap[:, :n], ALU.mult, ALU.mult,
                      r=[ys[c], rs, G.ppt], w=[stage[c]])
    for c in range(4):
        k.dma("sp", G.YC.ap[yc_row0 + c * 128:yc_row0 + (c + 1) * 128, :], stage[c].ap, r=[stage[c]], w=[G.YC])
    k.release(m)


def mixer_conformer(k, G, l):
    m = k.mark()
    GAP = 30
    NU = T + GAP
    ZW = NU + 30
    ys = [k.sb([128, NU], F32, "convA") for _ in range(4)]
    zps = [k.sb([128, ZW], F32, "zpA") for _ in range(2)]
    vals = [k.sb([128, T], F32, "valA") for _ in range(2)]
    gates = [k.sb([128, T], F32, "gateA") for _ in range(2)]
    for zp in zps:
        k.op("pool", lambda e: e.memset(zp.ap, 0.0), w=[zp])
    wo, _ = PP_COLS[f"conv_a_w{l}"]
    for c in range(4):
        zp = zps[c % 2]
        va = vals[c % 2]
        ga = gates[c % 2]
        k.dma("sp", va.ap, G.UF.ap[c * 128:(c + 1) * 128, :], r=[G.UF], w=[va])
        k.dma("act", ga.ap, G.UF.ap[512 + c * 128:512 + (c + 1) * 128, :], r=[G.UF], w=[ga])
        k.act(ga.ap, ga.ap, AF.Sigmoid, r=[ga], w=[ga])
        k.tt("pool", zp.ap[:, 15:15 + CTX], va.ap[:, 0:CTX], ga.ap[:, 0:CTX], ALU.mult, r=[va, ga], w=[zp])
        k.tt("pool", zp.ap[:, 45 + CTX:45 + CTX + SEQ], va.ap[:, CTX:T], ga.ap[:, CTX:T], ALU.mult,
             r=[va, ga], w=[zp])
        y = ys[c]
        wcol = lambda kk: G.ppt.ap[:, wo + c * 31 + kk:wo + c * 31 + kk + 1]
        k.ts("dve", y.ap, zp.ap[:, 0:NU], wcol(0), ALU.mult, ppcol(G, f"conv_a_b{l}", c), ALU.add,
             r=[zp, G.ppt], w=[y])
        for kk in range(1, 31):
            k.stt(y.ap, zp.ap[:, kk:kk + NU], wcol(kk), y.ap, ALU.mult, ALU.add, r=[zp, y, G.ppt], w=[y])
    finish_norm(k, G, ys, GAP, "ln_silu", l, 0, "ln_a_g", "ln_a_b")
    k.release(m)


def mixer_sconv(k, G, l):
    m = k.mark()
    GAP = 2
    NU = T + GAP
    ZW = NU + 2
    ys = [k.sb([128, NU], F32, "convC") for _ in range(4)]
    zps = [k.sb([128, ZW], F32, "zpC") for _ in range(2)]
    bgs = [k.sb([128, NU], F32, "bgC") for _ in range(2)]
    cgs = [k.sb([128, T], F32, "cgC") for _ in range(2)]
    vs = [k.sb([128, T], F32, "vC") for _ in range(2)]
    for zp in zps:
        k.op("pool", lambda e: e.memset(zp.ap, 0.0), w=[zp])
    for bg in bgs:
        k.op("pool", lambda e: e.memset(bg.ap, 0.0), w=[bg])
    wo, _ = PP_COLS[f"conv_c_w{l}"]
    for c in range(4):
        zp, bg, cg, v = zps[c % 2], bgs[c % 2], cgs[c % 2], vs[c % 2]
        load_gapped(k, G, bg, 0, CTX + GAP, G.UF, 1024 + c * 128, q="sp")
        k.dma("act", cg.ap, G.UF.ap[1536 + c * 128:1536 + (c + 1) * 128, :], r=[G.UF], w=[cg])
        k.dma("sp", v.ap, G.UF.ap[2048 + c * 128:2048 + (c + 1) * 128, :], r=[G.UF], w=[v])
        k.tt("pool", zp.ap[:, 1:1 + CTX], cg.ap[:, 0:CTX], v.ap[:, 0:CTX], ALU.mult, r=[cg, v], w=[zp])
        k.tt("pool", zp.ap[:, 3 + CTX:3 + CTX + SEQ], cg.ap[:, CTX:T], v.ap[:, CTX:T], ALU.mult, r=[cg, v], w=[zp])
        y = ys[c]
        wcol = lambda kk: G.ppt.ap[:, wo + c * 3 + kk:wo + c * 3 + kk + 1]
        k.ts("dve", y.ap, zp.ap[:, 0:NU], wcol(0), ALU.mult, r=[zp, G.ppt], w=[y])
        for kk in range(1, 3):
            k.stt(y.ap, zp.ap[:, kk:kk + NU], wcol(kk), y.ap, ALU.mult, ALU.add, r=[zp, y, G.ppt], w=[y])
        k.tt("dve", y.ap, y.ap, bg.ap, ALU.mult, r=[y, bg], w=[y])
    finish_norm(k, G, ys, GAP, "rms", l, 1024, "norm_c_g")
    k.release(m)


def mixer_lru(k, G, l):
    m = k.mark()
    GAP = 6
    NU = T + GAP
    XW = NU + 6
    ys = [k.sb([128, NU], F32, "yD") for _ in range(4)]
    xp = k.sb([128, XW], F32, "xpD")
    xcv = k.sb([128, NU], F32, "xcvD")
    ra = k.sb([128, NU], F32, "raD")
    gx = k.sb([128, NU], F32, "gxD")
    sq = k.sb([128, NU], F32, "sqD")
    hd = [k.sb([128, NU], F32, "hD") for _ in range(2)]
    gt = k.sb([128, NU], F32, "gtD")
    gt2 = k.sb([128, NU], F32, "gt2D")
    bd_a = k.sb([128, 128], F32, "bdA")
    bd_x = k.sb([128, 128], F32, "bdX")
    negsp = k.sb([128, 8], F32, "negsp")
    one_c = k.sb([128, 1], F32, "onec")
    pss = [k.ps([128, 512], F32, "psD") for _ in range(4)]
    k.op("pool", lambda e: e.memset(one_c.ap, 1.0), w=[one_c])
    k.op("pool", lambda e: e.memset(xp.ap, 0.0), w=[xp])
    k.op("pool", lambda e: e.memset(gt.ap, 0.0), w=[gt])
    for hh in hd:
        k.op("pool", lambda e: e.memset(hh.ap, 0.0), w=[hh])
    k.op("pool", lambda e: e.memset(bd_a.ap, 0.0), w=[bd_a])
    k.op("pool", lambda e: e.memset(bd_x.ap, 0.0), w=[bd_x])
    lo, _ = PP_COLS[f"lru_lam{l}"]
    k.act(negsp.ap, G.ppt.ap[:, lo:lo + 8], AF.Exp, r=[G.ppt], w=[negsp], scale=-1.0)
    k.act(negsp.ap, negsp.ap, AF.Ln, r=[negsp, one_c], w=[negsp], bias=one_c.ap)
    k.ts("dve", negsp.ap, negsp.ap, -8.0, ALU.mult, r=[negsp], w=[negsp])
    wo, _ = PP_COLS[f"lru_conv_w{l}"]
    uchunks = tok_chunks(0, NU, 512)
    pi = 0
    for c in range(4):
        load_gapped(k, G, xp, 3, 9 + CTX, G.UF, 3072 + c * 128, q="sp")
        load_gapped(k, G, gt, 0, CTX + GAP, G.UF, 2560 + c * 128, q="act")
        for d in range(2):
            col = d * 4 + c
            sh = 0 if d == 0 else 3
            wcol = lambda kk: G.ppt.ap[:, wo + col * 4 + kk:wo + col * 4 + kk + 1]
            k.ts("dve", xcv.ap, xp.ap[:, sh:sh + NU], wcol(0), ALU.mult, ppcol(G, f"lru_conv_b{l}", col), ALU.add,
                 r=[xp, G.ppt], w=[xcv])
            for kk in range(1, 4):
                k.stt(xcv.ap, xp.ap[:, sh + kk:sh + kk + NU], wcol(kk), xcv.ap, ALU.mult, ALU.add,
                      r=[xp, xcv, G.ppt], w=[xcv])
            for (bd, wsrc) in ((bd_a, G.lru_wa), (bd_x, G.lru_wx)):
                k.dma("sp", bd.ap[0:64, 0:64], wsrc.ap[l, d, 2 * c], r=[wsrc], w=[bd])
                k.dma("sp", bd.ap[64:128, 64:128], wsrc.ap[l, d, 2 * c + 1], r=[wsrc], w=[bd])
            for (u0, n) in uchunks:
                pa = pss[pi % 4]
                px = pss[(pi + 1) % 4]
                pi += 2
                k.mm(pa.ap[:, :n], bd_a.ap, xcv.ap[:, u0:u0 + n], start=True, stop=True, r=[bd_a, xcv], w=[pa])
                k.mm(px.ap[:, :n], bd_x.ap, xcv.ap[:, u0:u0 + n], start=True, stop=True, r=[bd_x, xcv], w=[px])
                k.act(ra.ap[:, u0:u0 + n], pa.ap[:, :n], AF.Sigmoid, r=[pa, G.ppt], w=[ra],
                      bias=ppcol(G, f"lru_ba{l}", col))
                k.act(gx.ap[:, u0:u0 + n], px.ap[:, :n], AF.Sigmoid, r=[px, G.ppt], w=[gx],
                      bias=ppcol(G, f"lru_bx{l}", col))
            k.act(ra.ap, ra.ap, AF.Exp, r=[ra, negsp], w=[ra], scale=negsp.ap[:, col:col + 1])
            k.tt("pool", gx.ap, gx.ap, xcv.ap, ALU.mult, r=[gx, xcv], w=[gx])
            k.tt("dve", sq.ap, ra.ap, ra.ap, ALU.mult, r=[ra], w=[sq])
            k.act(sq.ap, sq.ap, AF.Sqrt, r=[sq, one_c], w=[sq], scale=-1.0, bias=one_c.ap)
            k.tt("dve", gx.ap, gx.ap, sq.ap, ALU.mult, r=[gx, sq], w=[gx])
            h = hd[d]
            if d == 0:
                k.op("dve", lambda e: e.tensor_tensor_scan(out=h.ap[:, 0:CTX], data0=ra.ap[:, 0:CTX],
                                                           data1=gx.ap[:, 0:CTX], initial=0.0,
                                                           op0=ALU.mult, op1=ALU.add), r=[ra, gx], w=[h])
                k.op("dve", lambda e: e.tensor_tensor_scan(out=h.ap[:, CTX + GAP:NU], data0=ra.ap[:, CTX + GAP:NU],
                                                           data1=gx.ap[:, CTX + GAP:NU],
                                                           initial=h.ap[:, CTX - 1:CTX],
                                                           op0=ALU.mult, op1=ALU.add), r=[ra, gx, h], w=[h])
            else:
                k.op("dve", lambda e: e.tensor_tensor_scan(out=h.ap[:, CTX - 1::-1], data0=ra.ap[:, CTX - 1::-1],
                                                           data1=gx.ap[:, CTX - 1::-1], initial=0.0,
                                                           op0=ALU.mult, op1=ALU.add), r=[ra, gx], w=[h])
                lo_ = CTX + GAP - 1
                k.op("dve", lambda e: e.tensor_tensor_scan(out=h.ap[:, NU - 1:lo_:-1], data0=ra.ap[:, NU - 1:lo_:-1],
                                                           data1=gx.ap[:, NU - 1:lo_:-1],
                                                           initial=h.ap[:, 0:1],
                                                           op0=ALU.mult, op1=ALU.add), r=[ra, gx, h], w=[h])
        y = ys[c]
        k.act(gt2.ap, gt.ap, AF.Square, r=[gt], w=[gt2])
        k.ts("dve", gt2.ap, gt2.ap, 0.044715, ALU.mult, 1.0, ALU.add, r=[gt2], w=[gt2])
        k.tt("dve", gt2.ap, gt2.ap, gt.ap, ALU.mult, r=[gt2, gt], w=[gt2])
        k.act(gt2.ap, gt2.ap, AF.Sigmoid, r=[gt2], w=[gt2], scale=1.5957691216057308)
        k.tt("dve", gt2.ap, gt2.ap, gt.ap, ALU.mult, r=[gt2, gt], w=[gt2])
        k.tt("pool", y.ap[:, 0:CTX], hd[0].ap[:, 0:CTX], hd[1].ap[:, 0:CTX], ALU.add, r=[hd[0], hd[1]], w=[y])
        k.tt("pool", y.ap[:, CTX:NU], hd[0].ap[:, CTX:NU], hd[1].ap[:, CTX:NU], ALU.add, r=[hd[0], hd[1]], w=[y])
        k.tt("dve", y.ap, y.ap, gt2.ap, ALU.mult, r=[y, gt2], w=[y])
    finish_norm(k, G, ys, GAP, "rms", l, 1536, "norm_d_g")
    k.release(m)


def mixer_attn(k, G, l, need_ctx):
    m = k.mark()
    lam_init = 0.8 - 0.6 * math.exp(-0.3 * l)
    dl = k.sb([128, 256], F32, "dlam")
    k.dma("sp", dl.ap, G.diff_lambda.ap[l:l + 1, :].to_broadcast([128, 256]), r=[G.diff_lambda], w=[dl])
    pr = k.sb([128, 128], F32, "dlpr")
    s2 = k.sb([128, 2], F32, "dls")
    dlv = dl.ap.rearrange("p (a b) -> p a b", a=4)
    k.tt("dve", pr.ap[:, 0:64], dlv[:, 0, :], dlv[:, 1, :], ALU.mult, r=[dl], w=[pr])
    k.tt("dve", pr.ap[:, 64:128], dlv[:, 2, :], dlv[:, 3, :], ALU.mult, r=[dl], w=[pr])
    k.op("dve", lambda e: e.reduce_sum(out=s2.ap, in_=pr.ap.rearrange("p (a b) -> p a b", a=2), axis=AX.X),
         r=[pr], w=[s2])
    k.act(s2.ap, s2.ap, AF.Exp, r=[s2], w=[s2])
    neg_lam = k.sb([128, 1], F32, "neglam")
    k.tt("dve", neg_lam.ap, s2.ap[:, 1:2], s2.ap[:, 0:1], ALU.subtract, r=[s2], w=[neg_lam])
    k.ts("dve", neg_lam.ap, neg_lam.ap, -lam_init, ALU.add, r=[neg_lam], w=[neg_lam])
    gsc = k.sb([128, 1], F32, "gsc")
    k.ts("dve", gsc.ap, ppcol(G, f"diff_norm_g{l}"), 1.0 - lam_init, ALU.mult, r=[G.ppt], w=[gsc])
    ones_b = k.sb([128, 128], BF16, "onesb")
    k.copy("dve", ones_b.ap, G.ones_f.ap, r=[G.ones_f], w=[ones_b])
    vt = k.sb([128, NT, 512], BF16, "vtm")
    k.dma("sp", vt.ap, G.V.ap.rearrange("(t p) e -> p t e", p=128), r=[G.V], w=[vt])
    qT = [k.sb([64, T], BF16, "qT") for _ in range(2)]
    kT = [k.sb([64, T], BF16, "kT") for _ in range(2)]
    pts = [k.sb([128, 512], BF16, "pexp") for _ in range(3)]
    ps_s = [k.ps([128, 512], F32, "ps_s") for _ in range(2)]
    ps_acc = [k.ps([128, 512], F32, "ps_acc") for _ in range(2)]
    ps_den = [k.ps([128, 512], F32, "ps_den") for _ in range(2)]
    ps_n = k.ps([128, 512], F32, "ps_n")
    rden = [k.sb([128, 512], F32, "rden") for _ in range(2)]
    tnum = [k.sb([128, 512], F32, "tnum") for _ in range(2)]
    osb = k.sb([128, 512], F32, "osb")
    osq = k.sb([128, 512], F32, "osq")
    rs = k.sb([128, 512], F32, "rsb")
    stage = [k.sb([128, T], BF16, "ystB") for _ in range(2)]
    si = 0
    pi = 0
    for h in range(4):
        for mi in range(2):
            j = 2 * h + mi
            k.dma("sp", qT[mi].ap, G.QK.ap[j * 64:(j + 1) * 64, :], r=[G.QK], w=[qT[mi]])
            k.dma("act", kT[mi].ap, G.QK.ap[512 + j * 64:512 + (j + 1) * 64, :], r=[G.QK], w=[kT[mi]])
        st = stage[h % 2]
        qchunks = [(t0, n, NT) for (t0, n) in tok_chunks(CTX, T, 512)]
        if need_ctx:
            qchunks = [(0, CTX, 2)] + qchunks
        for (t0, n, nkt) in qchunks:
            for mi in range(2):
                for kt in range(nkt):
                    pss = ps_s[si % 2]
                    si += 1
                    k.mm(pss.ap[:, :n], kT[mi].ap[:, kt * 128:(kt + 1) * 128], qT[mi].ap[:, t0:t0 + n],
                         start=True, stop=True, r=[kT[mi], qT[mi]], w=[pss])
                    pt = pts[pi % 3]
                    pi += 1
                    k.act(pt.ap[:, :n], pss.ap[:, :n], AF.Exp, r=[pss], w=[pt], scale=0.125)
                    k.mm(ps_acc[mi].ap[:, :n], vt.ap[:, kt, h * 128:(h + 1) * 128], pt.ap[:, :n],
                         start=(kt == 0), stop=(kt == nkt - 1), r=[vt, pt], w=[ps_acc[mi]])
                    k.mm(ps_den[mi].ap[:, :n], ones_b.ap, pt.ap[:, :n],
                         start=(kt == 0), stop=(kt == nkt - 1), r=[ones_b, pt], w=[ps_den[mi]])
            for mi in range(2):
                k.op("dve", lambda e: e.reciprocal(out=rden[mi].ap[:, :n], in_=ps_den[mi].ap[:, :n]),
                     r=[ps_den[mi]], w=[rden[mi]])
                k.tt("dve", tnum[mi].ap[:, :n], ps_acc[mi].ap[:, :n], rden[mi].ap[:, :n], ALU.mult,
                     r=[ps_acc[mi], rden[mi]], w=[tnum[mi]])
            k.stt(osb.ap[:, :n], tnum[1].ap[:, :n], neg_lam.ap, tnum[0].ap[:, :n], ALU.mult, ALU.add,
                  r=[tnum[0], tnum[1], neg_lam], w=[osb])
            k.act(osq.ap[:, :n], osb.ap[:, :n], AF.Square, r=[osb], w=[osq])
            k.mm(ps_n.ap[:, :n], G.ones_f.ap, osq.ap[:, :n], start=True, stop=True, r=[G.ones_f, osq], w=[ps_n])
            k.act(rs.ap[:, :n], ps_n.ap[:, :n], AF.Sqrt, r=[ps_n, G.eps], w=[rs], scale=1.0 / 128, bias=G.eps.ap)
            k.op("dve", lambda e: e.reciprocal(out=rs.ap[:, :n], in_=rs.ap[:, :n]), r=[rs], w=[rs])
            k.stt(st.ap[:, t0:t0 + n], osb.ap[:, :n], gsc.ap, rs.ap[:, :n], ALU.mult, ALU.mult,
                  r=[osb, gsc, rs], w=[st])
        if need_ctx:
            k.dma("sp", G.YC.ap[512 + h * 128:512 + (h + 1) * 128, :], st.ap, r=[st], w=[G.YC])
        else:
            k.dma("sp", G.YC.ap[512 + h * 128:512 + (h + 1) * 128, CTX:T], st.ap[:, CTX:T], r=[st], w=[G.YC])
    k.release(m)


MIXSEL = "bacd"


def phase_mixers(k, G, l, need_ctx):
    if "b" in MIXSEL:
        mixer_attn(k, G, l, need_ctx)
    if "a" in MIXSEL:
        mixer_conformer(k, G, l)
    if "c" in MIXSEL:
        mixer_sconv(k, G, l)
    if "d" in MIXSEL:
        mixer_lru(k, G, l)


def phase_outproj(k, G, l, need_ctx):
    m = k.mark()
    wo = k.sb([128, KC, D], BF16, "wout")
    wsrc = G.WOB[l].ap.rearrange("p (kc n) -> p kc n", kc=KC)
    for j in range(4):
        k.dma("act", wo.ap[:, j * 4:(j + 1) * 4, :], wsrc[:, j * 4:(j + 1) * 4, :], r=[G.WOB[l]], w=[wo])
    rw = k.sb([128, KC, NE], BF16, "rw")
    k.dma("act", rw.ap, G.RWB[l].ap.rearrange("p (kc e) -> p kc e", kc=KC), r=[G.RWB[l]], w=[rw])
    rb = k.sb([128, NE], F32, "rb")
    k.dma("sp", rb.ap, G.router_b.ap[l:l + 1, :].to_broadcast([128, NE]), r=[G.router_b], w=[rb])
    g1 = k.sb([128, D], F32, "g1b")
    gs2 = k.sb([128, D], F32, "gs2b")
    sh2 = k.sb([128, D], F32, "sh2b")
    ycs = [k.sb([128, KC, 512], BF16, "ycblk") for _ in range(2)]
    xts = [k.sb([128, D], F32, "xt") for _ in range(2)]
    tmp = k.sb([128, D], F32, "tmp")
    scr = k.sb([128, D], BF16, "scr")
    hbs = [k.sb([128, D], BF16, "hb") for _ in range(2)]
    h2s = [k.sb([128, KC, 128], BF16, "h2s") for _ in range(2)]
    ssq = k.sb([128, 1], F32, "ssq")
    rstd = k.sb([128, 1], F32, "rstd")
    lg = k.sb([128, NE], F32, "lg")
    ex = k.sb([128, NE], F32, "ex")
    msk = k.sb([128, NE], F32, "msk")
    top8 = k.sb([128, 8], F32, "top8")
    nmx = k.sb([128, 1], F32, "nmx")
    ssum = k.sb([128, 1], F32, "ssum")
    cmb = [k.sb([128, NE], F32, "cmb") for _ in range(2)]
    ps_y = [k.ps([128, 512], F32, "psy") for _ in range(4)]
    pts = [k.ps([128, 1024], BF16, "pt") for _ in range(2)]
    ps_l = k.ps([128, NE], F32, "psl")
    ps_r = k.ps([128, 2 * NE], F32, "psr")
    rks = [k.sb([128, NE], F32, "rk") for _ in range(2)]
    lgs_ = [k.sb([128, NE], F32, "lgc") for _ in range(2)]
    t8s = [k.sb([128, 8], F32, "t8c") for _ in range(2)]
    cnt = k.sb([128, NE], F32, "cnt")
    k.op("dve", lambda e: e.memset(cnt.ap, 0.0), w=[cnt])
    lto, _ = PP_COLS["ltri"]
    ltri = G.ppt.ap[:, lto:lto + 128]
    mv = G.modv[l]
    tiles = list(range(NT)) if need_ctx else list(range(2, NT))
    cur_which = None
    cur_blk = None
    nblk = 0
    for i in tiles:
        which = 1 if i < 2 else 0
        if which != cur_which:
            for (t, sec) in ((g1, 2), (gs2, 3), (sh2, 4)):
                k.dma("sp", t.ap, mv.ap[which:which + 1, sec, :].to_broadcast([128, D]), r=[mv], w=[t])
            cur_which = which
        blk = i // 4
        if blk != cur_blk:
            yc = ycs[nblk % 2]
            nblk += 1
            nt_ = min(512, T - blk * 512)
            for kc in range(KC):
                k.dma("sp" if kc % 2 == 0 else "act", yc.ap[:, kc, :nt_],
                      G.YC.ap[kc * 128:(kc + 1) * 128, blk * 512:blk * 512 + nt_], r=[G.YC], w=[yc])
            cur_blk = blk
        xt = xts[i % 2]
        hb = hbs[i % 2]
        h2 = h2s[i % 2]
        cb_ = cmb[i % 2]
        k.dma("sp", xt.ap, G.xres.ap[i * 128:(i + 1) * 128, :], r=[G.xres], w=[xt])
        off = (i % 4) * 128
        for dblk in range(4):
            ps = ps_y[dblk]
            for kc in range(KC):
                k.mm(ps.ap, yc.ap[:, kc, off:off + 128], wo.ap[:, kc, dblk * 512:(dblk + 1) * 512],
                     start=(kc == 0), stop=(kc == KC - 1), r=[yc, wo], w=[ps])
            sl = slice(dblk * 512, (dblk + 1) * 512)
            k.tt("dve", tmp.ap[:, sl], ps.ap, g1.ap[:, sl], ALU.mult, r=[ps, g1], w=[tmp])
            k.tt("pool", xt.ap[:, sl], xt.ap[:, sl], tmp.ap[:, sl], ALU.add, r=[xt, tmp], w=[xt])
        k.dma("sp", G.xres.ap[i * 128:(i + 1) * 128, :], xt.ap, r=[xt], w=[G.xres])
        norm_mod_tile(k, G, xt, gs2, sh2, hb, scr, ssq, rstd, tmp)
        for half in range(2):
            pt = pts[half]
            for j in range(8):
                kc = half * 8 + j
                k.op("pe", lambda e: e.transpose(out=pt.ap[:, j * 128:(j + 1) * 128],
                                                 in_=hb.ap[:, kc * 128:(kc + 1) * 128], identity=G.ident_b.ap),
                     r=[hb, G.ident_b], w=[pt])
            k.copy("act" if half == 0 else "dve", h2.ap[:, half * 8:(half + 1) * 8, :],
                   pt.ap.rearrange("p (a b) -> p a b", a=8), r=[pt], w=[h2])
        k.dma("sp", G.H2TM.ap[i * 128:(i + 1) * 128, :], hb.ap, r=[hb], w=[G.H2TM])
        for kc in range(KC):
            k.mm(ps_l.ap, h2.ap[:, kc, :], rw.ap[:, kc, :], start=(kc == 0), stop=(kc == KC - 1),
                 r=[h2, rw], w=[ps_l])
        k.tt("dve", lg.ap, ps_l.ap, rb.ap, ALU.add, r=[ps_l, rb], w=[lg])
        k.op("dve", lambda e: e.max(out=top8.ap, in_=lg.ap), r=[lg], w=[top8])
        k.ts("dve", msk.ap, lg.ap, top8.ap[:, 3:4], ALU.is_ge, r=[lg, top8], w=[msk])
        k.ts("dve", nmx.ap, top8.ap[:, 0:1], -1.0, ALU.mult, r=[top8], w=[nmx])
        k.act(ex.ap, lg.ap, AF.Exp, r=[lg, nmx], w=[ex], bias=nmx.ap)
        k.tt("dve", ex.ap, ex.ap, msk.ap, ALU.mult, r=[ex, msk], w=[ex])
        k.op("dve", lambda e: e.reduce_sum(out=ssum.ap, in_=ex.ap, axis=AX.X), r=[ex], w=[ssum])
        k.op("dve", lambda e: e.reciprocal(out=ssum.ap, in_=ssum.ap), r=[ssum], w=[ssum])
        k.ts("dve", cb_.ap, ex.ap, ssum.ap, ALU.mult, r=[ex, ssum], w=[cb_])
        k.dma("sp", G.COMB.ap[i * 128:(i + 1) * 128, :], cb_.ap, r=[cb_], w=[G.COMB])
        rk, lgc, t8c = rks[i % 2], lgs_[i % 2], t8s[i % 2]
        k.mm(ps_r.ap[:, 0:NE], ltri, msk.ap, start=True, stop=True, r=[G.ppt, msk], w=[ps_r])
        k.mm(ps_r.ap[:, NE:2 * NE], G.ones_f.ap, msk.ap, start=True, stop=True, r=[G.ones_f, msk], w=[ps_r])
        k.tt("dve", rk.ap, ps_r.ap[:, 0:NE], cnt.ap, ALU.add, r=[ps_r, cnt], w=[rk])
        k.tt("dve", cnt.ap, ps_r.ap[:, NE:2 * NE], cnt.ap, ALU.add, r=[ps_r, cnt], w=[cnt])
        k.copy("dve", lgc.ap, lg.ap, r=[lg], w=[lgc])
        k.copy("dve", t8c.ap, top8.ap, r=[top8], w=[t8c])
        k.dma("sp", G.RANKD.ap[i * 128:(i + 1) * 128, :], rk.ap, r=[rk], w=[G.RANKD])
        k.dma("sp", G.LGD.ap[i * 128:(i + 1) * 128, :], lgc.ap, r=[lgc], w=[G.LGD])
        k.dma("sp", G.TOPD.ap[i * 128:(i + 1) * 128, :], t8c.ap, r=[t8c], w=[G.TOPD])
    k.dma("sp", G.CNTD.ap, cnt.ap, r=[cnt], w=[G.CNTD])
    k.release(m)


class WStream:
    def __init__(self, k, bufs, srcs, q="pool", hold=1):
        self.k, self.bufs, self.srcs, self.q, self.hold = k, bufs, srcs, q, hold
        self.nxt = 0

    def get(self, n):
        nb = len(self.bufs)
        while self.nxt < len(self.srcs) and self.nxt <= n + nb - self.hold:
            j = self.nxt
            out_fn, in_ap, srcbuf = self.srcs[j]
            b = self.bufs[j % nb]
            self.k.dma(self.q, out_fn(b), in_ap, r=[srcbuf], w=[b])
            self.nxt += 1
        return self.bufs[n % nb]


def precast_outproj(k, G, l):
    wov = G.w_out.ap[l].rearrange("(kc p) n -> p kc n", p=128)
    dst = G.WOB[l].ap.rearrange("p (kc n) -> p kc n", kc=KC)
    for j in range(4):
        k.dma("pool", dst[:, :, j * 512:(j + 1) * 512], wov[:, :, j * 512:(j + 1) * 512], r=[G.w_out], w=[G.WOB[l]])
    k.dma("pool", G.RWB[l].ap.rearrange("p (kc e) -> p kc e", kc=KC),
          G.router_w.ap[l].rearrange("(kc p) e -> p kc e", p=128), r=[G.router_w], w=[G.RWB[l]])


def precast_win(k, G, l):
    wv = G.w_in.ap[l].rearrange("(kc p) n -> p kc n", p=128)
    for cb in range(10):
        k.dma("pool", G.WIB[l].ap[cb].rearrange("p (kc c) -> p kc c", kc=KC), wv[:, :, cb * 512:(cb + 1) * 512],
              r=[G.w_in], w=[G.WIB[l]])


def precast_list(k, G, l):
    w1v = G.exp_w1.ap[l].rearrange("e (kc p) n -> e p kc n", p=128)
    w2v = G.exp_w2.ap[l].rearrange("e (fc p) n -> e p fc n", p=128)
    out = []
    for e in range(NE):
        for cbk in range(4):
            out.append(lambda e=e, cbk=cbk: k.bgdma(
                G.W1B[l][cbk].ap[e * 128:(e + 1) * 128, :].rearrange("p (kc c) -> p kc c", kc=KC),
                w1v[e][:, :, cbk * 512:(cbk + 1) * 512]))
        for hf in range(2):
            out.append(lambda e=e, hf=hf: k.bgdma(
                G.W2B[l][hf].ap[e * 128:(e + 1) * 128, :].rearrange("p (fc c) -> p fc c", fc=4),
                w2v[e][:, hf * 4:(hf + 1) * 4, :]))
    return out


def pump(G, n):
    for _ in range(n):
        if G.bg:
            G.bg.pop(0)()


def phase_moe(k, G, l, need_ctx, last):
    m0 = k.mark()
    tiles = list(range(NT)) if need_ctx else list(range(2, NT))
    tb, ntt = tiles[0], len(tiles)
    NTL = ntt * 4 + NE
    NSL = NTL * 128
    tsl = slice(tb * 128, (tb + ntt) * 128)

    def tload(src, width, name):
        t = k.sb([128, ntt, width], F32, name)
        k.dma("sp", t.ap, src.ap[tsl, :].rearrange("(t p) e -> p t e", p=128), r=[src], w=[t])
        return t

    lgs = tload(G.LGD, NE, "lgs")
    cmbs = tload(G.COMB, NE, "cmbs")
    rks = tload(G.RANKD, NE, "rks")
    tops = tload(G.TOPD, 8, "tops")
    cnt = k.sb([128, NE], F32, "cntm")
    k.dma("sp", cnt.ap, G.CNTD.ap, r=[G.CNTD], w=[cnt])
    io_, _ = PP_COLS["iota"]
    iota = G.ppt.ap[:, io_:io_ + NTL]
    pidx = ppcol(G, "pidx")
    ntile = k.sb([128, NE], F32, "ntile")
    ones32 = k.sb([128, NE], F32, "ones32")
    incl = k.sb([128, NE], F32, "incl")
    base = k.sb([128, NE], F32, "base")
    k.op("dve", lambda e: e.memset(ntile.ap, 0.0), w=[ntile])
    k.op("dve", lambda e: e.memset(ones32.ap, 1.0), w=[ones32])
    for mth in range(ntt):
        k.stt(ntile.ap, cnt.ap, 128.0 * mth, ntile.ap, ALU.is_gt, ALU.add, r=[cnt, ntile], w=[ntile])
    k.op("dve", lambda e: e.tensor_tensor_scan(out=incl.ap, data0=ones32.ap, data1=ntile.ap, initial=0.0,
                                               op0=ALU.mult, op1=ALU.add), r=[ones32, ntile], w=[incl])
    k.tt("dve", base.ap, incl.ap, ntile.ap, ALU.subtract, r=[incl, ntile], w=[base])
    k.ts("dve", base.ap, base.ap, 128.0, ALU.mult, r=[base], w=[base])
    posf = k.sb([128, ntt, 4], F32, "posf")
    gat = k.sb([128, ntt, 4], F32, "gat")
    posi = k.sb([128, ntt, 4], I32, "posi")
    pf = k.sb([128, NE], F32, "pf")
    ohs = [k.sb([128, NE], F32, "oh") for _ in range(2)]
    sc1 = [k.sb([128, NE], F32, "sc1") for _ in range(2)]
    sc2 = [k.sb([128, NE], F32, "sc2") for _ in range(2)]
    n = 0
    for ti in range(ntt):
        k.tt("dve", pf.ap, rks.ap[:, ti, :], base.ap, ALU.add, r=[rks, base], w=[pf])
        for kk in range(4):
            oh, s1_, s2_ = ohs[n % 2], sc1[n % 2], sc2[n % 2]
            n += 1
            k.ts("dve", oh.ap, lgs.ap[:, ti, :], tops.ap[:, ti, kk:kk + 1], ALU.is_equal, r=[lgs, tops], w=[oh])
            k.tt("dve", s1_.ap, oh.ap, pf.ap, ALU.mult, r=[oh, pf], w=[s1_])
            k.op("dve", lambda e: e.reduce_sum(out=posf.ap[:, ti, kk:kk + 1], in_=s1_.ap, axis=AX.X),
                 r=[s1_], w=[posf])
            k.tt("dve", s2_.ap, oh.ap, cmbs.ap[:, ti, :], ALU.mult, r=[oh, cmbs], w=[s2_])
            k.op("dve", lambda e: e.reduce_sum(out=gat.ap[:, ti, kk:kk + 1], in_=s2_.ap, axis=AX.X),
                 r=[s2_], w=[gat])
    k.copy("dve", posi.ap, posf.ap, r=[posf], w=[posi])
    texp = k.sb([128, NTLMAX], F32, "texp")
    widf = k.sb([128, NTLMAX], F32, "widf")
    bidf = k.sb([128, NTLMAX], F32, "bidf")
    eq = k.sb([128, NTLMAX], F32, "eqt")
    widx = k.sb([128, NTLMAX], I32, "widx")
    bidx = k.sb([128, NTLMAX], I32, "bidx")
    k.op("dve", lambda e: e.memset(texp.ap, 0.0), w=[texp])
    k.op("dve", lambda e: e.memset(eq.ap, 0.0), w=[eq])
    for e_ in range(NE):
        k.stt(texp.ap[:, :NTL], iota, incl.ap[:, e_:e_ + 1], texp.ap[:, :NTL], ALU.is_ge, ALU.add,
              r=[G.ppt, incl, texp], w=[texp])
    k.ts("dve", widf.ap, texp.ap, 128.0, ALU.mult, pidx, ALU.add, r=[texp, G.ppt], w=[widf])
    k.tt("dve", eq.ap[:, 1:NTL], texp.ap[:, 1:NTL], texp.ap[:, 0:NTL - 1], ALU.is_equal, r=[texp], w=[eq])
    k.stt(widf.ap, eq.ap, BIG, widf.ap, ALU.mult, ALU.add, r=[eq, widf], w=[widf])
    k.copy("dve", widx.ap, widf.ap, r=[widf], w=[widx])
    k.ts("dve", bidf.ap, texp.ap, float(NE - 1), ALU.min, 128.0, ALU.mult, r=[texp], w=[bidf])
    k.ts("dve", bidf.ap, bidf.ap, pidx, ALU.add, r=[bidf, G.ppt], w=[bidf])
    k.copy("dve", bidx.ap, bidf.ap, r=[bidf], w=[bidx])
    ms = k.mark()
    xsc = [k.sb([128, D], BF16, "xsc") for _ in range(3)]
    for ti, i in enumerate(tiles):
        xt = xsc[ti % 3]
        k.dma("sp", xt.ap, G.H2TM.ap[i * 128:(i + 1) * 128, :], r=[G.H2TM], w=[xt])
        for kk in range(4):
            k.idma(out=G.XS.ap, in_=xt.ap, out_off=posi.ap[:, ti, kk:kk + 1], bounds=NSL - 1,
                   r=[xt, posi], w=[G.XS])
    k.release(ms)
    me = k.mark()
    w1 = [k.sb([128, KC, 512], BF16, "w1res") for _ in range(4)]
    w2 = [k.sb([128, 4, D], BF16, "w2res") for _ in range(2)]
    b1s = [k.sb([128, 16], F32, "b1s") for _ in range(2)]
    xs = [k.sb([128, D], BF16, "xs") for _ in range(3)]
    h2s = [k.sb([128, KC, 128], BF16, "h2s") for _ in range(2)]
    ats = [k.sb([128, 8, 128], BF16, "actT") for _ in range(2)]
    yts = [k.sb([128, D], F32, "yt") for _ in range(2)]
    gt = [k.sb([128, 128], F32, "g") for _ in range(2)]
    sg = [k.sb([128, 128], F32, "sig") for _ in range(2)]
    u1 = [k.sb([128, 128], F32, "u1") for _ in range(2)]
    pts = [k.ps([128, 1024], BF16, "pt") for _ in range(2)]
    ps_gu = [k.ps([128, 512], F32, "psgu") for _ in range(2)]
    ps_o = [k.ps([128, 512], F32, "pso") for _ in range(4)]
    NROW = NE * 128

    def load_x(j):
        xt = xs[j % 3]
        k.dma("sp", xt.ap, G.XS.ap[j * 128:(j + 1) * 128, :], r=[G.XS], w=[xt])

    def transp(j):
        xt, h2 = xs[j % 3], h2s[j % 2]
        for half in range(2):
            pt = pts[half]
            for jj in range(8):
                kc = half * 8 + jj
                k.op("pe", lambda e: e.transpose(out=pt.ap[:, jj * 128:(jj + 1) * 128],
                                                 in_=xt.ap[:, kc * 128:(kc + 1) * 128], identity=G.ident_b.ap),
                     r=[xt, G.ident_b], w=[pt])
            k.copy("act" if half == 0 else "dve", h2.ap[:, half * 8:(half + 1) * 8, :],
                   pt.ap.rearrange("p (a b) -> p a b", a=8), r=[pt], w=[h2])

    def gather_w1(j):
        for cbk in range(4):
            k.idma(out=w1[cbk].ap.rearrange("p kc c -> p (kc c)"), in_=G.W1B[l][cbk].ap, in_off=widx.ap[:, j:j + 1],
                   bounds=NROW - 1, r=[G.W1B[l][cbk], widx], w=[w1[cbk]])

    def gather_w2(j):
        for hf in range(2):
            k.idma(out=w2[hf].ap.rearrange("p fc c -> p (fc c)"), in_=G.W2B[l][hf].ap, in_off=widx.ap[:, j:j + 1],
                   bounds=NROW - 1, r=[G.W2B[l][hf], widx], w=[w2[hf]])

    def gather_b(j):
        bb = b1s[j % 2]
        k.idma(out=bb.ap, in_=G.b1t[l].ap, in_off=bidx.ap[:, j:j + 1], bounds=NROW - 1, r=[G.b1t[l], bidx], w=[bb])
        k.ts("dve", bb.ap[:, 8:16], bb.ap[:, 8:16], 1.0, ALU.add, r=[bb], w=[bb])

    fi = [0]

    def first(j):
        at, h2, bb = ats[j % 2], h2s[j % 2], b1s[j % 2]
        for cbk in range(4):
            wb = w1[cbk]
            for s in range(2):
                fc = cbk * 2 + s
                pgu = ps_gu[fi[0] % 2]
                g, sig, uu = gt[fi[0] % 2], sg[fi[0] % 2], u1[fi[0] % 2]
                fi[0] += 1
                wsl = wb.ap[:, :, s * 256:(s + 1) * 256].rearrange("p kc (f two) -> p kc f two", two=2)
                for kc in range(KC):
                    k.mm(pgu.ap[:, 0:128], wsl[:, kc, :, 0], h2.ap[:, kc, :], start=(kc == 0),
                         stop=(kc == KC - 1), r=[wb, h2], w=[pgu])
                for kc in range(KC):
                    k.mm(pgu.ap[:, 128:256], wsl[:, kc, :, 1], h2.ap[:, kc, :], start=(kc == 0),
                         stop=(kc == KC - 1), r=[wb, h2], w=[pgu])
                k.ts("dve", g.ap, pgu.ap[:, 0:128], bb.ap[:, fc:fc + 1], ALU.add, 7.0, ALU.min, r=[pgu, bb], w=[g])
                k.act(sig.ap, g.ap, AF.Sigmoid, r=[g], w=[sig], scale=1.702)
                k.ts("dve", uu.ap, pgu.ap[:, 128:256], bb.ap[:, 8 + fc:9 + fc], ALU.add, -6.0, ALU.max,
                     r=[pgu, bb], w=[uu])
                k.tt("dve", g.ap, g.ap, sig.ap, ALU.mult, r=[g, sig], w=[g])
                k.stt(at.ap[:, fc, :], uu.ap, 8.0, g.ap, ALU.min, ALU.mult, r=[uu, g], w=[at])

    oi = [0]

    def second(j):
        at, yt = ats[j % 2], yts[j % 2]
        for dblk in range(4):
            po = ps_o[oi[0] % 4]
            oi[0] += 1
            for fc in range(8):
                wb = w2[fc // 4]
                k.mm(po.ap, at.ap[:, fc, :], wb.ap[:, fc % 4, dblk * 512:(dblk + 1) * 512], start=(fc == 0),
                     stop=(fc == 7), r=[at, wb], w=[po])
            k.copy("act" if dblk % 2 == 0 else "dve", yt.ap[:, dblk * 512:(dblk + 1) * 512], po.ap, r=[po], w=[yt])
        k.dma("sp", G.YS.ap[j * 128:(j + 1) * 128, :], yt.ap, r=[yt], w=[G.YS])

    k.bg_wait(("pool",))
    load_x(0)
    load_x(1)
    gather_w1(0)
    gather_w2(0)
    gather_b(0)
    transp(0)
    for j in range(NTL):
        if j + 2 < NTL:
            load_x(j + 2)
        if j + 1 < NTL:
            gather_b(j + 1)
            transp(j + 1)
        first(j)
        if j + 1 < NTL:
            gather_w1(j + 1)
        if j > 0:
            second(j - 1)
            gather_w2(j)
        pump(G, 2)
    second(NTL - 1)
    pump(G, len(G.bg))
    k.release(me)
    b2 = k.sb([NE, D], F32, "b2")
    k.dma("sp", b2.ap, G.exp_b2.ap[l], r=[G.exp_b2], w=[b2])
    mv = G.modv[l]
    ygs = [k.sb([128, D], F32, "yg") for _ in range(8)]
    accs = [k.sb([128, D], F32, "acc") for _ in range(2)]
    xts = [k.sb([128, D], F32, "xt") for _ in range(2)]
    combT = k.sb([NE, 128], F32, "combT")
    ps_o = [k.ps([128, 512], F32, "pso") for _ in range(4)]
    g2 = [None, None]
    if last:
        fg = k.sb([128, D], F32, "fgb")
        k.dma("sp", fg.ap, G.final_g.ap.to_broadcast([128, D]), r=[G.final_g], w=[fg])
        scr = k.sb([128, D], BF16, "scr")
        ssq = k.sb([128, 1], F32, "ssq")
        rstd = k.sb([128, 1], F32, "rstd")
        ots = [k.sb([128, D], F32, "ot") for _ in range(2)]
    oi = 0
    for ti, i in enumerate(tiles):
        which = 1 if i < 2 else 0
        if g2[which] is None:
            g2[which] = k.sb([128, D], F32, "g2b")
            k.dma("sp", g2[which].ap, mv.ap[which:which + 1, 5, :].to_broadcast([128, D]), r=[mv], w=[g2[which]])
        yg = [ygs[(ti % 2) * 4 + kk] for kk in range(4)]
        for kk in range(4):
            k.idma(out=yg[kk].ap, in_=G.YS.ap, in_off=posi.ap[:, ti, kk:kk + 1], bounds=NSL - 1,
                   r=[G.YS, posi], w=[yg[kk]])
        xt = xts[ti % 2]
        acc = accs[ti % 2]
        k.dma("sp", xt.ap, G.xres.ap[i * 128:(i + 1) * 128, :], r=[G.xres], w=[xt])
        pT = ps_o[oi % 4]
        oi += 1
        k.op("pe", lambda e: e.transpose(out=pT.ap[:NE, :128], in_=cmbs.ap[:, ti, :], identity=G.ident_f.ap),
             r=[cmbs, G.ident_f], w=[pT])
        k.copy("dve", combT.ap, pT.ap[:NE, :128], r=[pT], w=[combT])
        for dblk in range(4):
            po = ps_o[oi % 4]
            oi += 1
            sl = slice(dblk * 512, (dblk + 1) * 512)
            k.mm(po.ap, combT.ap, b2.ap[:, sl], start=True, stop=True, r=[combT, b2], w=[po])
            k.stt(acc.ap[:, sl], yg[0].ap[:, sl], gat.ap[:, ti, 0:1], po.ap, ALU.mult, ALU.add,
                  r=[yg[0], gat, po], w=[acc])
        for kk in range(1, 4):
            k.stt(acc.ap, yg[kk].ap, gat.ap[:, ti, kk:kk + 1], acc.ap, ALU.mult, ALU.add,
                  r=[yg[kk], gat, acc], w=[acc])
        k.tt("dve", acc.ap, acc.ap, g2[which].ap, ALU.mult, r=[acc, g2[which]], w=[acc])
        k.tt("dve", xt.ap, xt.ap, acc.ap, ALU.add, r=[xt, acc], w=[xt])
        if not last:
            k.dma("sp", G.xres.ap[i * 128:(i + 1) * 128, :], xt.ap, r=[xt], w=[G.xres])
        else:
            ot = ots[ti % 2]
            k.act(scr.ap, xt.ap, AF.Square, r=[xt], w=[scr, ssq], accum_out=ssq.ap)
            k.act(rstd.ap, ssq.ap, AF.Sqrt, r=[ssq, G.eps], w=[rstd], scale=1.0 / D, bias=G.eps.ap)
            k.op("dve", lambda e: e.reciprocal(out=rstd.ap, in_=rstd.ap), r=[rstd], w=[rstd])
            k.stt(ot.ap, xt.ap, rstd.ap, fg.ap, ALU.mult, ALU.mult, r=[xt, rstd, fg], w=[ot])
            k.dma("sp", G.out.ap[(i - 2) * 128:(i - 1) * 128, :], ot.ap, r=[ot], w=[G.out])
    k.release(m0)


_CACHE = {}


def kernel(**inputs):
    n = 8
    if "nc" not in _CACHE:
        _CACHE["nc"] = build_program(upto="all")[0]
    nc = _CACHE["nc"]
    maps = make_in_maps(inputs, list(range(n)))
    res = run_bass_kernel_spmd(nc, maps, core_ids=list(range(n)))
    out = np.stack([np.asarray(r["out"], dtype=np.float32) for r in res.results], axis=0)
    return out
```
